# Optimizing a Trainium2 kernel written in Bass

```python
import math
import jax, jax.numpy as jnp
from jax import lax
import numpy as np

D_MODEL = 1024
BATCH = 32
SEQ = 2048
DEPTH = 2

D_FF = 2816
FFN_RES = 0.5
EPS = 1e-6

SSM_HEADS = 16
SSM_HEAD_DIM = 64
SSM_D = SSM_HEADS * SSM_HEAD_DIM
SSM_GROUPS = 4
SSM_STATE = 128
SSM_CONV = 4
SSM_CHUNK = 128
SSM_XBC = SSM_D + 2 * SSM_GROUPS * SSM_STATE

MLA_HEADS = 8
MLA_Q_RANK = 384
MLA_KV_RANK = 256
MLA_NOPE = 64
MLA_ROPE = 32
MLA_QK = MLA_NOPE + MLA_ROPE
MLA_V = 64
ROPE_THETA = 10000.0

SWA_Q_HEADS = 8
SWA_KV_HEADS = 2
SWA_HD = 64
SWA_WINDOW = 128

GLA_HEADS = 4
GLA_DK = 64
GLA_DV = 128
GLA_RANK = 16
GLA_TAU = 16.0
GLA_CHUNK = 64

Q_BLOCK = 128

IN_EVEN = SSM_D + SSM_XBC + SSM_HEADS + MLA_Q_RANK + MLA_KV_RANK + MLA_ROPE
OUT_EVEN = SSM_D + MLA_HEADS * MLA_V
IN_ODD = (SWA_Q_HEADS * SWA_HD + 2 * SWA_KV_HEADS * SWA_HD
          + 2 * GLA_HEADS * GLA_DK + GLA_HEADS * GLA_DV + GLA_RANK + GLA_HEADS * GLA_DV)
OUT_ODD = SWA_Q_HEADS * SWA_HD + GLA_HEADS * GLA_DV
N_EVEN = (DEPTH + 1) // 2
N_ODD = DEPTH // 2

kernel_name = "hybrid_ssd_mla_swa_gla_macaron"


def split_last(t, sizes):
    idx, acc = [], 0
    for s in sizes[:-1]:
        acc += s
        idx.append(acc)
    return jnp.split(t, idx, axis=-1)


def rms_norm(x, g):
    xf = x.astype(jnp.float32)
    y = xf * lax.rsqrt(jnp.mean(xf * xf, axis=-1, keepdims=True) + EPS)
    return (y * g.astype(jnp.float32)).astype(x.dtype)


def swiglu(h, w_gate, w_up, w_down):
    return (jax.nn.silu(h @ w_gate) * (h @ w_up)) @ w_down


def rope_tables(pos, dim, dtype):
    inv = ROPE_THETA ** (-jnp.arange(0, dim, 2, dtype=jnp.float32) / dim)
    ang = pos.astype(jnp.float32)[..., None] * inv
    return jnp.cos(ang)[:, :, None, :].astype(dtype), jnp.sin(ang)[:, :, None, :].astype(dtype)


def apply_rope(t, cos, sin):
    t1, t2 = jnp.split(t, 2, axis=-1)
    return jnp.concatenate([t1 * cos - t2 * sin, t2 * cos + t1 * sin], axis=-1)


def alibi_slopes(n):
    return jnp.asarray(2.0 ** (-8.0 * np.arange(1, n + 1) / n), dtype=jnp.float32)


def causal_depthwise_conv(x, w, b):
    k = w.shape[0]
    y = lax.conv_general_dilated(x, w[:, None, :].astype(x.dtype), window_strides=(1,),
                                 padding=[(k - 1, 0)], dimension_numbers=("NWC", "WIO", "NWC"),
                                 feature_group_count=x.shape[-1])
    return y + b


def segsum_exp(a):
    cs = jnp.cumsum(a, axis=-1)
    diff = cs[..., :, None] - cs[..., None, :]
    L = a.shape[-1]
    mask = jnp.tril(jnp.ones((L, L), dtype=bool))
    return jnp.exp(jnp.where(mask, diff, -jnp.inf))


def ssd_chunked(x, dt, A, Bm, Cm):
    b_, S, H, P = x.shape
    G, N = Bm.shape[-2:]
    E = H // G
    L = SSM_CHUNK
    nc = S // L
    xd = (x.astype(jnp.float32) * dt[..., None]).reshape(b_, nc, L, G, E, P)
    a = jnp.moveaxis((dt * A).reshape(b_, nc, L, G, E), 2, -1)
    a_cs = jnp.cumsum(a, axis=-1)
    Bc = Bm.astype(jnp.float32).reshape(b_, nc, L, G, N)
    Cc = Cm.astype(jnp.float32).reshape(b_, nc, L, G, N)
    CB = jnp.einsum("bclgn,bcsgn->bcgls", Cc, Bc)
    Lmat = segsum_exp(a)
    y_diag = jnp.einsum("bcgls,bcgels,bcsgep->bclgep", CB, Lmat, xd)
    decay_states = jnp.exp(a_cs[..., -1:] - a_cs)
    states = jnp.einsum("bclgn,bcgel,bclgep->bcgepn", Bc, decay_states, xd)
    chunk_decay = jnp.exp(a_cs[..., -1])

    def step(h, inp):
        st, dec = inp
        return h * dec[..., None, None] + st, h

    init = jnp.zeros((b_, G, E, P, N), jnp.float32)
    _, prev = lax.scan(step, init, (jnp.moveaxis(states, 1, 0), jnp.moveaxis(chunk_decay, 1, 0)))
    prev = jnp.moveaxis(prev, 0, 1)
    y_off = jnp.einsum("bclgn,bcgepn,bcgel->bclgep", Cc, prev, jnp.exp(a_cs))
    return (y_diag + y_off).reshape(b_, S, H, P)


def causal_block_attention(q, k, v, scale):
    b_, S, H, dk = q.shape
    nb = S // Q_BLOCK
    qb = jnp.swapaxes(q.reshape(b_, nb, Q_BLOCK, H, dk), 0, 1)
    kpos = jnp.arange(S)

    def one(args):
        qi, i = args
        s = jnp.einsum("bqhd,bkhd->bhqk", qi, k).astype(jnp.float32) * scale
        qpos = i * Q_BLOCK + jnp.arange(Q_BLOCK)
        s = jnp.where(kpos[None, :] <= qpos[:, None], s, -jnp.inf)
        p = jax.nn.softmax(s, axis=-1).astype(v.dtype)
        return jnp.einsum("bhqk,bkhd->bqhd", p, v)

    o = lax.map(one, (qb, jnp.arange(nb)))
    return jnp.swapaxes(o, 0, 1).reshape(b_, S, H, v.shape[-1])


def swa_sink_attention(q, k, v, sinks, pos):
    b_, S, Hq, d = q.shape
    Hkv = k.shape[2]
    G = Hq // Hkv
    W = SWA_WINDOW
    nb = S // W
    qb = q.reshape(b_, nb, W, Hkv, G, d)

    def with_prev(t):
        tb = t.reshape((b_, nb, W) + t.shape[2:])
        prev = jnp.concatenate([jnp.zeros_like(tb[:, :1]), tb[:, :-1]], axis=1)
        return jnp.concatenate([prev, tb], axis=2)

    kb, vb, pk = with_prev(k), with_prev(v), with_prev(pos)
    pq = pos.reshape(b_, nb, W)
    s = jnp.einsum("bnqhgd,bnkhd->bnhgqk", qb, kb).astype(jnp.float32) * (d ** -0.5)
    dist = jnp.abs(pq[:, :, :, None] - pk[:, :, None, :]).astype(jnp.float32)
    slopes = alibi_slopes(Hq).reshape(Hkv, G)
    s = s - slopes[None, None, :, :, None, None] * dist[:, :, None, None]
    qi = jnp.arange(nb)[:, None] * W + jnp.arange(W)[None, :]
    ki = jnp.arange(nb)[:, None] * W - W + jnp.arange(2 * W)[None, :]
    valid = (ki[:, None, :] >= 0) & (ki[:, None, :] <= qi[:, :, None]) & (qi[:, :, None] - ki[:, None, :] < W)
    s = jnp.where(valid[None, :, None, None], s, -jnp.inf)
    sink = jnp.broadcast_to(sinks.astype(jnp.float32).reshape(Hkv, G)[None, None, :, :, None, None],
                            s.shape[:-1] + (1,))
    p = jax.nn.softmax(jnp.concatenate([s, sink], axis=-1), axis=-1)[..., :-1].astype(v.dtype)
    o = jnp.einsum("bnhgqk,bnkhd->bnqhgd", p, vb)
    return o.reshape(b_, S, Hq * d)


def gla_chunked(q, k, v, g):
    b_, S, H, dk = q.shape
    dv = v.shape[-1]
    L = GLA_CHUNK
    nc = S // L
    qc = q.astype(jnp.float32).reshape(b_, nc, L, H, dk)
    kc = k.astype(jnp.float32).reshape(b_, nc, L, H, dk)
    vc = v.astype(jnp.float32).reshape(b_, nc, L, H, dv)
    bcum = jnp.cumsum(g.astype(jnp.float32).reshape(b_, nc, L, H, dk), axis=2)
    b_last = bcum[:, :, -1]
    q_dec = qc * jnp.exp(bcum)
    k_inv = kc * jnp.exp(-bcum)
    k_end = kc * jnp.exp(b_last[:, :, None] - bcum)
    att = jnp.einsum("bclhd,bcshd->bchls", q_dec, k_inv)
    att = jnp.where(jnp.tril(jnp.ones((L, L), dtype=bool)), att, 0.0)
    o_intra = jnp.einsum("bchls,bcshv->bclhv", att, vc)
    kv_chunk = jnp.einsum("bcshd,bcshv->bchdv", k_end, vc)

    def step(state, inp):
        kvc, dec = inp
        return state * dec[..., None] + kvc, state

    init = jnp.zeros((b_, H, dk, dv), jnp.float32)
    _, prev = lax.scan(step, init, (jnp.moveaxis(kv_chunk, 1, 0), jnp.moveaxis(jnp.exp(b_last), 1, 0)))
    prev = jnp.moveaxis(prev, 0, 1)
    o_inter = jnp.einsum("bclhd,bchdv->bclhv", q_dec, prev)
    return (o_intra + o_inter).reshape(b_, S, H, dv).astype(q.dtype)


def ssd_mla_mixer(h, pos, w_in, conv_w, conv_b, dt_bias, a_log, d_skip, ssm_norm,
                  q_a_norm, w_q_b, kv_a_norm, w_kv_b, q_norm, k_norm, w_out):
    b_, S, _ = h.shape
    z, xbc, dt, q_a, kv_a = split_last(h @ w_in, [SSM_D, SSM_XBC, SSM_HEADS, MLA_Q_RANK, MLA_KV_RANK + MLA_ROPE])
    xbc = jax.nn.silu(causal_depthwise_conv(xbc, conv_w, conv_b))
    xs, Bm, Cm = split_last(xbc, [SSM_D, SSM_GROUPS * SSM_STATE, SSM_GROUPS * SSM_STATE])
    xs = xs.reshape(b_, S, SSM_HEADS, SSM_HEAD_DIM)
    dt = jax.nn.softplus((dt + dt_bias).astype(jnp.float32))
    A = -jnp.exp(a_log.astype(jnp.float32))
    y = ssd_chunked(xs, dt, A, Bm.reshape(b_, S, SSM_GROUPS, SSM_STATE), Cm.reshape(b_, S, SSM_GROUPS, SSM_STATE))
    y = (y + d_skip.astype(jnp.float32)[:, None] * xs.astype(jnp.float32)).astype(h.dtype).reshape(b_, S, SSM_D)
    yg = (y * jax.nn.silu(z)).reshape(b_, S, SSM_GROUPS, SSM_D // SSM_GROUPS)
    y = rms_norm(yg, ssm_norm.reshape(SSM_GROUPS, SSM_D // SSM_GROUPS)).reshape(b_, S, SSM_D)
    q = (rms_norm(q_a, q_a_norm) @ w_q_b).reshape(b_, S, MLA_HEADS, MLA_QK)
    kv_c, k_pe = split_last(kv_a, [MLA_KV_RANK, MLA_ROPE])
    kv = (rms_norm(kv_c, kv_a_norm) @ w_kv_b).reshape(b_, S, MLA_HEADS, MLA_NOPE + MLA_V)
    k_nope, v = split_last(kv, [MLA_NOPE, MLA_V])
    k = jnp.concatenate([k_nope, jnp.broadcast_to(k_pe[:, :, None, :], (b_, S, MLA_HEADS, MLA_ROPE))], axis=-1)
    q = rms_norm(q, q_norm)
    k = rms_norm(k, k_norm)
    cos, sin = rope_tables(pos, MLA_ROPE, h.dtype)
    q = jnp.concatenate([q[..., :MLA_NOPE], apply_rope(q[..., MLA_NOPE:], cos, sin)], axis=-1)
    k = jnp.concatenate([k[..., :MLA_NOPE], apply_rope(k[..., MLA_NOPE:], cos, sin)], axis=-1)
    o = causal_block_attention(q, k, v, MLA_QK ** -0.5).reshape(b_, S, MLA_HEADS * MLA_V)
    return jnp.concatenate([y, o], axis=-1) @ w_out


def swa_gla_mixer(h, pos, w_in, q_norm, k_norm, sinks, w_gate_b, gate_bias, gla_norm, w_out):
    b_, S, _ = h.shape
    q, k, v, gq, gk, gv, ga, gr = split_last(h @ w_in, [
        SWA_Q_HEADS * SWA_HD, SWA_KV_HEADS * SWA_HD, SWA_KV_HEADS * SWA_HD,
        GLA_HEADS * GLA_DK, GLA_HEADS * GLA_DK, GLA_HEADS * GLA_DV, GLA_RANK, GLA_HEADS * GLA_DV])
    q = rms_norm(q.reshape(b_, S, SWA_Q_HEADS, SWA_HD), q_norm)
    k = rms_norm(k.reshape(b_, S, SWA_KV_HEADS, SWA_HD), k_norm)
    v = v.reshape(b_, S, SWA_KV_HEADS, SWA_HD)
    o_swa = swa_sink_attention(q, k, v, sinks, pos)
    g = jax.nn.log_sigmoid((ga @ w_gate_b + gate_bias).astype(jnp.float32)) / GLA_TAU
    o = gla_chunked(gq.reshape(b_, S, GLA_HEADS, GLA_DK) * (GLA_DK ** -0.5),
                    gk.reshape(b_, S, GLA_HEADS, GLA_DK),
                    gv.reshape(b_, S, GLA_HEADS, GLA_DV),
                    g.reshape(b_, S, GLA_HEADS, GLA_DK))
    o = rms_norm(o, gla_norm) * jax.nn.silu(gr.reshape(b_, S, GLA_HEADS, GLA_DV))
    return jnp.concatenate([o_swa, o.reshape(b_, S, GLA_HEADS * GLA_DV)], axis=-1) @ w_out


def setup_inputs(seed: int = 0) -> dict:
    key = jax.random.key(seed)
    keys = iter(jax.random.split(key, 64))

    def nrm(shape, fan_in):
        return jax.random.normal(next(keys), shape, jnp.float32) * (fan_in ** -0.5)

    def gain(shape):
        return 1.0 + 0.05 * jax.random.normal(next(keys), shape, jnp.float32)

    def small(shape, s=0.02):
        return s * jax.random.normal(next(keys), shape, jnp.float32)

    x = jax.random.normal(next(keys), (BATCH, SEQ, D_MODEL), jnp.float32)
    positions = jnp.tile(jnp.arange(SEQ, dtype=jnp.int32)[None, :], (BATCH, 1))
    dt0 = jnp.exp(jax.random.uniform(next(keys), (N_EVEN, SSM_HEADS), jnp.float32)
                  * (math.log(0.1) - math.log(0.001)) + math.log(0.001))
    dt_bias = dt0 + jnp.log(-jnp.expm1(-dt0))
    a_log = jnp.log(jax.random.uniform(next(keys), (N_EVEN, SSM_HEADS), jnp.float32, 1.0, 16.0))
    return {
        "x": x,
        "positions": positions,
        "pre_norm": gain((DEPTH, D_MODEL)),
        "pre_w_gate": nrm((DEPTH, D_MODEL, D_FF), D_MODEL),
        "pre_w_up": nrm((DEPTH, D_MODEL, D_FF), D_MODEL),
        "pre_w_down": nrm((DEPTH, D_FF, D_MODEL), D_FF),
        "mix_norm": gain((DEPTH, D_MODEL)),
        "post_norm": gain((DEPTH, D_MODEL)),
        "post_w_gate": nrm((DEPTH, D_MODEL, D_FF), D_MODEL),
        "post_w_up": nrm((DEPTH, D_MODEL, D_FF), D_MODEL),
        "post_w_down": nrm((DEPTH, D_FF, D_MODEL), D_FF),
        "e_w_in": nrm((N_EVEN, D_MODEL, IN_EVEN), D_MODEL),
        "e_conv_w": nrm((N_EVEN, SSM_CONV, SSM_XBC), SSM_CONV),
        "e_conv_b": small((N_EVEN, SSM_XBC)),
        "e_dt_bias": dt_bias,
        "e_a_log": a_log,
        "e_d_skip": gain((N_EVEN, SSM_HEADS)),
        "e_ssm_norm": gain((N_EVEN, SSM_D)),
        "e_q_a_norm": gain((N_EVEN, MLA_Q_RANK)),
        "e_w_q_b": nrm((N_EVEN, MLA_Q_RANK, MLA_HEADS * MLA_QK), MLA_Q_RANK),
        "e_kv_a_norm": gain((N_EVEN, MLA_KV_RANK)),
        "e_w_kv_b": nrm((N_EVEN, MLA_KV_RANK, MLA_HEADS * (MLA_NOPE + MLA_V)), MLA_KV_RANK),
        "e_q_norm": gain((N_EVEN, MLA_QK)),
        "e_k_norm": gain((N_EVEN, MLA_QK)),
        "e_w_out": nrm((N_EVEN, OUT_EVEN, D_MODEL), OUT_EVEN),
        "o_w_in": nrm((N_ODD, D_MODEL, IN_ODD), D_MODEL),
        "o_q_norm": gain((N_ODD, SWA_HD)),
        "o_k_norm": gain((N_ODD, SWA_HD)),
        "o_sinks": small((N_ODD, SWA_Q_HEADS), 0.5),
        "o_w_gate_b": nrm((N_ODD, GLA_RANK, GLA_HEADS * GLA_DK), GLA_RANK),
        "o_gate_bias": small((N_ODD, GLA_HEADS * GLA_DK), 0.1),
        "o_gla_norm": gain((N_ODD, GLA_DV)),
        "o_w_out": nrm((N_ODD, OUT_ODD, D_MODEL), OUT_ODD),
    }


def reference(x, positions, pre_norm, pre_w_gate, pre_w_up, pre_w_down, mix_norm,
              post_norm, post_w_gate, post_w_up, post_w_down,
              e_w_in, e_conv_w, e_conv_b, e_dt_bias, e_a_log, e_d_skip, e_ssm_norm,
              e_q_a_norm, e_w_q_b, e_kv_a_norm, e_w_kv_b, e_q_norm, e_k_norm, e_w_out,
              o_w_in, o_q_norm, o_k_norm, o_sinks, o_w_gate_b, o_gate_bias, o_gla_norm, o_w_out):
    for layer in range(DEPTH):
        x = x + FFN_RES * swiglu(rms_norm(x, pre_norm[layer]), pre_w_gate[layer], pre_w_up[layer], pre_w_down[layer])
        h = rms_norm(x, mix_norm[layer])
        j = layer // 2
        if layer % 2 == 0:
            x = x + ssd_mla_mixer(h, positions, e_w_in[j], e_conv_w[j], e_conv_b[j], e_dt_bias[j], e_a_log[j],
                                  e_d_skip[j], e_ssm_norm[j], e_q_a_norm[j], e_w_q_b[j], e_kv_a_norm[j],
                                  e_w_kv_b[j], e_q_norm[j], e_k_norm[j], e_w_out[j])
        else:
            x = x + swa_gla_mixer(h, positions, o_w_in[j], o_q_norm[j], o_k_norm[j], o_sinks[j],
                                  o_w_gate_b[j], o_gate_bias[j], o_gla_norm[j], o_w_out[j])
        x = x + FFN_RES * swiglu(rms_norm(x, post_norm[layer]), post_w_gate[layer], post_w_up[layer], post_w_down[layer])
    return x
```

```python
import numpy as np
from contextlib import ExitStack
import concourse.bass as bass
import concourse.mybir as mybir
from concourse.bass_utils import run_bass_kernel_spmd

F32 = mybir.dt.float32
BF16 = mybir.dt.bfloat16
I32 = mybir.dt.int32
AF = mybir.ActivationFunctionType
ALU = mybir.AluOpType

D = 1024
T = 2048
DFF = 2816
NCH = DFF // 128
EPS = 1e-6
ENGS = ("pe", "act", "dve", "pool", "sp")
import os
SERIAL = bool(os.environ.get('DBG_SERIAL'))


class Prog:
    def __init__(self):
        self.ops = {e: [] for e in ENGS}
        self.cnt = {e: 0 for e in ENGS}
        self.waited = {e: {} for e in ENGS}
        self.last_w = {}
        self.readers = {}
        self.dcnt = {}

    def _deps(self, eng, reads, writes):
        deps = {}

        def add(s, v):
            if deps.get(s, 0) < v:
                deps[s] = v

        for k in reads:
            if k in self.last_w:
                add(*self.last_w[k])
        for k in writes:
            if k in self.last_w:
                add(*self.last_w[k])
            for s, v in self.readers.get(k, {}).items():
                add(s, v)
        waits = []
        for s, v in deps.items():
            if s == "e:pe" and eng == "pe":
                continue
            if self.waited[eng].get(s, 0) < v:
                self.waited[eng][s] = v
                waits.append((s, v))
        return waits

    def _commit(self, tok, reads, writes):
        for k in writes:
            self.last_w[k] = tok
            self.readers[k] = {}
        for k in reads:
            r = self.readers.setdefault(k, {})
            if r.get(tok[0], 0) < tok[1]:
                r[tok[0]] = tok[1]

    def op(self, eng, fn, reads=(), writes=()):
        waits = self._deps(eng, reads, writes)
        self.cnt[eng] += 1
        tok = ("e:" + eng, self.cnt[eng])
        self.ops[eng].append((waits, fn, tok[0], 1))
        self._commit(tok, reads, writes)
        if SERIAL:
            self.barrier()

    def dma(self, eng, fn, key, reads=(), writes=(), n=1):
        waits = self._deps(eng, reads, writes)
        s = "d:" + key
        self.dcnt[s] = self.dcnt.get(s, 0) + 16 * n
        tok = (s, self.dcnt[s])
        self.ops[eng].append((waits, fn, s, 16))
        self._commit(tok, reads, writes)
        if SERIAL:
            self.barrier()

    def barrier(self):
        for e in ENGS:
            for e2 in ENGS:
                if e2 == "sp" or self.cnt[e2] == 0:
                    continue
                s = "e:" + e2
                v = self.cnt[e2]
                if self.waited[e].get(s, 0) < v:
                    self.waited[e][s] = v
                    self.ops[e].append(([(s, v)], None, None, 0))
            for s, v in self.dcnt.items():
                if self.waited[e].get(s, 0) < v:
                    self.waited[e][s] = v
                    self.ops[e].append(([(s, v)], None, None, 0))

    def emit(self, nc):
        with ExitStack() as es:
            sems = {}
            names = set()
            for e in ENGS:
                for waits, fn, s, inc in self.ops[e]:
                    if s is not None:
                        names.add(s)
                    for w in waits:
                        names.add(w[0])
            for s in sorted(names):
                sems[s] = es.enter_context(nc.semaphore(s.replace(":", "_")))
            block = es.enter_context(nc.Block())

            def run(e):
                def body(eng):
                    for waits, fn, s, inc in self.ops[e]:
                        for ws, wv in waits:
                            eng.wait_ge(sems[ws], wv)
                        if fn is None:
                            continue
                        r = fn(eng)
                        if isinstance(r, (list, tuple)):
                            for ins in r:
                                ins.then_inc(sems[s], inc)
                        else:
                            r.then_inc(sems[s], inc)
                    if e == "sp":
                        for s, v in self.dcnt.items():
                            eng.wait_ge(sems[s], v)

                return body

            block.tensor(run("pe"))
            block.scalar(run("act"))
            block.vector(run("dve"))
            block.gpsimd(run("pool"))
            block.sync(run("sp"))


class Ctx:
    pass


def tsl(j, n=512):
    return slice(j * n, (j + 1) * n)


def fm(v):
    v = np.asarray(v, np.float32)
    return np.ascontiguousarray(v.reshape(-1, 128).T)


def make_masks():
    p = np.arange(128)[:, None]
    f = np.arange(128)[None, :]
    mc = (f >= p)
    mp = (f < p)
    bd = (p // 64 == f // 64)
    gm = bd & (p <= f)
    m64 = np.broadcast_to((np.arange(512)[None, :] % 64 != 0), (128, 512))
    ident = (p == f)
    neg = -30000.0 * mp
    fw = np.arange(896)[None, :]
    mcw = (fw - 384 >= p)
    return (np.ascontiguousarray(np.concatenate([mc, mp, bd, gm, ident, neg], axis=1).astype(np.float32)),
            np.ascontiguousarray(np.concatenate([m64, mcw], axis=1).astype(np.float32)))


NMASK = 768
NMASKB = 512 + 896


def pack_consts(inp):
    cols = {}
    parts = []
    off = 0

    def put(name, arr):
        nonlocal off
        arr = np.asarray(arr, np.float32)
        assert arr.shape[0] == 128
        cols[name] = (off, arr.shape[1])
        parts.append(arr)
        off += arr.shape[1]

    for l in range(2):
        put(f"pre_norm{l}", fm(inp["pre_norm"][l]))
        put(f"mix_norm{l}", fm(inp["mix_norm"][l]))
        put(f"post_norm{l}", fm(inp["post_norm"][l]))
    put("eps", np.full((128, 1), EPS, np.float32))
    put("one", np.ones((128, 1), np.float32))
    put("o_qg2", np.tile(np.asarray(inp["o_q_norm"][0], np.float32), 2)[:, None])
    put("o_kg2", np.tile(np.asarray(inp["o_k_norm"][0], np.float32), 2)[:, None])
    cw = np.asarray(inp["e_conv_w"][0], np.float32)
    put("e_convw", np.ascontiguousarray(cw.T.reshape(16, 128, 4).transpose(1, 0, 2).reshape(128, 64)))
    put("e_convb", fm(inp["e_conv_b"][0]))
    put("e_dtb", np.broadcast_to(np.asarray(inp["e_dt_bias"][0], np.float32)[None, :], (128, 16)))
    put("e_alog", np.broadcast_to(np.asarray(inp["e_a_log"][0], np.float32)[None, :], (128, 16)))
    put("e_dskip", fm(np.repeat(np.asarray(inp["e_d_skip"][0], np.float32), 64)))
    put("e_ssmn", fm(inp["e_ssm_norm"][0]))
    put("e_qan", fm(inp["e_q_a_norm"][0]))
    put("e_kvan", fm(inp["e_kv_a_norm"][0]))
    for nm_, key_ in (("gq", "e_q_norm"), ("gk", "e_k_norm")):
        g96 = np.asarray(inp[key_][0], np.float32)
        col = np.zeros((128, 1), np.float32)
        col[0:96, 0] = g96
        put(nm_, col)
        colp = np.zeros((128, 1), np.float32)
        colp[64:80, 0] = g96[80:96]
        colp[80:96, 0] = g96[64:80]
        put(nm_ + "p", colp)
    invf = np.zeros((128, 1), np.float32)
    fr = (10000.0 ** (-np.arange(16, dtype=np.float64) / 16.0) / (2 * np.pi)).astype(np.float32)
    invf[64:80, 0] = fr
    invf[80:96, 0] = fr
    put("invf", invf)
    sgn = np.zeros((128, 1), np.float32)
    sgn[64:80, 0] = -1.0
    sgn[80:96, 0] = 1.0
    put("sgn", sgn)
    put("o_gbias", fm(inp["o_gate_bias"][0]))
    put("o_glan", np.asarray(inp["o_gla_norm"][0], np.float32)[:, None])
    put("sinks", np.broadcast_to(np.asarray(inp["o_sinks"][0], np.float32)[None, :], (128, 8)))
    put("SL", np.broadcast_to((-8.0 * 2.0 ** (-np.arange(1, 9, dtype=np.float64))).astype(np.float32)[None, :], (128, 8)))
    return np.ascontiguousarray(np.concatenate(parts, axis=1)), cols


def rmsnorm_stage(P, C, gcol):
    for j in range(4):
        ts = tsl(j)
        sq = C.sq

        def f_sq(eng, ts=ts):
            return eng.tensor_tensor(sq[:, :, :], C.xT[:, :, ts], C.xT[:, :, ts], ALU.mult)

        P.op("pool", f_sq, reads=[("xT", d, j) for d in range(8)], writes=[("sq",)])
        bank = C.bank()

        def f_mm(eng, bank=bank):
            r = None
            for k in range(8):
                r = eng.matmul(C.ps[:, bank, :], C.ones_bf[:, :], sq[:, k, :], start=(k == 0), stop=(k == 7))
            return r

        P.op("pe", f_mm, reads=[("sq",)], writes=[("ps", bank)])

        def f_r1(eng, bank=bank):
            return eng.activation(C.rstd[:, :], C.ps[:, bank, :], AF.Sqrt, bias=C.consts[:, C.eps_col:C.eps_col + 1],
                                  scale=1.0 / D)

        P.op("act", f_r1, reads=[("ps", bank)], writes=[("rstd",)])

        def f_r2(eng):
            return eng.reciprocal(C.rstd[:, :], C.rstd[:, :])

        P.op("dve", f_r2, reads=[("rstd",)], writes=[("rstd",)])
        for k in range(8):
            def f_h(eng, k=k, ts=ts):
                return eng.scalar_tensor_tensor(
                    C.hT[:, k, ts], C.xT[:, k, ts], C.consts[:, gcol + k:gcol + k + 1], C.rstd[:, :],
                    ALU.mult, ALU.mult)

            P.op("dve", f_h, reads=[("xT", k, j), ("rstd",)], writes=[("hT", k, j)])


FFN_GROUPS = [(0, 8), (8, 7), (15, 7)]
ARENA = 102 * 1024
import os
DBG_PHASE = int(os.environ.get('DBG_PHASE', '3'))
DBG_NB = int(os.environ.get('DBG_NB', '0'))
DBG_STEP = int(os.environ.get('DBG_STEP', '9'))


class Arena:
    def __init__(self, C):
        self.C = C
        self.off = 0

    def _take(self, n, esz):
        self.off = (self.off + 3) // 4 * 4
        o = self.off
        self.off += n * esz
        assert self.off <= ARENA, self.off
        return o

    def bf(self, *free):
        n = int(np.prod(free))
        o = self._take(n, 2)
        return self._shape(self.C.ar[:, o // 2:o // 2 + n], free)

    def f32(self, *free):
        n = int(np.prod(free))
        o = self._take(n, 4)
        return self._shape(self.C.ar_f[:, o // 4:o // 4 + n], free)

    def i32(self, *free):
        n = int(np.prod(free))
        o = self._take(n, 4)
        return self._shape(self.C.ar_i[:, o // 4:o // 4 + n], free)

    @staticmethod
    def _shape(v, free):
        if len(free) == 1:
            return v
        if len(free) == 2:
            return v.rearrange("p (a b) -> p a b", a=free[0])
        if len(free) == 3:
            return v.rearrange("p (a b c) -> p a b c", a=free[0], b=free[1])
        raise ValueError


def bc(ap, dims):
    return bass.AP(ap.tensor, ap.offset, [list(ap.ap[0])] + [list(d) for d in dims])


def ffn_alloc(C):
    A = Arena(C)
    C.aT = A.bf(8, T)
    C.wgu = [[A.bf(8, 512) for i in range(2)] for s in range(2)]
    C.wd_sb = A.bf(8, D)
    C.sg = [A.f32(512) for s in range(2)]
    C.sq = A.bf(8, 512)
    C.rstd = A.f32(512)


def ffn_stage(P, C, gcol, wg, wu, wd):
    rmsnorm_stage(P, C, gcol)
    wg_v = wg.rearrange("(k p) f -> p k f", p=128)
    wu_v = wu.rearrange("(k p) f -> p k f", p=128)
    wd_v = wd.rearrange("(c p) d -> p c d", p=128)
    for (c0, ng) in FFN_GROUPS:
        def f_wd(eng, c0=c0, ng=ng):
            return eng.dma_start(out=C.wd_sb[:, 0:ng, :], in_=wd_v[:, c0:c0 + ng, :])

        P.dma("pool", f_wd, "wd", writes=[("wd",)])
        pieces = []
        cc = 0
        while cc < ng:
            pn = min(4, ng - cc)
            pieces.append((cc, pn))
            cc += pn
        for (pc, pn) in pieces:
            slot = C.wslot
            C.wslot = (C.wslot + 1) % 2
            f0 = (c0 + pc) * 128

            def f_wg(eng, slot=slot, f0=f0, pn=pn):
                return eng.dma_start(out=C.wgu[slot][0][:, :, 0:pn * 128], in_=wg_v[:, :, f0:f0 + pn * 128])

            def f_wu(eng, slot=slot, f0=f0, pn=pn):
                return eng.dma_start(out=C.wgu[slot][1][:, :, 0:pn * 128], in_=wu_v[:, :, f0:f0 + pn * 128])

            P.dma("pool", f_wg, f"wg{slot}", writes=[("wg", slot)])
            P.dma("pool", f_wu, f"wu{slot}", writes=[("wu", slot)])
            for ci in range(pn):
                ca = pc + ci
                for j in range(4):
                    ts = tsl(j)
                    bg = C.bank()
                    bu = C.bank()

                    def f_mm(eng, slot=slot, ci=ci, ts=ts, bg=bg, bu=bu):
                        r = None
                        for k in range(8):
                            r = eng.matmul(C.ps[:, bg, :], C.wgu[slot][0][:, k, ci * 128:(ci + 1) * 128],
                                           C.hT[:, k, ts], start=(k == 0), stop=(k == 7))
                        for k in range(8):
                            r = eng.matmul(C.ps[:, bu, :], C.wgu[slot][1][:, k, ci * 128:(ci + 1) * 128],
                                           C.hT[:, k, ts], start=(k == 0), stop=(k == 7))
                        return r

                    P.op("pe", f_mm, reads=[("wg", slot), ("wu", slot)] + [("hT", k, j) for k in range(8)],
                         writes=[("ps", bg), ("ps", bu)])
                    ss = C.sgslot
                    C.sgslot = (C.sgslot + 1) % 2

                    def f_silu(eng, ss=ss, bg=bg):
                        return eng.activation(C.sg[ss][:, :], C.ps[:, bg, :], AF.Silu)

                    P.op("act", f_silu, reads=[("ps", bg)], writes=[("sg", ss)])

                    def f_mul(eng, ss=ss, bu=bu, ca=ca, ts=ts):
                        return eng.tensor_tensor(C.aT[:, ca, ts], C.sg[ss][:, :], C.ps[:, bu, :], ALU.mult)

                    P.op("dve", f_mul, reads=[("sg", ss), ("ps", bu)], writes=[("aT", ca, j)])
        for d in range(8):
            for j in range(4):
                ts = tsl(j)
                by = C.bank()

                def f_mm(eng, d=d, ts=ts, by=by, ng=ng):
                    r = None
                    for ca in range(ng):
                        r = eng.matmul(C.ps[:, by, :], C.wd_sb[:, ca, d * 128:(d + 1) * 128], C.aT[:, ca, ts],
                                       start=(ca == 0), stop=(ca == ng - 1))
                    return r

                P.op("pe", f_mm, reads=[("wd",)] + [("aT", ca, j) for ca in range(ng)], writes=[("ps", by)])

                def f_res(eng, d=d, ts=ts, by=by):
                    return eng.scalar_tensor_tensor(C.xT[:, d, ts], C.ps[:, by, :], 0.5, C.xT[:, d, ts],
                                                    ALU.mult, ALU.add)

                P.op("dve", f_res, reads=[("ps", by), ("xT", d, j)], writes=[("xT", d, j)])


def norm_heads64(P, C, bank, gcol_name, out_ap, sq, rstd):
    gc = C.ccols[gcol_name][0]

    def f_sq(eng):
        return eng.activation(sq, C.ps[:, bank, :], AF.Square)

    P.op("act", f_sq, reads=[("ps", bank)], writes=[("nsq",)])
    b2 = C.bank()

    def f_mm(eng):
        return eng.matmul(C.ps[:, b2, :], C.bd_bf[:, :], sq, start=True, stop=True)

    P.op("pe", f_mm, reads=[("nsq",), ("bd",)], writes=[("ps", b2)])

    def f_r1(eng):
        return eng.activation(rstd, C.ps[:, b2, :], AF.Sqrt, bias=C.consts[:, C.eps_col:C.eps_col + 1], scale=1.0 / 64)

    P.op("act", f_r1, reads=[("ps", b2)], writes=[("nrstd",)])

    def f_r2(eng):
        return eng.reciprocal(rstd, rstd)

    P.op("dve", f_r2, reads=[("nrstd",)], writes=[("nrstd",)])

    def f_o(eng):
        return eng.scalar_tensor_tensor(out_ap, C.ps[:, bank, :], C.consts[:, gc:gc + 1], rstd, ALU.mult, ALU.mult)

    return f_o


def swa_stage(P, C, s):
    dr = C.dr
    w_in = C.need("o_w_in", [D, 2320], lambda inp: inp["o_w_in"][0])
    w_out = C.need("o_w_out", [D, D], lambda inp: inp["o_w_out"][0])
    A = Arena(C)
    wqkv = A.bf(8, 768)
    wk2 = A.bf(8, 2, 128)
    woS = A.bf(8, D)
    qT = A.bf(4, 512)
    kT2 = A.bf(2, T)
    Vaug = A.bf(16, 2, 128)
    oT = A.bf(8, 512)
    posB = A.f32(T)
    posBi = bass.AP(C.ar_i[:, 0:1].tensor, posB.offset, [list(posB.ap[0]), [1, T]])
    pk = A.f32(16)
    pki = A.i32(16)
    dist = A.f32(128)
    D8 = A.f32(8, 128)
    tmp = A.f32(512)
    Pe = A.bf(512)
    Pm = [A.bf(512) for _ in range(4)]
    sq = A.bf(512)
    rstd = A.f32(512)
    dn = A.f32(512)
    dn0 = A.f32(512)
    es = A.f32(8)
    cc = C.ccols
    w_in_v = w_in.rearrange("(k p) f -> p k f", p=128)

    P.dma("pool", lambda e: e.dma_start(out=wqkv, in_=w_in_v[:, :, 0:768]), "wA", writes=[("wqkv",)])

    def f_wk(e):
        r = []
        for g in range(2):
            for hh in range(2):
                r.append(e.dma_start(out=wk2[:, :, g, hh * 64:(hh + 1) * 64], in_=w_in_v[:, :, 512 + g * 64:512 + (g + 1) * 64]))
        return r

    P.dma("pool", f_wk, "wB", writes=[("wk2",)], n=4)
    P.dma("pool", lambda e: e.dma_start(out=woS[0:64, :, :], in_=w_out[0:512, :].rearrange("(h p) d -> p h d", p=64)),
          "wC", writes=[("woS",)])
    P.dma("sp", lambda e: e.dma_start(out=posBi, in_=bass.AP(C.pos.tensor, C.pos[s:s + 1, :].offset, [[0, 128], [1, T]])),
          "posB", writes=[("posB",)])
    P.dma("sp", lambda e: e.dma_start(out=pki, in_=C.posT[s]), "pk", writes=[("pki",)])
    P.op("dve", lambda e: e.tensor_copy(posB, posBi), reads=[("posB",)], writes=[("posB",)])
    P.op("dve", lambda e: e.tensor_copy(pk, pki), reads=[("pki",)], writes=[("pk",)])
    P.op("act", lambda e: e.activation(es, C.consts[:, cc["sinks"][0]:cc["sinks"][0] + 8], AF.Exp), reads=[("consts",)], writes=[("es",)])
    P.op("pool", lambda e: e.memset(Vaug[:, :, :, 64:128], 1.0), writes=[("Vaug", n) for n in range(16)])
    SLc = cc["SL"][0]

    for j in range(4):
        ts = tsl(j)
        for c in range(4):
            b = C.bank()

            def f_mm(eng, c=c, b=b, ts=ts):
                r = None
                for k in range(8):
                    r = eng.matmul(C.ps[:, b, :], wqkv[:, k, c * 128:(c + 1) * 128], C.hT[:, k, ts], start=(k == 0), stop=(k == 7))
                return r

            P.op("pe", f_mm, reads=[("wqkv",)] + [("hT", k, j) for k in range(8)], writes=[("ps", b)])
            f_o = norm_heads64(P, C, b, "o_qg2", qT[:, c, :], sq, rstd)
            P.op("dve", f_o, reads=[("ps", b), ("nrstd",)], writes=[("qT", c)])
        for g in range(2):
            b = C.bank()

            def f_mm(eng, g=g, b=b, ts=ts):
                r = None
                for k in range(8):
                    r = eng.matmul(C.ps[:, b, :], wk2[:, k, g, :], C.hT[:, k, ts], start=(k == 0), stop=(k == 7))
                return r

            P.op("pe", f_mm, reads=[("wk2",)] + [("hT", k, j) for k in range(8)], writes=[("ps", b)])
            f_o = norm_heads64(P, C, b, "o_kg2", kT2[:, g, ts], sq, rstd)
            P.op("dve", f_o, reads=[("ps", b), ("nrstd",)], writes=[("kT2", g, j)])
        for nl in range(4):
            n = 4 * j + nl
            b = C.bank()

            def f_mm(eng, n=n, b=b):
                r = None
                for k in range(8):
                    r = eng.matmul(C.ps[:, b, 0:128], C.hT[:, k, n * 128:(n + 1) * 128], wqkv[:, k, 640:768], start=(k == 0), stop=(k == 7))
                return r

            P.op("pe", f_mm, reads=[("wqkv",)] + [("hT", k, j) for k in range(8)], writes=[("ps", b)])

            def f_v(eng, n=n, b=b):
                return eng.tensor_copy(Vaug[:, n, :, 0:64], C.ps[:, b, 0:128].rearrange("p (g d) -> p g d", g=2))

            P.op("dve", f_v, reads=[("ps", b)], writes=[("Vaug", n)])
        for nl in range((4 if DBG_NB <= 0 else (DBG_NB if j == 0 else 0)) if DBG_PHASE >= 2 else 0):
            n = 4 * j + nl
            qs = slice(nl * 128, (nl + 1) * 128)
            kbs = [n - 1, n] if n > 0 else [n]
            for kb in kbs:
                def f_dist(eng, n=n, kb=kb):
                    return eng.tensor_scalar(dist, posB[:, n * 128:(n + 1) * 128], pk[:, kb:kb + 1], None, ALU.subtract)

                P.op("dve", f_dist, reads=[("posB",), ("pk",)], writes=[("dist",)])
                P.op("dve", lambda e: e.scalar_tensor_tensor(dist, dist, -1.0, dist, ALU.mult, ALU.max), reads=[("dist",)], writes=[("dist",)])

                def f_d8(eng):
                    return eng.tensor_tensor(D8, bc(dist, [[0, 8], [1, 128]]),
                                             bc(C.consts[:, SLc:SLc + 8], [[1, 8], [0, 128]]), ALU.mult)

                P.op("pool", f_d8, reads=[("dist",), ("consts",)], writes=[("D8",)])
                for g in range(2 if DBG_STEP >= 2 else 0):
                    bA = C.bank()
                    bB = C.bank()

                    def f_sc(eng, g=g, bA=bA, bB=bB, kb=kb, qs=qs):
                        r = None
                        for hl in range(4):
                            c = 2 * g + hl // 2
                            hp = slice((hl % 2) * 64, (hl % 2) * 64 + 64)
                            bb = bA if hl % 2 == 0 else bB
                            r = eng.matmul(C.ps[:, bb, (hl // 2) * 128:(hl // 2 + 1) * 128], kT2[hp, g, kb * 128:(kb + 1) * 128],
                                           qT[hp, c, qs], start=True, stop=True)
                        return r

                    P.op("pe", f_sc, reads=[("kT2", g, kb // 4), ("qT", 2 * g), ("qT", 2 * g + 1)], writes=[("ps", bA), ("ps", bB)])
                    if DBG_STEP < 3:
                        continue
                    for par, bb in ((0, bA), (1, bB)):
                        def f_t(eng, g=g, bb=bb, par=par):
                            return eng.tensor_tensor(tmp.rearrange("p (h f) -> p h f", h=4)[:, par:4:2, :],
                                                     D8[:, 4 * g + par:4 * g + 4:2, :],
                                                     C.ps[:, bb, 0:256].rearrange("p (h f) -> p h f", h=2), ALU.add)

                        P.op("dve", f_t, reads=[("D8",), ("ps", bb)], writes=[("tmp",)])
                    P.op("act", lambda e: e.activation(Pe, tmp, AF.Exp, scale=0.125), reads=[("tmp",)], writes=[("Pe",)])
                    pi = C.pmi
                    C.pmi = (C.pmi + 1) % 4
                    mcol = 0 if kb == n else 128

                    def f_m(eng, pi=pi, mcol=mcol):
                        return eng.tensor_tensor(Pm[pi].rearrange("p (h f) -> p h f", h=4), Pe.rearrange("p (h f) -> p h f", h=4),
                                                 bc(C.mask[:, mcol:mcol + 128], [[0, 4], [1, 128]]), ALU.mult)

                    P.op("pool", f_m, reads=[("Pe",), ("mask",)], writes=[("Pm", pi)])
                    if DBG_STEP < 4:
                        continue

                    def f_pv(eng, g=g, kb=kb, pi=pi, first=(kb == kbs[0]), last=(kb == n)):
                        return eng.matmul(C.ps[:, g, :], Vaug[:, kb, g, :], Pm[pi], start=first, stop=last)

                    P.op("pe", f_pv, reads=[("Vaug", kb), ("Pm", pi)], writes=[("ps", g)])
            for g in range(2 if DBG_STEP >= 5 else 0):
                def f_dn(eng, g=g):
                    return eng.tensor_tensor(dn[64:128, :].rearrange("p (h f) -> p h f", h=4),
                                             C.ps[64:128, g, :].rearrange("p (h f) -> p h f", h=4),
                                             bc(es[64:128, 4 * g:4 * g + 4], [[1, 4], [0, 128]]), ALU.add)

                P.op("dve", f_dn, reads=[("ps", g), ("es",)], writes=[("dn",)])
                P.op("dve", lambda e: e.reciprocal(dn[64:128, :], dn[64:128, :]), reads=[("dn",)], writes=[("dn",)])
                P.op("dve", lambda e: e.tensor_copy(dn0[0:64, :], dn[64:128, :]), reads=[("dn",)], writes=[("dn0",)])

                def f_o(eng, g=g, qs=qs):
                    return eng.tensor_tensor(oT[0:64, 4 * g:4 * g + 4, qs], C.ps[0:64, g, :].rearrange("p (h f) -> p h f", h=4),
                                             dn0[0:64, :].rearrange("p (h f) -> p h f", h=4), ALU.mult)

                P.op("dve", f_o, reads=[("ps", g), ("dn0",)], writes=[("oT",)])
        for d in range(8 if DBG_PHASE >= 3 else 0):
            b = C.bank()

            def f_mm(eng, d=d, b=b):
                r = None
                for h in range(8):
                    r = eng.matmul(C.ps[:, b, :], woS[0:64, h, d * 128:(d + 1) * 128], oT[0:64, h, :], start=(h == 0), stop=(h == 7))
                return r

            P.op("pe", f_mm, reads=[("woS",), ("oT",)], writes=[("ps", b)])

            def f_res(eng, d=d, b=b, ts=ts):
                return eng.tensor_tensor(C.xT[:, d, ts], C.ps[:, b, :], C.xT[:, d, ts], ALU.add)

            P.op("dve", f_res, reads=[("ps", b), ("xT", d, j)], writes=[("xT", d, j)])


def gla_stage(P, C, s):
    w_in = C.need("o_w_in", [D, 2320], lambda inp: inp["o_w_in"][0])
    w_out = C.need("o_w_out", [D, D], lambda inp: inp["o_w_out"][0])
    w_gb = C.need("o_w_gate_b", [16, 256], lambda inp: inp["o_w_gate_b"][0])
    cc = C.ccols
    A = Arena(C)
    wG = A.bf(8, 1552)
    wgb = A.bf(256)
    woG = A.bf(4, D)
    gaT = A.bf(512)
    ebuf = A.f32(512)
    lbuf = A.f32(512)
    cl = A.f32(2, 512)
    eb = A.f32(512)
    einv = A.f32(512)
    dend = A.f32(512)
    decs = A.f32(2, 8)
    nb = A.f32(2)
    q_dec = A.bf(2, 512)
    k_inv = A.bf(2, 512)
    k_end = A.bf(2, 512)
    grs = A.bf(4, 512)
    gv_tok = A.bf(4, 512)
    ket = A.bf(4, 2, 128)
    attm = A.bf(4, 128)
    S = [A.f32(128) for _ in range(2)]
    S_bf = [A.bf(128) for _ in range(2)]
    sq = A.bf(256)
    rstd = A.f32(256)
    ytmp = A.f32(256)
    oG = A.bf(4, 512)
    w_in_v = w_in.rearrange("(k p) f -> p k f", p=128)
    onec = C.consts[:, cc["one"][0]:cc["one"][0] + 1]
    glan = C.consts[:, cc["o_glan"][0]:cc["o_glan"][0] + 1]
    gbc = cc["o_gbias"][0]

    P.dma("pool", lambda e: e.dma_start(out=wG, in_=w_in_v[:, :, 768:2320]), "wA", writes=[("wG",)])
    P.dma("pool", lambda e: e.dma_start(out=wgb[0:16, :], in_=w_gb), "wB", writes=[("wgb",)])
    P.dma("pool", lambda e: e.dma_start(out=woG, in_=w_out[512:1024, :].rearrange("(c p) d -> p c d", p=128)), "wC", writes=[("woG",)])
    P.op("dve", lambda e: e.tensor_scalar(nb, C.consts[:, gbc:gbc + 2], -1.0, None, ALU.mult), reads=[("consts",)], writes=[("nb",)])
    for cp in range(2):
        P.op("dve", lambda e, cp=cp: e.memset(S[cp], 0.0), writes=[("S", cp)])
        P.op("dve", lambda e, cp=cp: e.memset(S_bf[cp], 0.0), writes=[("Sbf", cp)])

    def proj(cols, j, M=128):
        b = C.bank()
        ts = tsl(j)

        def f(eng):
            r = None
            for k in range(8):
                r = eng.matmul(C.ps[0:M, b, :], wG[:, k, cols], C.hT[:, k, ts], start=(k == 0), stop=(k == 7))
            return r

        P.op("pe", f, reads=[("wG",)] + [("hT", k, j) for k in range(8)], writes=[("ps", b)])
        return b

    for j in range(4):
        ts = tsl(j)
        b = proj(slice(1024, 1040), j, M=16)
        P.op("act", lambda e, b=b: e.copy(gaT[0:16, :], C.ps[0:16, b, :]), reads=[("ps", b)], writes=[("gaT",)])
        for cp in range(2):
            b = C.bank()
            P.op("pe", lambda e, b=b, cp=cp: e.matmul(C.ps[:, b, :], wgb[0:16, cp * 128:(cp + 1) * 128], gaT[0:16, :], start=True, stop=True),
                 reads=[("wgb",), ("gaT",)], writes=[("ps", b)])
            P.op("act", lambda e, b=b, cp=cp: e.activation(ebuf, C.ps[:, b, :], AF.Exp, bias=nb[:, cp:cp + 1], scale=-1.0),
                 reads=[("ps", b), ("nb",)], writes=[("ebuf",)])
            P.op("act", lambda e: e.activation(lbuf, ebuf, AF.Ln, bias=onec, scale=1.0), reads=[("ebuf",)], writes=[("lbuf",)])
            P.op("dve", lambda e, cp=cp: e.tensor_tensor_scan(cl[:, cp, :], C.maskb[:, 0:512], lbuf, 0.0, ALU.mult, ALU.add),
                 reads=[("lbuf",), ("mask",)], writes=[("cl", cp)])
            P.op("act", lambda e, cp=cp: e.activation(eb, cl[:, cp, :], AF.Exp, scale=-1.0 / 16), reads=[("cl", cp)], writes=[("eb",)])
            P.op("act", lambda e, cp=cp: e.activation(einv, cl[:, cp, :], AF.Exp, scale=1.0 / 16), reads=[("cl", cp)], writes=[("einv",)])
            clv = cl[:, cp, :]
            clast_b = bc(clv[:, 63:64], [[64, 8], [0, 64]])
            clast = bc(clv[:, 63:64], [[64, 8]])
            P.op("dve", lambda e, cp=cp, clast_b=clast_b: e.tensor_tensor(dend.rearrange("p (c l) -> p c l", c=8),
                                                                       cl[:, cp, :].rearrange("p (c l) -> p c l", c=8), clast_b, ALU.subtract),
                 reads=[("cl", cp)], writes=[("dend",)])
            P.op("act", lambda e: e.activation(dend, dend, AF.Exp, scale=1.0 / 16), reads=[("dend",)], writes=[("dend",)])
            P.op("act", lambda e, cp=cp, clast=clast: e.activation(decs[:, cp, :], clast, AF.Exp, scale=-1.0 / 16),
                 reads=[("cl", cp)], writes=[("decs", cp)])
            b = proj(slice(cp * 128, (cp + 1) * 128), j)
            P.op("dve", lambda e, b=b, cp=cp: e.scalar_tensor_tensor(q_dec[:, cp, :], C.ps[:, b, :], 0.125, eb, ALU.mult, ALU.mult),
                 reads=[("ps", b), ("eb",)], writes=[("q_dec", cp)])
            b = proj(slice(256 + cp * 128, 256 + (cp + 1) * 128), j)
            P.op("dve", lambda e, b=b, cp=cp: e.tensor_tensor(k_inv[:, cp, :], C.ps[:, b, :], einv, ALU.mult),
                 reads=[("ps", b), ("einv",)], writes=[("k_inv", cp)])
            P.op("dve", lambda e, b=b, cp=cp: e.tensor_tensor(k_end[:, cp, :], C.ps[:, b, :], dend, ALU.mult),
                 reads=[("ps", b), ("dend",)], writes=[("k_end", cp)])
        for hh in range(4):
            b = proj(slice(1040 + hh * 128, 1040 + (hh + 1) * 128), j)
            P.op("act", lambda e, b=b, hh=hh: e.activation(grs[:, hh, :], C.ps[:, b, :], AF.Silu), reads=[("ps", b)], writes=[("grs", hh)])
        for nl in range(4):
            n = 4 * j + nl
            b = C.bank()

            def f_gv(eng, b=b, n=n):
                r = None
                for k in range(8):
                    r = eng.matmul(C.ps[:, b, :], C.hT[:, k, n * 128:(n + 1) * 128], wG[:, k, 512:1024], start=(k == 0), stop=(k == 7))
                return r

            P.op("pe", f_gv, reads=[("wG",)] + [("hT", k, j) for k in range(8)], writes=[("ps", b)])
            P.op("act", lambda e, b=b, nl=nl: e.copy(gv_tok[:, nl, :], C.ps[:, b, :]), reads=[("ps", b)], writes=[("gv", nl)])
            for cp in range(2):
                b = C.bank()
                P.op("pe", lambda e, b=b, cp=cp, nl=nl: e.matmul(C.ps[:, b, 0:128], k_end[:, cp, nl * 128:(nl + 1) * 128], C.ident_bf[:, :], start=True, stop=True),
                     reads=[("k_end", cp), ("ident",)], writes=[("ps", b)])
                P.op("dve", lambda e, b=b, cp=cp, nl=nl: e.tensor_copy(ket[:, nl, cp, :], C.ps[:, b, 0:128]), reads=[("ps", b)], writes=[("ket", nl, cp)])
        for nl in range(4):
            bs = slice(nl * 128, (nl + 1) * 128)
            bX = C.bank()
            bY = C.bank()

            def f_att(eng, bX=bX, bY=bY, bs=bs):
                r = None
                for h in range(4):
                    cp, half = h // 2, h % 2
                    hp = slice(half * 64, half * 64 + 64)
                    bb = bX if half == 0 else bY
                    r = eng.matmul(C.ps[:, bb, cp * 128:(cp + 1) * 128], k_inv[hp, cp, bs], q_dec[hp, cp, bs], start=True, stop=True)
                return r

            P.op("pe", f_att, reads=[("k_inv", 0), ("k_inv", 1), ("q_dec", 0), ("q_dec", 1)], writes=[("ps", bX), ("ps", bY)])
            for half, bb in ((0, bX), (1, bY)):
                P.op("dve", lambda e, half=half, bb=bb: e.tensor_tensor(attm[:, half:4:2, :], C.ps[:, bb, 0:256].rearrange("p (h f) -> p h f", h=2),
                                                                   bc(C.mask[:, 384:512], [[0, 2], [1, 128]]), ALU.mult),
                     reads=[("ps", bb), ("mask",)], writes=[("attm", half)])
            for x in range(2):
                xs = slice(nl * 128 + x * 64, nl * 128 + x * 64 + 64)
                rows = slice(x * 64, x * 64 + 64)
                ci = nl * 2 + x

                def f_inter(eng, xs=xs, x=x):
                    r = None
                    for h in range(4):
                        cp, half = h // 2, h % 2
                        hp = slice(half * 64, half * 64 + 64)
                        r = eng.matmul(C.ps[:, half, cp * 128 + x * 64:cp * 128 + x * 64 + 64], S_bf[cp][hp, :], q_dec[hp, cp, xs],
                                       start=(x == 0 and cp == 0), stop=False, skip_group_check=True)
                    return r

                P.op("pe", f_inter, reads=[("Sbf", 0), ("Sbf", 1), ("q_dec", 0), ("q_dec", 1)], writes=[("ps", 0), ("ps", 1)])
                for cp in range(2):
                    bk = C.bank()
                    P.op("pe", lambda e, bk=bk, cp=cp, rows=rows, nl=nl: e.matmul(C.ps[:, bk, 0:256], ket[rows, nl, cp, :],
                                                                            gv_tok[rows, nl, cp * 256:(cp + 1) * 256], start=True, stop=True),
                         reads=[("ket", nl, cp), ("gv", nl)], writes=[("ps", bk)])
                    for half in range(2):
                        hp = slice(half * 64, half * 64 + 64)
                        P.op("dve", lambda e, bk=bk, cp=cp, hp=hp, half=half, ci=ci: e.scalar_tensor_tensor(
                            S[cp][hp, :], S[cp][hp, :], decs[hp, cp, ci:ci + 1], C.ps[hp, bk, half * 128:(half + 1) * 128], ALU.mult, ALU.add),
                             reads=[("ps", bk), ("decs", cp), ("S", cp)], writes=[("S", cp)])
                    P.op("act", lambda e, cp=cp: e.copy(S_bf[cp], S[cp]), reads=[("S", cp)], writes=[("Sbf", cp)])

            def f_intra(eng, nl=nl):
                r = None
                for h in range(4):
                    cp, half = h // 2, h % 2
                    r = eng.matmul(C.ps[:, half, cp * 128:(cp + 1) * 128], gv_tok[:, nl, h * 128:(h + 1) * 128], attm[:, h, :], start=False, stop=True,
                                   skip_group_check=True)
                return r

            P.op("pe", f_intra, reads=[("gv", nl), ("attm", 0), ("attm", 1)], writes=[("ps", 0), ("ps", 1)])
            for half in range(2):
                P.op("act", lambda e, half=half: e.activation(sq, C.ps[:, half, 0:256], AF.Square), reads=[("ps", half)], writes=[("gsq",)])
                b2 = C.bank()
                P.op("pe", lambda e, b2=b2: e.matmul(C.ps[:, b2, 0:256], C.ones_bf[:, :], sq, start=True, stop=True), reads=[("gsq",), ("ones",)], writes=[("ps", b2)])
                P.op("act", lambda e, b2=b2: e.activation(rstd, C.ps[:, b2, 0:256], AF.Sqrt, bias=C.consts[:, C.eps_col:C.eps_col + 1], scale=1.0 / 128),
                     reads=[("ps", b2)], writes=[("grstd",)])
                P.op("dve", lambda e: e.reciprocal(rstd, rstd), reads=[("grstd",)], writes=[("grstd",)])
                P.op("dve", lambda e, half=half: e.scalar_tensor_tensor(ytmp, C.ps[:, half, 0:256], glan, rstd, ALU.mult, ALU.mult),
                     reads=[("ps", half), ("grstd",)], writes=[("ytmp",)])
                P.op("dve", lambda e, half=half, bs=bs: e.tensor_tensor(oG[:, half:4:2, bs], ytmp.rearrange("p (h f) -> p h f", h=2),
                                                                   grs[:, half:4:2, bs], ALU.mult),
                     reads=[("ytmp",), ("grs", half), ("grs", half + 2)], writes=[("oG",)])
        for d in range(8):
            b = C.bank()

            def f_mm(eng, d=d, b=b):
                r = None
                for c in range(4):
                    r = eng.matmul(C.ps[:, b, :], woG[:, c, d * 128:(d + 1) * 128], oG[:, c, :], start=(c == 0), stop=(c == 3))
                return r

            P.op("pe", f_mm, reads=[("woG",), ("oG",)], writes=[("ps", b)])
            P.op("dve", lambda e, d=d, b=b, ts=ts: e.tensor_tensor(C.xT[:, d, ts], C.ps[:, b, :], C.xT[:, d, ts], ALU.add),
                 reads=[("ps", b), ("xT", d, j)], writes=[("xT", d, j)])


def ssd_stage(P, C, s):
    w_in = C.need("e_w_in", [D, 3760], lambda inp: inp["e_w_in"][0])
    w_out = C.need("e_w_out", [1536, D], lambda inp: inp["e_w_out"][0])
    cc = C.ccols
    A = Arena(C)
    wp = [A.bf(8, 512) for _ in range(2)]
    wdt = A.bf(8, 16)
    woS = A.bf(8, D)
    zs = A.bf(8, 512)
    xsT = A.bf(8, 512)
    BT = A.bf(4, 512)
    CT = A.bf(4, 512)
    rb = [A.f32(515) for _ in range(2)]
    acc = [A.f32(512) for _ in range(2)]
    halo = A.f32(16, 3)
    x1 = A.f32(16)
    dt = A.f32(16)
    a_ = A.f32(16)
    aneg = A.f32(16)
    cst = A.f32(16)
    ncs = A.f32(16)
    ds = A.f32(16)
    cdec = A.f32(16)
    dtds = A.f32(16)
    xd = A.bf(1024)
    xdw = A.bf(1024)
    B_tok = A.bf(4, 128)
    CBs = A.f32(4, 128)
    Ecs = [A.f32(128) for _ in range(2)]
    E = [A.f32(128) for _ in range(2)]
    Coff = [A.bf(128) for _ in range(2)]
    Mh = [A.bf(128) for _ in range(2)]
    st = A.f32(1024)
    st_bf = A.bf(1024)
    yg = A.f32(8, 128)
    sq = A.bf(8, 128)
    rstd = A.f32(512)
    oS = A.bf(8, 512)
    w_in_v = w_in.rearrange("(k p) f -> p k f", p=128)
    onec = C.consts[:, cc["one"][0]:cc["one"][0] + 1]
    epsc = C.consts[:, C.eps_col:C.eps_col + 1]
    cwc, cbc, dsk, ssn = cc["e_convw"][0], cc["e_convb"][0], cc["e_dskip"][0], cc["e_ssmn"][0]
    MC = C.mask[:, 0:128]
    NEG = C.mask[:, 640:768]
    IDf = C.mask[:, 512:640]

    P.dma("pool", lambda e: e.dma_start(out=wdt, in_=w_in_v[:, :, 3072:3088]), "wB", writes=[("wdt",)])
    P.dma("pool", lambda e: e.dma_start(out=woS, in_=w_out[0:1024, :].rearrange("(c p) d -> p c d", p=128)), "wC", writes=[("woS",)])
    P.op("act", lambda e: e.activation(aneg, C.consts[:, cc["e_alog"][0]:cc["e_alog"][0] + 16], AF.Exp), reads=[("consts",)], writes=[("aneg",)])
    P.op("dve", lambda e: e.tensor_scalar(aneg, aneg, -1.0, None, ALU.mult), reads=[("aneg",)], writes=[("aneg",)])
    P.op("dve", lambda e: e.memset(st, 0.0), writes=[("st",)])
    P.op("dve", lambda e: e.memset(st_bf, 0.0), writes=[("st_bf",)])
    P.op("dve", lambda e: e.memset(halo, 0.0), writes=[("halo",)])
    ws = [0]
    rbi = [0]

    for j in range(4):
        ts = tsl(j)
        for pi in range(6):
            slot = ws[0]
            ws[0] = (ws[0] + 1) % 2
            P.dma("pool", lambda e, slot=slot, pi=pi: e.dma_start(out=wp[slot], in_=w_in_v[:, :, pi * 512:(pi + 1) * 512]),
                  f"wp{slot}", writes=[("wp", slot)])
            for ci in range(4):
                c = pi * 4 + ci
                b = C.bank()

                def f_mm(eng, slot=slot, ci=ci, b=b, ts=ts):
                    r = None
                    for k in range(8):
                        r = eng.matmul(C.ps[:, b, :], wp[slot][:, k, ci * 128:(ci + 1) * 128], C.hT[:, k, ts], start=(k == 0), stop=(k == 7))
                    return r

                P.op("pe", f_mm, reads=[("wp", slot)] + [("hT", k, j) for k in range(8)], writes=[("ps", b)])
                if c < 8:
                    P.op("act", lambda e, b=b, c=c: e.activation(zs[:, c, :], C.ps[:, b, :], AF.Silu), reads=[("ps", b)], writes=[("zs", c)])
                    continue
                xc = c - 8
                ri = rbi[0]
                rbi[0] = (rbi[0] + 1) % 2
                R, AC = rb[ri], acc[ri]
                P.op("act", lambda e, b=b, R=R: e.copy(R[:, 3:515], C.ps[:, b, :]), reads=[("ps", b)], writes=[("rb", ri)])
                P.op("dve", lambda e, R=R, xc=xc: e.tensor_copy(R[:, 0:3], halo[:, xc, :]), reads=[("halo",), ("rb", ri)], writes=[("rb", ri)])

                def wcol(xc, t):
                    return C.consts[:, cwc + xc * 4 + t:cwc + xc * 4 + t + 1]

                P.op("dve", lambda e, R=R, AC=AC, xc=xc: e.tensor_scalar(AC, R[:, 3:515], wcol(xc, 3), None, ALU.mult),
                     reads=[("rb", ri)], writes=[("acc", ri)])
                for t in (2, 1, 0):
                    P.op("dve", lambda e, R=R, AC=AC, xc=xc, t=t: e.scalar_tensor_tensor(AC, R[:, t:t + 512], wcol(xc, t), AC, ALU.mult, ALU.add),
                         reads=[("rb", ri), ("acc", ri)], writes=[("acc", ri)])
                P.op("dve", lambda e, R=R, xc=xc: e.tensor_copy(halo[:, xc, :], R[:, 512:515]), reads=[("rb", ri), ("halo",)], writes=[("halo",)])
                if xc < 8:
                    dest, key = xsT[:, xc, :], ("xsT", xc)
                elif xc < 12:
                    dest, key = BT[:, xc - 8, :], ("BT", xc - 8)
                else:
                    dest, key = CT[:, xc - 12, :], ("CT", xc - 12)
                P.op("act", lambda e, AC=AC, dest=dest, xc=xc: e.activation(dest, AC, AF.Silu, bias=C.consts[:, cbc + xc:cbc + xc + 1], scale=1.0),
                     reads=[("acc", ri)], writes=[key])
        for nl in range(4):
            n = 4 * j + nl
            bs = slice(nl * 128, (nl + 1) * 128)
            b = C.bank()

            def f_dt(eng, b=b, n=n):
                r = None
                for k in range(8):
                    r = eng.matmul(C.ps[:, b, 0:16], C.hT[:, k, n * 128:(n + 1) * 128], wdt[:, k, :], start=(k == 0), stop=(k == 7))
                return r

            P.op("pe", f_dt, reads=[("wdt",)] + [("hT", k, j) for k in range(8)], writes=[("ps", b)])
            P.op("dve", lambda e, b=b: e.tensor_tensor(x1, C.ps[:, b, 0:16], C.consts[:, cc["e_dtb"][0]:cc["e_dtb"][0] + 16], ALU.add),
                 reads=[("ps", b)], writes=[("x1",)])
            P.op("act", lambda e: e.activation(x1, x1, AF.Exp), reads=[("x1",)], writes=[("x1",)])
            P.op("act", lambda e: e.activation(dt, x1, AF.Ln, bias=onec, scale=1.0), reads=[("x1",)], writes=[("dt",)])
            P.op("dve", lambda e: e.tensor_tensor(a_, dt, aneg, ALU.mult), reads=[("dt",), ("aneg",)], writes=[("a",)])
            b = C.bank()

            def f_cs(eng, b=b):
                eng.matmul(C.ps[:, b, 0:16], MC, a_, start=True, stop=True)
                return eng.matmul(C.ps[:, b, 16:32], bc(onec, [[0, 128]]), a_, start=True, stop=True)

            P.op("pe", f_cs, reads=[("a",), ("mask",)], writes=[("ps", b)])
            P.op("dve", lambda e, b=b: e.tensor_copy(cst, C.ps[:, b, 0:16]), reads=[("ps", b)], writes=[("cst",)])
            P.op("dve", lambda e: e.tensor_scalar(ncs, cst, -1.0, None, ALU.mult), reads=[("cst",)], writes=[("ncs",)])
            P.op("dve", lambda e, b=b: e.tensor_tensor(ds, C.ps[:, b, 16:32], cst, ALU.subtract), reads=[("ps", b), ("cst",)], writes=[("ds",)])
            P.op("act", lambda e: e.activation(ds, ds, AF.Exp), reads=[("ds",)], writes=[("ds",)])
            P.op("act", lambda e, b=b: e.activation(cdec, C.ps[:, b, 16:32], AF.Exp), reads=[("ps", b)], writes=[("cdec",)])
            P.op("dve", lambda e: e.tensor_tensor(dtds, dt, ds, ALU.mult), reads=[("dt",), ("ds",)], writes=[("dtds",)])
            for hb in range(2):
                b = C.bank()

                def f_tr(eng, b=b, hb=hb, bs=bs):
                    r = None
                    for cq in range(4):
                        c = hb * 4 + cq
                        r = eng.matmul(C.ps[:, b, cq * 128:(cq + 1) * 128], xsT[:, c, bs], C.ident_bf[:, :], start=True, stop=True)
                    return r

                P.op("pe", f_tr, reads=[("xsT", hb * 4 + q) for q in range(4)] + [("ident",)], writes=[("ps", b)])
                P.op("dve", lambda e, b=b, hb=hb: e.tensor_tensor(xd[:, hb * 512:(hb + 1) * 512].rearrange("p (h q) -> p h q", h=8),
                                                             C.ps[:, b, :].rearrange("p (h q) -> p h q", h=8),
                                                             bc(dt[:, hb * 8:hb * 8 + 8], [[1, 8], [0, 64]]), ALU.mult),
                     reads=[("ps", b), ("dt",)], writes=[("xd", hb)])
                P.op("dve", lambda e, b=b, hb=hb: e.tensor_tensor(xdw[:, hb * 512:(hb + 1) * 512].rearrange("p (h q) -> p h q", h=8),
                                                             C.ps[:, b, :].rearrange("p (h q) -> p h q", h=8),
                                                             bc(dtds[:, hb * 8:hb * 8 + 8], [[1, 8], [0, 64]]), ALU.mult),
                     reads=[("ps", b), ("dtds",)], writes=[("xdw", hb)])
            b = C.bank()

            def f_trb(eng, b=b, bs=bs):
                r = None
                for g in range(4):
                    r = eng.matmul(C.ps[:, b, g * 128:(g + 1) * 128], BT[:, g, bs], C.ident_bf[:, :], start=True, stop=True)
                return r

            P.op("pe", f_trb, reads=[("BT", g) for g in range(4)] + [("ident",)], writes=[("ps", b)])
            P.op("act", lambda e, b=b: e.copy(B_tok.rearrange("p g n -> p (g n)"), C.ps[:, b, :]), reads=[("ps", b)], writes=[("B_tok",)])
            b = C.bank()

            def f_cb(eng, b=b, bs=bs):
                r = None
                for g in range(4):
                    r = eng.matmul(C.ps[:, b, g * 128:(g + 1) * 128], BT[:, g, bs], CT[:, g, bs], start=True, stop=True)
                return r

            P.op("pe", f_cb, reads=[("BT", g) for g in range(4)] + [("CT", g) for g in range(4)], writes=[("ps", b)])
            P.op("act", lambda e, b=b: e.copy(CBs.rearrange("p g n -> p (g n)"), C.ps[:, b, :]), reads=[("ps", b)], writes=[("CBs",)])
            for h in range(16):
                g = h // 4
                hi = h % 2
                b = C.bank()

                def f_csb(eng, b=b, h=h):
                    al = bc(a_[:, h:h + 1], [[0, 128]])
                    eng.matmul(C.ps[:, b, 0:128], al, MC, start=True, stop=True)
                    eng.matmul(C.ps[:, b, 128:256], al, MC, start=False, stop=False, skip_group_check=True)
                    return eng.matmul(C.ps[:, b, 128:256], IDf, NEG, start=False, stop=True, skip_group_check=True)

                P.op("pe", f_csb, reads=[("a",), ("mask",)], writes=[("ps", b)])
                P.op("act", lambda e, b=b, hi=hi: e.activation(Ecs[hi], C.ps[:, b, 0:128], AF.Exp), reads=[("ps", b)], writes=[("Ecs", hi)])
                P.op("act", lambda e, b=b, hi=hi, h=h: e.activation(E[hi], C.ps[:, b, 128:256], AF.Exp, bias=ncs[:, h:h + 1], scale=1.0),
                     reads=[("ps", b), ("ncs",)], writes=[("E", hi)])
                P.op("pool", lambda e, hi=hi, g=g, bs=bs: e.tensor_tensor(Coff[hi], CT[:, g, bs], Ecs[hi], ALU.mult),
                     reads=[("Ecs", hi), ("CT", g)], writes=[("Coff", hi)])
                P.op("dve", lambda e, hi=hi, g=g: e.tensor_tensor(Mh[hi], E[hi], CBs[:, g, :], ALU.mult),
                     reads=[("E", hi), ("CBs",)], writes=[("Mh", hi)])

                def f_y(eng, h=h, hi=hi):
                    yb = h // 8
                    col = ((h % 8) // 2) * 128
                    first = (h % 8) < 2
                    if hi == 0:
                        out = C.ps[0:64, yb, col:col + 128]
                        kw = {}
                    else:
                        out = C.ps[64:128, yb, col:col + 128]
                        kw = {"tile_position": (0, 64)}
                    eng.matmul(out, xd[:, h * 64:(h + 1) * 64], Mh[hi], start=first, stop=False, skip_group_check=True, **kw)
                    return eng.matmul(out, st_bf[:, h * 64:(h + 1) * 64], Coff[hi], start=False, stop=True, skip_group_check=True, **kw)

                P.op("pe", f_y, reads=[("xd", h // 8), ("Mh", hi), ("st_bf",), ("Coff", hi)], writes=[("ps", h // 8)])
            bst = [C.bank(), C.bank()]

            def f_st(eng, bst=bst):
                r = None
                for g in range(4):
                    r = eng.matmul(C.ps[:, bst[g // 2], (g % 2) * 256:(g % 2) * 256 + 256], B_tok[:, g, :], xdw[:, g * 256:(g + 1) * 256],
                                   start=True, stop=True)
                return r

            P.op("pe", f_st, reads=[("B_tok",), ("xdw", 0), ("xdw", 1)], writes=[("ps", bst[0]), ("ps", bst[1])])
            P.op("dve", lambda e: e.tensor_tensor(st.rearrange("p (h q) -> p h q", h=16), st.rearrange("p (h q) -> p h q", h=16),
                                             bc(cdec, [[1, 16], [0, 64]]), ALU.mult), reads=[("st",), ("cdec",)], writes=[("st",)])
            for hb in range(2):
                P.op("dve", lambda e, hb=hb, bst=bst: e.tensor_tensor(st[:, hb * 512:(hb + 1) * 512], st[:, hb * 512:(hb + 1) * 512], C.ps[:, bst[hb], :], ALU.add),
                     reads=[("st",), ("ps", bst[hb])], writes=[("st",)])
            P.op("act", lambda e: e.copy(st_bf, st), reads=[("st",)], writes=[("st_bf",)])
            for c in range(8):
                yb, col = c // 4, (c % 4) * 128
                P.op("dve", lambda e, c=c, yb=yb, col=col, bs=bs: e.scalar_tensor_tensor(yg[:, c, :], xsT[:, c, bs], C.consts[:, dsk + c:dsk + c + 1],
                                                                                C.ps[:, yb, col:col + 128], ALU.mult, ALU.add),
                     reads=[("xsT", c), ("ps", yb)], writes=[("yg", c)])
                P.op("pool", lambda e, c=c, bs=bs: e.tensor_tensor(yg[:, c, :], yg[:, c, :], zs[:, c, bs], ALU.mult),
                     reads=[("yg", c), ("zs", c)], writes=[("yg", c)])
            P.op("act", lambda e: e.activation(sq, yg, AF.Square), reads=[("yg", c) for c in range(8)], writes=[("ssq",)])
            b = C.bank()

            def f_ss(eng, b=b):
                r = None
                for gi in range(4):
                    eng.matmul(C.ps[:, b, gi * 128:(gi + 1) * 128], C.ones_bf[:, :], sq[:, 2 * gi, :], start=(gi == 0), stop=False, skip_group_check=True)
                    r = eng.matmul(C.ps[:, b, gi * 128:(gi + 1) * 128], C.ones_bf[:, :], sq[:, 2 * gi + 1, :], start=False, stop=True, skip_group_check=True)
                return r

            P.op("pe", f_ss, reads=[("ssq",), ("ones",)], writes=[("ps", b)])
            P.op("act", lambda e, b=b: e.activation(rstd, C.ps[:, b, :], AF.Sqrt, bias=epsc, scale=1.0 / 256), reads=[("ps", b)], writes=[("srstd",)])
            P.op("dve", lambda e: e.reciprocal(rstd, rstd), reads=[("srstd",)], writes=[("srstd",)])
            for c in range(8):
                P.op("dve", lambda e, c=c, bs=bs: e.scalar_tensor_tensor(oS[:, c, bs], yg[:, c, :], C.consts[:, ssn + c:ssn + c + 1],
                                                                    rstd[:, (c // 2) * 128:(c // 2 + 1) * 128], ALU.mult, ALU.mult),
                     reads=[("yg", c), ("srstd",)], writes=[("oS",)])
        for d in range(8):
            b = C.bank()

            def f_mm(eng, d=d, b=b):
                r = None
                for c in range(8):
                    r = eng.matmul(C.ps[:, b, :], woS[:, c, d * 128:(d + 1) * 128], oS[:, c, :], start=(c == 0), stop=(c == 7))
                return r

            P.op("pe", f_mm, reads=[("woS",), ("oS",)], writes=[("ps", b)])
            P.op("dve", lambda e, d=d, b=b, ts=ts: e.tensor_tensor(C.xT[:, d, ts], C.ps[:, b, :], C.xT[:, d, ts], ALU.add),
                 reads=[("ps", b), ("xT", d, j)], writes=[("xT", d, j)])


def rope_tables(P, C, s, j, posi, ang, tmpf, tmpi, C96, S96):
    cc = C.ccols
    invf = C.consts[:, cc["invf"][0]:cc["invf"][0] + 1]
    sgn = C.consts[:, cc["sgn"][0]:cc["sgn"][0] + 1]
    P.dma("sp", lambda e: e.dma_start(out=posi, in_=bass.AP(C.pos.tensor, C.pos[s:s + 1, j * 512:(j + 1) * 512].offset, [[0, 128], [1, 512]])),
          "posB", writes=[("posi",)])
    P.op("dve", lambda e: e.tensor_copy(ang, posi), reads=[("posi",)], writes=[("ang",)])
    P.op("dve", lambda e: e.tensor_scalar(ang, ang, invf, None, ALU.mult), reads=[("ang",)], writes=[("ang",)])

    def frac(buf):
        P.op("dve", lambda e: e.tensor_copy(tmpi, buf), reads=[("ang",)], writes=[("tmpi",)])
        P.op("dve", lambda e: e.tensor_copy(tmpf, tmpi), reads=[("tmpi",)], writes=[("tmpf",)])
        P.op("dve", lambda e: e.tensor_tensor(buf, buf, tmpf, ALU.subtract), reads=[("tmpf",), ("ang",)], writes=[("ang",)])
        P.op("dve", lambda e: e.tensor_single_scalar(tmpf, buf, 0.5, ALU.is_gt), reads=[("ang",)], writes=[("tmpf",)])
        P.op("dve", lambda e: e.tensor_tensor(buf, buf, tmpf, ALU.subtract), reads=[("tmpf",), ("ang",)], writes=[("ang",)])
        P.op("dve", lambda e: e.tensor_single_scalar(tmpf, buf, -0.5, ALU.is_lt), reads=[("ang",)], writes=[("tmpf",)])
        P.op("dve", lambda e: e.tensor_tensor(buf, buf, tmpf, ALU.add), reads=[("tmpf",), ("ang",)], writes=[("ang",)])

    frac(ang)
    P.op("act", lambda e: e.activation(S96, ang, AF.Sin, scale=float(2 * np.pi)), reads=[("ang",)], writes=[("S96",)])
    P.op("dve", lambda e: e.tensor_scalar(S96, S96, sgn, None, ALU.mult), reads=[("S96",)], writes=[("S96",)])
    P.op("dve", lambda e: e.tensor_scalar(ang, ang, 0.25, None, ALU.add), reads=[("ang",), ("S96",)], writes=[("ang",)])
    frac(ang)
    P.op("act", lambda e: e.activation(C96, ang, AF.Sin, scale=float(2 * np.pi)), reads=[("ang",)], writes=[("C96",)])


def mla_kv_stage(P, C, s):
    w_in = C.need("e_w_in", [D, 3760], lambda inp: inp["e_w_in"][0])
    w_kvb = C.need("e_w_kv_b", [256, 1024], lambda inp: inp["e_w_kv_b"][0])
    cc = C.ccols
    A = Arena(C)
    kT = A.bf(8, T)
    V_tok = A.bf(16, 512)
    wA = A.bf(8, 288)
    wpp = A.bf(8, 32)
    wkvb = A.bf(2, 1024)
    kvn = A.bf(2, 512)
    sq = A.bf(512)
    sqk = A.bf(512)
    rstd = A.f32(512)
    posi = A.i32(512)
    ang = A.f32(512)
    tmpf = A.f32(512)
    tmpi = A.i32(512)
    C96 = A.f32(512)
    S96 = A.f32(512)
    kr = A.f32(512)
    t1 = A.f32(512)
    w_in_v = w_in.rearrange("(k p) f -> p k f", p=128)
    epsc = C.consts[:, C.eps_col:C.eps_col + 1]
    kvan = cc["e_kvan"][0]
    gk = C.consts[:, cc["gk"][0]:cc["gk"][0] + 1]
    gkp = C.consts[:, cc["gkp"][0]:cc["gkp"][0] + 1]
    R = slice(64, 96)

    P.dma("pool", lambda e: e.dma_start(out=wA, in_=w_in_v[:, :, 3472:3760]), "wA", writes=[("wA",)])
    P.dma("pool", lambda e: [e.dma_start(out=wpp[:, :, 0:16], in_=w_in_v[:, :, 3744:3760]),
                             e.dma_start(out=wpp[:, :, 16:32], in_=w_in_v[:, :, 3728:3744])], "wB", writes=[("wpp",)], n=2)
    P.dma("pool", lambda e: e.dma_start(out=wkvb, in_=w_kvb.rearrange("(k p) f -> p k f", p=128)), "wC", writes=[("wkvb",)])

    for j in range(4):
        ts = tsl(j)
        rope_tables(P, C, s, j, posi, ang, tmpf, tmpi, C96, S96)
        bk = []
        for c in range(2):
            b = C.bank()
            bk.append(b)

            def f_mm(eng, c=c, b=b, ts=ts):
                r = None
                for k in range(8):
                    r = eng.matmul(C.ps[:, b, :], wA[:, k, c * 128:(c + 1) * 128], C.hT[:, k, ts], start=(k == 0), stop=(k == 7))
                return r

            P.op("pe", f_mm, reads=[("wA",)] + [("hT", k, j) for k in range(8)], writes=[("ps", b)])
        bss = C.bank()
        for c in range(2):
            P.op("act", lambda e, c=c, bk=bk: e.activation(sq, C.ps[:, bk[c], :], AF.Square), reads=[("ps", bk[c])], writes=[("msq",)])
            P.op("pe", lambda e, c=c, bss=bss: e.matmul(C.ps[:, bss, :], C.ones_bf[:, :], sq, start=(c == 0), stop=(c == 1)),
                 reads=[("msq",), ("ones",)], writes=[("ps", bss)])
        P.op("act", lambda e, bss=bss: e.activation(rstd, C.ps[:, bss, :], AF.Sqrt, bias=epsc, scale=1.0 / 256), reads=[("ps", bss)], writes=[("mrstd",)])
        P.op("dve", lambda e: e.reciprocal(rstd, rstd), reads=[("mrstd",)], writes=[("mrstd",)])
        for c in range(2):
            P.op("dve", lambda e, c=c, bk=bk: e.scalar_tensor_tensor(kvn[:, c, :], C.ps[:, bk[c], :], C.consts[:, kvan + c:kvan + c + 1], rstd, ALU.mult, ALU.mult),
                 reads=[("ps", bk[c]), ("mrstd",)], writes=[("kvn", c)])
        bx = C.bank()
        by = C.bank()

        def f_pe(eng, bx=bx, ts=ts):
            r = None
            for k in range(8):
                r = eng.matmul(C.ps[64:96, bx, :], wA[:, k, 256:288], C.hT[:, k, ts], start=(k == 0), stop=(k == 7), tile_position=(0, 64))
            return r

        def f_pp(eng, by=by, ts=ts):
            r = None
            for k in range(8):
                r = eng.matmul(C.ps[64:96, by, :], wpp[:, k, :], C.hT[:, k, ts], start=(k == 0), stop=(k == 7), tile_position=(0, 64))
            return r

        P.op("pe", f_pe, reads=[("wA",)] + [("hT", k, j) for k in range(8)], writes=[("ps", bx)])
        P.op("pe", f_pp, reads=[("wpp",)] + [("hT", k, j) for k in range(8)], writes=[("ps", by)])
        P.op("dve", lambda e, bx=bx: e.scalar_tensor_tensor(kr[R, :], C.ps[R, bx, :], gk[R, :], C96[R, :], ALU.mult, ALU.mult),
             reads=[("ps", bx), ("C96",)], writes=[("kr",)])
        P.op("dve", lambda e, by=by: e.scalar_tensor_tensor(t1[R, :], C.ps[R, by, :], gkp[R, :], S96[R, :], ALU.mult, ALU.mult),
             reads=[("ps", by), ("S96",)], writes=[("t1",)])
        P.op("dve", lambda e: e.tensor_tensor(kr[R, :], kr[R, :], t1[R, :], ALU.add), reads=[("kr",), ("t1",)], writes=[("kr",)])
        P.op("act", lambda e, bx=bx: e.activation(sqk[R, :], C.ps[R, bx, :], AF.Square), reads=[("ps", bx)], writes=[("sqk",)])
        for h in range(8):
            b = C.bank()

            def f_kn(eng, h=h, b=b):
                r = None
                for k in range(2):
                    r = eng.matmul(C.ps[0:64, b, :], wkvb[:, k, h * 128:h * 128 + 64], kvn[:, k, :], start=(k == 0), stop=(k == 1))
                return r

            P.op("pe", f_kn, reads=[("wkvb",), ("kvn", 0), ("kvn", 1)], writes=[("ps", b)])
            P.op("act", lambda e, b=b: e.activation(sqk[0:64, :], C.ps[0:64, b, :], AF.Square), reads=[("ps", b)], writes=[("sqk",)])
            b2 = C.bank()
            P.op("pe", lambda e, b2=b2: e.matmul(C.ps[0:96, b2, :], C.ones_bf[0:96, 0:96], sqk[0:96, :], start=True, stop=True),
                 reads=[("sqk",), ("ones",)], writes=[("ps", b2)])
            P.op("act", lambda e, b2=b2: e.activation(rstd[0:96, :], C.ps[0:96, b2, :], AF.Sqrt, bias=epsc[0:96, :], scale=1.0 / 96),
                 reads=[("ps", b2)], writes=[("mrstd",)])
            P.op("dve", lambda e: e.reciprocal(rstd[0:96, :], rstd[0:96, :]), reads=[("mrstd",)], writes=[("mrstd",)])
            P.op("dve", lambda e, h=h, b=b, ts=ts: e.scalar_tensor_tensor(kT[0:64, h, ts], C.ps[0:64, b, :], gk[0:64, :], rstd[0:64, :], ALU.mult, ALU.mult),
                 reads=[("ps", b), ("mrstd",)], writes=[("kT", h, j)])
            P.op("dve", lambda e, h=h, ts=ts: e.tensor_tensor(kT[R, h, ts], kr[R, :], rstd[R, :], ALU.mult),
                 reads=[("kr",), ("mrstd",)], writes=[("kT", h, j)])
        for nl in range(4):
            n = 4 * j + nl
            b = C.bank()

            def f_v(eng, b=b, nl=nl):
                r = None
                for k in range(2):
                    r = eng.matmul(C.ps[:, b, :], kvn[:, k, nl * 128:(nl + 1) * 128],
                                   wkvb[:, k, :].rearrange("p (h x) -> p h x", h=8)[:, :, 64:128], start=(k == 0), stop=(k == 1))
                return r

            P.op("pe", f_v, reads=[("wkvb",), ("kvn", 0), ("kvn", 1)], writes=[("ps", b)])
            P.op("act", lambda e, b=b, n=n: e.copy(V_tok[:, n, :], C.ps[:, b, :]), reads=[("ps", b)], writes=[("V_tok", n)])


def mla_attn_stage(P, C, s):
    w_in = C.need("e_w_in", [D, 3760], lambda inp: inp["e_w_in"][0])
    w_qb = C.need("e_w_q_b", [384, 768], lambda inp: inp["e_w_q_b"][0])
    w_out = C.need("e_w_out", [1536, D], lambda inp: inp["e_w_out"][0])
    cc = C.ccols
    A = Arena(C)
    kT = A.bf(8, T)
    V_tok = A.bf(16, 512)
    wA = A.bf(8, 384)
    wqb = A.bf(3, 768)
    wqbp = A.bf(3, 8, 32)
    woM = A.bf(4, D)
    qan = A.bf(3, 512)
    sq = A.bf(512)
    rstd = A.f32(512)
    posi = A.i32(512)
    ang = A.f32(512)
    tmpf = A.f32(512)
    tmpi = A.i32(512)
    C96 = A.f32(512)
    S96 = A.f32(512)
    t1 = A.f32(512)
    t2 = A.f32(512)
    qT = [A.bf(512) for _ in range(2)]
    Pb = [A.bf(512) for _ in range(3)]
    rden = A.f32(512)
    oT = A.bf(4, 512)
    w_in_v = w_in.rearrange("(k p) f -> p k f", p=128)
    epsc = C.consts[:, C.eps_col:C.eps_col + 1]
    qanc = cc["e_qan"][0]
    gq = C.consts[:, cc["gq"][0]:cc["gq"][0] + 1]
    gqp = C.consts[:, cc["gqp"][0]:cc["gqp"][0] + 1]
    R = slice(64, 96)
    SC = float(96 ** -0.5)
    w_qb_v = w_qb.rearrange("(k p) (h x) -> p k h x", p=128, h=8)

    P.dma("pool", lambda e: e.dma_start(out=wA, in_=w_in_v[:, :, 3088:3472]), "wA", writes=[("wA",)])
    P.dma("pool", lambda e: e.dma_start(out=wqb, in_=w_qb.rearrange("(k p) f -> p k f", p=128)), "wB", writes=[("wqb",)])
    P.dma("pool", lambda e: [e.dma_start(out=wqbp[:, k, :, 0:16], in_=w_qb_v[:, k, :, 80:96]) for k in range(3)]
          + [e.dma_start(out=wqbp[:, k, :, 16:32], in_=w_qb_v[:, k, :, 64:80]) for k in range(3)], "wC", writes=[("wqbp",)], n=6)
    P.dma("pool", lambda e: e.dma_start(out=woM, in_=w_out[1024:1536, :].rearrange("(c p) d -> p c d", p=128)), "wD", writes=[("woM",)])
    pbi = [0]
    qi = [0]

    for j in range(4):
        ts = tsl(j)
        rope_tables(P, C, s, j, posi, ang, tmpf, tmpi, C96, S96)
        bq = []
        for c in range(3):
            b = C.bank()
            bq.append(b)

            def f_mm(eng, c=c, b=b, ts=ts):
                r = None
                for k in range(8):
                    r = eng.matmul(C.ps[:, b, :], wA[:, k, c * 128:(c + 1) * 128], C.hT[:, k, ts], start=(k == 0), stop=(k == 7))
                return r

            P.op("pe", f_mm, reads=[("wA",)] + [("hT", k, j) for k in range(8)], writes=[("ps", b)])
        bss = C.bank()
        for c in range(3):
            P.op("act", lambda e, c=c, bq=bq: e.activation(sq, C.ps[:, bq[c], :], AF.Square), reads=[("ps", bq[c])], writes=[("msq",)])
            P.op("pe", lambda e, c=c, bss=bss: e.matmul(C.ps[:, bss, :], C.ones_bf[:, :], sq, start=(c == 0), stop=(c == 2)),
                 reads=[("msq",), ("ones",)], writes=[("ps", bss)])
        P.op("act", lambda e, bss=bss: e.activation(rstd, C.ps[:, bss, :], AF.Sqrt, bias=epsc, scale=1.0 / 384), reads=[("ps", bss)], writes=[("mrstd",)])
        P.op("dve", lambda e: e.reciprocal(rstd, rstd), reads=[("mrstd",)], writes=[("mrstd",)])
        for c in range(3):
            P.op("dve", lambda e, c=c, bq=bq: e.scalar_tensor_tensor(qan[:, c, :], C.ps[:, bq[c], :], C.consts[:, qanc + c:qanc + c + 1], rstd, ALU.mult, ALU.mult),
                 reads=[("ps", bq[c]), ("mrstd",)], writes=[("qan", c)])
        for h in range(8):
            hi = h % 2
            b = C.bank()
            bp = C.bank()

            def f_q(eng, h=h, b=b, bp=bp):
                r = None
                for k in range(3):
                    r = eng.matmul(C.ps[0:96, b, :], wqb[:, k, h * 96:(h + 1) * 96], qan[:, k, :], start=(k == 0), stop=(k == 2))
                for k in range(3):
                    r = eng.matmul(C.ps[64:96, bp, :], wqbp[:, k, h, :], qan[:, k, :], start=(k == 0), stop=(k == 2), tile_position=(0, 64))
                return r

            P.op("pe", f_q, reads=[("wqb",), ("wqbp",)] + [("qan", c) for c in range(3)], writes=[("ps", b), ("ps", bp)])
            P.op("act", lambda e, b=b: e.activation(sq[0:96, :], C.ps[0:96, b, :], AF.Square), reads=[("ps", b)], writes=[("msq",)])
            b2 = C.bank()
            P.op("pe", lambda e, b2=b2: e.matmul(C.ps[0:96, b2, :], C.ones_bf[0:96, 0:96], sq[0:96, :], start=True, stop=True),
                 reads=[("msq",), ("ones",)], writes=[("ps", b2)])
            P.op("act", lambda e, b2=b2: e.activation(rstd[0:96, :], C.ps[0:96, b2, :], AF.Sqrt, bias=epsc[0:96, :], scale=1.0 / 96),
                 reads=[("ps", b2)], writes=[("mrstd",)])
            P.op("dve", lambda e: e.reciprocal(rstd[0:96, :], rstd[0:96, :]), reads=[("mrstd",)], writes=[("mrstd",)])
            P.op("dve", lambda e, b=b: e.scalar_tensor_tensor(t1[0:96, :], C.ps[0:96, b, :], gq[0:96, :], C96[0:96, :], ALU.mult, ALU.mult),
                 reads=[("ps", b), ("C96",)], writes=[("t1",)])
            P.op("dve", lambda e, bp=bp: e.scalar_tensor_tensor(t2[R, :], C.ps[R, bp, :], gqp[R, :], S96[R, :], ALU.mult, ALU.mult),
                 reads=[("ps", bp), ("S96",)], writes=[("t2",)])
            P.op("dve", lambda e: e.tensor_tensor(t1[R, :], t1[R, :], t2[R, :], ALU.add), reads=[("t1",), ("t2",)], writes=[("t1",)])
            qk = qi[0]
            qi[0] = (qi[0] + 1) % 2
            Q = qT[qk]
            P.op("dve", lambda e, Q=Q: e.tensor_tensor(Q[0:96, :], t1[0:96, :], rstd[0:96, :], ALU.mult), reads=[("t1",), ("mrstd",)], writes=[("qT", qk)])
            nkb = 4 * j + 4
            po = slice(hi * 64, hi * 64 + 64)
            kw = {"tile_position": (0, 64)} if hi == 1 else {}
            for kb in range(nkb):
                b = C.bank()
                P.op("pe", lambda e, b=b, h=h, kb=kb, Q=Q: e.matmul(C.ps[:, b, :], kT[0:96, h, kb * 128:(kb + 1) * 128], Q[0:96, :], start=True, stop=True),
                     reads=[("kT", h, kb // 4), ("qT", qk)], writes=[("ps", b)])
                pk = pbi[0]
                pbi[0] = (pbi[0] + 1) % 3
                PB = Pb[pk]
                P.op("act", lambda e, b=b, PB=PB: e.activation(PB, C.ps[:, b, :], AF.Exp, scale=SC), reads=[("ps", b)], writes=[("Pb", pk)])
                if kb >= 4 * j:
                    o = (kb - 4 * j) * 128
                    m0 = 512 + 384 - o
                    P.op("pool", lambda e, PB=PB, m0=m0: e.tensor_tensor(PB, PB, C.maskb[:, m0:m0 + 512], ALU.mult),
                         reads=[("Pb", pk), ("mask",)], writes=[("Pb", pk)])

                def f_pv(eng, h=h, kb=kb, PB=PB, po=po, kw=kw, nkb=nkb):
                    eng.matmul(C.ps[po, 0, :], V_tok[:, kb, h * 64:(h + 1) * 64], PB, start=(kb == 0), stop=(kb == nkb - 1), skip_group_check=True, **kw)
                    return eng.matmul(C.ps[po, 1, :], C.ones_bf[:, 0:64], PB, start=(kb == 0), stop=(kb == nkb - 1), skip_group_check=True, **kw)

                P.op("pe", f_pv, reads=[("V_tok", kb), ("Pb", pk), ("ones",)], writes=[("ps", 0), ("ps", 1)])
            if hi == 1:
                P.op("dve", lambda e: e.reciprocal(rden, C.ps[:, 1, :]), reads=[("ps", 1)], writes=[("rden",)])
                P.op("dve", lambda e, h=h: e.tensor_tensor(oT[:, h // 2, :], C.ps[:, 0, :], rden, ALU.mult), reads=[("ps", 0), ("rden",)], writes=[("oT",)])
        for d in range(8):
            b = C.bank()

            def f_mm(eng, d=d, b=b):
                r = None
                for c in range(4):
                    r = eng.matmul(C.ps[:, b, :], woM[:, c, d * 128:(d + 1) * 128], oT[:, c, :], start=(c == 0), stop=(c == 3))
                return r

            P.op("pe", f_mm, reads=[("woM",), ("oT",)], writes=[("ps", b)])
            P.op("dve", lambda e, d=d, b=b, ts=ts: e.tensor_tensor(C.xT[:, d, ts], C.ps[:, b, :], C.xT[:, d, ts], ALU.add),
                 reads=[("ps", b), ("xT", d, j)], writes=[("xT", d, j)])


def load_x(P, C, xin):
    v = xin.rearrange("(k p) t -> p k t", p=128)
    for k in range(8):
        def f(eng, k=k):
            return eng.dma_start(out=C.xT[:, k, :], in_=v[:, k, :])

        P.dma("sp", f, f"xin{k}", writes=[("xT", k, j) for j in range(4)])


def store_x(P, C, yout):
    v = yout.rearrange("(k p) t -> p k t", p=128)
    for k in range(8):
        def f(eng, k=k):
            return eng.dma_start(out=v[:, k, :], in_=C.xT[:, k, :])

        P.dma("sp", f, f"xout{k}", reads=[("xT", k, j) for j in range(4)])


def build(nseq, stages, ccols, ncc):
    nc = bass.Bass("TRN2", target_bir_lowering=False)
    dr = {}
    hostprep = {}

    def din(name, shape, dt=F32):
        dr[name] = nc.dram_tensor(name, list(shape), dt, kind="ExternalInput").ap()
        return dr[name]

    def need(name, shape, fn, dt=F32):
        if name not in dr:
            din(name, shape, dt)
            hostprep[name] = fn
        return dr[name]

    xin = din("xT_in", [nseq * D, T])
    din("consts", [128, ncc])
    din("maskc", [128, NMASK])
    din("maskb", [128, NMASKB])
    yout = nc.dram_tensor("yT_out", [nseq * D, T], F32, kind="ExternalOutput").ap()

    P = Prog()
    C = Ctx()
    C.need = need
    C.ccols = ccols
    with ExitStack() as es:
        def sb(name, shape, dt):
            return es.enter_context(nc.sbuf_tensor(name, list(shape), dt))

        C.xT = sb("xT", [128, 8, T], F32)
        C.hT = sb("hT", [128, 8, T], BF16)
        C.consts = sb("consts_sb", [128, ncc], F32)
        C.ones_bf = sb("ones_bf", [128, 128], BF16)
        C.ar = sb("arena", [128, ARENA // 2], BF16)
        C.ar_f = C.ar.bitcast(F32)
        C.ar_i = C.ar.bitcast(I32)
        C.mask = sb("maskc_sb", [128, NMASK], F32)
        C.ident_bf = sb("ident_bf", [128, 128], BF16)
        C.maskb = sb("maskb_sb", [128, NMASKB], BF16)
        C.bd_bf = sb("bd_bf", [128, 128], BF16)
        C.pos = din("pos", [nseq, T], I32)
        C.posT = din("posT", [nseq, 128, 16], I32)
        C.dr = dr
        ffn_alloc(C)
        C.ps = es.enter_context(nc.psum_tensor("ps", [128, 8, 512], F32))
        C.wslot = 0
        C.eps_col = ccols['eps'][0]
        C.sgslot = 0
        C.nbank = 0
        C.pmi = 0

        def bank():
            b = 2 + C.nbank
            C.nbank = (C.nbank + 1) % 6
            return b

        C.bank = bank

        P.dma("sp", lambda eng: eng.dma_start(out=C.consts[:, :], in_=dr["consts"]), "consts", writes=[("consts",)])
        P.dma("sp", lambda eng: eng.dma_start(out=C.mask[:, :], in_=dr["maskc"]), "maskc", writes=[("mask",)])
        P.dma("pool", lambda eng: eng.dma_start(out=C.maskb[:, :], in_=dr["maskb"]), "maskb", writes=[("mask",)])
        P.op("dve", lambda eng: eng.memset(C.ones_bf[:, :], 1.0), writes=[("ones",)])
        P.op("dve", lambda eng: eng.tensor_copy(C.bd_bf[:, :], C.mask[:, 256:384]), reads=[("mask",)], writes=[("bd",)])
        P.op("dve", lambda eng: eng.tensor_copy(C.ident_bf[:, :], C.mask[:, 512:640]), reads=[("mask",)], writes=[("ident",)])
        P.barrier()

        for s in range(nseq):
            load_x(P, C, xin[s * D:(s + 1) * D, :])
            for st in stages:
                kind = st[0]
                if kind == "ffn":
                    _, nm, l = st
                    wg = need(f"{nm}_w_gate{l}", [D, DFF], lambda inp, nm=nm, l=l: inp[f"{nm}_w_gate"][l])
                    wu = need(f"{nm}_w_up{l}", [D, DFF], lambda inp, nm=nm, l=l: inp[f"{nm}_w_up"][l])
                    wd = need(f"{nm}_w_down{l}", [DFF, D], lambda inp, nm=nm, l=l: inp[f"{nm}_w_down"][l])
                    ffn_stage(P, C, ccols[f"{nm}_norm{l}"][0], wg, wu, wd)
                elif kind == "norm":
                    rmsnorm_stage(P, C, ccols[f"mix_norm{st[1]}"][0])
                elif kind == "swa":
                    swa_stage(P, C, s)
                elif kind == "gla":
                    gla_stage(P, C, s)
                elif kind == "ssd":
                    ssd_stage(P, C, s)
                elif kind == "mla":
                    mla_kv_stage(P, C, s)
                    P.barrier()
                    if os.environ.get("DBG_DUMP"):
                        dk = nc.dram_tensor("dbg_k", [128, 8 * T], BF16, kind="ExternalOutput").ap()
                        dv = nc.dram_tensor("dbg_v", [128, 16 * 512], BF16, kind="ExternalOutput").ap()
                        P.dma("sp", lambda e: [e.dma_start(out=dk[:, i * 1024:(i + 1) * 1024], in_=C.ar[:, i * 1024:(i + 1) * 1024]) for i in range(16)], "dbgk", n=16)
                        P.dma("sp", lambda e: [e.dma_start(out=dv[:, i * 1024:(i + 1) * 1024], in_=C.ar[:, 8 * T + i * 1024:8 * T + (i + 1) * 1024]) for i in range(8)], "dbgv", n=8)
                        P.barrier()
                    else:
                        mla_attn_stage(P, C, s)
                else:
                    raise ValueError(kind)
                P.barrier()
            store_x(P, C, yout[s * D:(s + 1) * D, :])
        P.emit(nc)
    return nc, hostprep


ALL_STAGES = [("ffn", "pre", 0), ("norm", 0), ("ssd", 0), ("mla", 0), ("ffn", "post", 0),
              ("ffn", "pre", 1), ("norm", 1), ("swa", 1), ("gla", 1), ("ffn", "post", 1)]


def run(inputs, stages=ALL_STAGES, ncores=8, nseq=4, trace=False):
    x = np.asarray(inputs["x"], np.float32)
    pos = np.asarray(inputs["positions"], np.int32)
    consts, ccols = pack_consts(inputs)
    nc, hostprep = build(nseq, stages, ccols, consts.shape[1])
    mk = make_masks()
    shared = {"consts": consts, "maskc": mk[0], "maskb": mk[1]}
    for name, fn in hostprep.items():
        shared[name] = np.ascontiguousarray(np.asarray(fn(inputs), np.float32))
    in_maps = []
    for c in range(ncores):
        xs = x[c * nseq:(c + 1) * nseq]
        xT = np.ascontiguousarray(xs.transpose(0, 2, 1)).reshape(nseq * D, T)
        ps = np.ascontiguousarray(pos[c * nseq:(c + 1) * nseq])
        pT = np.ascontiguousarray(ps.reshape(nseq, 16, 128).transpose(0, 2, 1))
        m = {"xT_in": xT, "pos": ps, "posT": pT}
        m.update(shared)
        in_maps.append(m)
    res = run_bass_kernel_spmd(nc, in_maps, core_ids=list(range(ncores)), trace=trace)
    outs = []
    for c in range(ncores):
        yT = np.asarray(res.results[c]["yT_out"]).reshape(nseq, D, T)
        outs.append(yT.transpose(0, 2, 1))
    out = np.ascontiguousarray(np.concatenate(outs, axis=0)).astype(np.float32)
    return out, res


def kernel(**inputs):
    out, _ = run(inputs)
    return out
```

```python
import numpy as np
from contextlib import ExitStack
import concourse.bass as bass
import concourse.mybir as mybir
from concourse.bass_utils import run_bass_kernel_spmd

F32 = mybir.dt.float32
BF16 = mybir.dt.bfloat16
I32 = mybir.dt.int32
AF = mybir.ActivationFunctionType
ALU = mybir.AluOpType

D = 1024
T = 2048
DFF = 2816
NCH = DFF // 128
EPS = 1e-6
ENGS = ("pe", "act", "dve", "pool", "sp")
import os
SERIAL = bool(os.environ.get('DBG_SERIAL'))


class Prog:
    def __init__(self):
        self.ops = {e: [] for e in ENGS}
        self.cnt = {e: 0 for e in ENGS}
        self.waited = {e: {} for e in ENGS}
        self.last_w = {}
        self.readers = {}
        self.dcnt = {}

    def _deps(self, eng, reads, writes):
        deps = {}

        def add(s, v):
            if deps.get(s, 0) < v:
                deps[s] = v

        for k in reads:
            if k in self.last_w:
                add(*self.last_w[k])
        for k in writes:
            if k in self.last_w:
                add(*self.last_w[k])
            for s, v in self.readers.get(k, {}).items():
                add(s, v)
        waits = []
        for s, v in deps.items():
            if s == "e:pe" and eng == "pe":
                continue
            if self.waited[eng].get(s, 0) < v:
                self.waited[eng][s] = v
                waits.append((s, v))
        return waits

    def _commit(self, tok, reads, writes):
        for k in writes:
            self.last_w[k] = tok
            self.readers[k] = {}
        for k in reads:
            r = self.readers.setdefault(k, {})
            if r.get(tok[0], 0) < tok[1]:
                r[tok[0]] = tok[1]

    def op(self, eng, fn, reads=(), writes=()):
        waits = self._deps(eng, reads, writes)
        self.cnt[eng] += 1
        tok = ("e:" + eng, self.cnt[eng])
        self.ops[eng].append((waits, fn, tok[0], 1))
        self._commit(tok, reads, writes)
        if SERIAL:
            self.barrier()

    def dma(self, eng, fn, key, reads=(), writes=(), n=1):
        waits = self._deps(eng, reads, writes)
        s = "d:" + key
        self.dcnt[s] = self.dcnt.get(s, 0) + 16 * n
        tok = (s, self.dcnt[s])
        self.ops[eng].append((waits, fn, s, 16))
        self._commit(tok, reads, writes)
        if SERIAL:
            self.barrier()

    def barrier(self):
        for e in ENGS:
            for e2 in ENGS:
                if e2 == "sp" or self.cnt[e2] == 0:
                    continue
                s = "e:" + e2
                v = self.cnt[e2]
                if self.waited[e].get(s, 0) < v:
                    self.waited[e][s] = v
                    self.ops[e].append(([(s, v)], None, None, 0))
            for s, v in self.dcnt.items():
                if self.waited[e].get(s, 0) < v:
                    self.waited[e][s] = v
                    self.ops[e].append(([(s, v)], None, None, 0))

    def emit(self, nc):
        with ExitStack() as es:
            sems = {}
            names = set()
            for e in ENGS:
                for waits, fn, s, inc in self.ops[e]:
                    if s is not None:
                        names.add(s)
                    for w in waits:
                        names.add(w[0])
            for s in sorted(names):
                sems[s] = es.enter_context(nc.semaphore(s.replace(":", "_")))
            block = es.enter_context(nc.Block())

            def run(e):
                def body(eng):
                    for waits, fn, s, inc in self.ops[e]:
                        for ws, wv in waits:
                            eng.wait_ge(sems[ws], wv)
                        if fn is None:
                            continue
                        r = fn(eng)
                        if isinstance(r, (list, tuple)):
                            for ins in r:
                                ins.then_inc(sems[s], inc)
                        else:
                            r.then_inc(sems[s], inc)
                    if e == "sp":
                        for s, v in self.dcnt.items():
                            eng.wait_ge(sems[s], v)

                return body

            block.tensor(run("pe"))
            block.scalar(run("act"))
            block.vector(run("dve"))
            block.gpsimd(run("pool"))
            block.sync(run("sp"))


class Ctx:
    pass


def tsl(j, n=512):
    return slice(j * n, (j + 1) * n)


def fm(v):
    v = np.asarray(v, np.float32)
    return np.ascontiguousarray(v.reshape(-1, 128).T)


def make_masks():
    p = np.arange(128)[:, None]
    f = np.arange(128)[None, :]
    mc = (f >= p)
    mp = (f < p)
    bd = (p // 64 == f // 64)
    gm = bd & (p <= f)
    m64 = np.broadcast_to((np.arange(512)[None, :] % 64 != 0), (128, 512))
    ident = (p == f)
    neg = -30000.0 * mp
    fw = np.arange(896)[None, :]
    mcw = (fw - 384 >= p)
    return (np.ascontiguousarray(np.concatenate([mc, mp, bd, gm, ident, neg], axis=1).astype(np.float32)),
            np.ascontiguousarray(np.concatenate([m64, mcw], axis=1).astype(np.float32)))


NMASK = 768
NMASKB = 512 + 896


def pack_consts(inp):
    cols = {}
    parts = []
    off = 0

    def put(name, arr):
        nonlocal off
        arr = np.asarray(arr, np.float32)
        assert arr.shape[0] == 128
        cols[name] = (off, arr.shape[1])
        parts.append(arr)
        off += arr.shape[1]

    for l in range(2):
        put(f"pre_norm{l}", fm(inp["pre_norm"][l]))
        put(f"mix_norm{l}", fm(inp["mix_norm"][l]))
        put(f"post_norm{l}", fm(inp["post_norm"][l]))
    put("eps", np.full((128, 1), EPS, np.float32))
    put("one", np.ones((128, 1), np.float32))
    put("o_qg2", np.tile(np.asarray(inp["o_q_norm"][0], np.float32), 2)[:, None])
    put("o_kg2", np.tile(np.asarray(inp["o_k_norm"][0], np.float32), 2)[:, None])
    cw = np.asarray(inp["e_conv_w"][0], np.float32)
    put("e_convw", np.ascontiguousarray(cw.T.reshape(16, 128, 4).transpose(1, 0, 2).reshape(128, 64)))
    put("e_convb", fm(inp["e_conv_b"][0]))
    put("e_dtb", np.broadcast_to(np.asarray(inp["e_dt_bias"][0], np.float32)[None, :], (128, 16)))
    put("e_alog", np.broadcast_to(np.asarray(inp["e_a_log"][0], np.float32)[None, :], (128, 16)))
    put("e_dskip", fm(np.repeat(np.asarray(inp["e_d_skip"][0], np.float32), 64)))
    put("e_ssmn", fm(inp["e_ssm_norm"][0]))
    put("e_qan", fm(inp["e_q_a_norm"][0]))
    put("e_kvan", fm(inp["e_kv_a_norm"][0]))
    for nm_, key_ in (("gq", "e_q_norm"), ("gk", "e_k_norm")):
        g96 = np.asarray(inp[key_][0], np.float32)
        col = np.zeros((128, 1), np.float32)
        col[0:96, 0] = g96
        put(nm_, col)
        colp = np.zeros((128, 1), np.float32)
        colp[64:80, 0] = g96[80:96]
        colp[80:96, 0] = g96[64:80]
        put(nm_ + "p", colp)
    invf = np.zeros((128, 1), np.float32)
    fr = (10000.0 ** (-np.arange(16, dtype=np.float64) / 16.0) / (2 * np.pi)).astype(np.float32)
    invf[64:80, 0] = fr
    invf[80:96, 0] = fr
    put("invf", invf)
    sgn = np.zeros((128, 1), np.float32)
    sgn[64:80, 0] = -1.0
    sgn[80:96, 0] = 1.0
    put("sgn", sgn)
    put("o_gbias", fm(inp["o_gate_bias"][0]))
    put("o_glan", np.asarray(inp["o_gla_norm"][0], np.float32)[:, None])
    put("sinks", np.broadcast_to(np.asarray(inp["o_sinks"][0], np.float32)[None, :], (128, 8)))
    put("SL", np.broadcast_to((-8.0 * 2.0 ** (-np.arange(1, 9, dtype=np.float64))).astype(np.float32)[None, :], (128, 8)))
    return np.ascontiguousarray(np.concatenate(parts, axis=1)), cols


def rmsnorm_stage(P, C, gcol):
    for j in range(4):
        ts = tsl(j)
        sq = C.sq

        def f_sq(eng, ts=ts):
            return eng.tensor_tensor(sq[:, :, :], C.xT[:, :, ts], C.xT[:, :, ts], ALU.mult)

        P.op("pool", f_sq, reads=[("xT", d, j) for d in range(8)], writes=[("sq",)])
        bank = C.bank()

        def f_mm(eng, bank=bank):
            r = None
            for k in range(8):
                r = eng.matmul(C.ps[:, bank, :], C.ones_bf[:, :], sq[:, k, :], start=(k == 0), stop=(k == 7))
            return r

        P.op("pe", f_mm, reads=[("sq",)], writes=[("ps", bank)])

        def f_r1(eng, bank=bank):
            return eng.activation(C.rstd[:, :], C.ps[:, bank, :], AF.Sqrt, bias=C.consts[:, C.eps_col:C.eps_col + 1],
                                  scale=1.0 / D)

        P.op("act", f_r1, reads=[("ps", bank)], writes=[("rstd",)])

        def f_r2(eng):
            return eng.reciprocal(C.rstd[:, :], C.rstd[:, :])

        P.op("dve", f_r2, reads=[("rstd",)], writes=[("rstd",)])
        for k in range(8):
            def f_h(eng, k=k, ts=ts):
                return eng.scalar_tensor_tensor(
                    C.hT[:, k, ts], C.xT[:, k, ts], C.consts[:, gcol + k:gcol + k + 1], C.rstd[:, :],
                    ALU.mult, ALU.mult)

            P.op("dve", f_h, reads=[("xT", k, j), ("rstd",)], writes=[("hT", k, j)])


FFN_GROUPS = [(0, 8), (8, 7), (15, 7)]
ARENA = 102 * 1024
import os
DBG_PHASE = int(os.environ.get('DBG_PHASE', '3'))
DBG_NB = int(os.environ.get('DBG_NB', '0'))
DBG_STEP = int(os.environ.get('DBG_STEP', '9'))


class Arena:
    def __init__(self, C):
        self.C = C
        self.off = 0

    def _take(self, n, esz):
        self.off = (self.off + 3) // 4 * 4
        o = self.off
        self.off += n * esz
        assert self.off <= ARENA, self.off
        return o

    def bf(self, *free):
        n = int(np.prod(free))
        o = self._take(n, 2)
        return self._shape(self.C.ar[:, o // 2:o // 2 + n], free)

    def f32(self, *free):
        n = int(np.prod(free))
        o = self._take(n, 4)
        return self._shape(self.C.ar_f[:, o // 4:o // 4 + n], free)

    def i32(self, *free):
        n = int(np.prod(free))
        o = self._take(n, 4)
        return self._shape(self.C.ar_i[:, o // 4:o // 4 + n], free)

    @staticmethod
    def _shape(v, free):
        if len(free) == 1:
            return v
        if len(free) == 2:
            return v.rearrange("p (a b) -> p a b", a=free[0])
        if len(free) == 3:
            return v.rearrange("p (a b c) -> p a b c", a=free[0], b=free[1])
        raise ValueError


def bc(ap, dims):
    return bass.AP(ap.tensor, ap.offset, [list(ap.ap[0])] + [list(d) for d in dims])


def ffn_alloc(C):
    A = Arena(C)
    C.aT = A.bf(8, T)
    C.wgu = [[A.bf(8, 512) for i in range(2)] for s in range(2)]
    C.wd_sb = A.bf(8, D)
    C.sg = [A.f32(512) for s in range(2)]
    C.sq = A.bf(8, 512)
    C.rstd = A.f32(512)


def ffn_stage(P, C, gcol, wg, wu, wd):
    rmsnorm_stage(P, C, gcol)
    wg_v = wg.rearrange("(k p) f -> p k f", p=128)
    wu_v = wu.rearrange("(k p) f -> p k f", p=128)
    wd_v = wd.rearrange("(c p) d -> p c d", p=128)
    for (c0, ng) in FFN_GROUPS:
        def f_wd(eng, c0=c0, ng=ng):
            return eng.dma_start(out=C.wd_sb[:, 0:ng, :], in_=wd_v[:, c0:c0 + ng, :])

        P.dma("pool", f_wd, "wd", writes=[("wd",)])
        pieces = []
        cc = 0
        while cc < ng:
            pn = min(4, ng - cc)
            pieces.append((cc, pn))
            cc += pn
        for (pc, pn) in pieces:
            slot = C.wslot
            C.wslot = (C.wslot + 1) % 2
            f0 = (c0 + pc) * 128

            def f_wg(eng, slot=slot, f0=f0, pn=pn):
                return eng.dma_start(out=C.wgu[slot][0][:, :, 0:pn * 128], in_=wg_v[:, :, f0:f0 + pn * 128])

            def f_wu(eng, slot=slot, f0=f0, pn=pn):
                return eng.dma_start(out=C.wgu[slot][1][:, :, 0:pn * 128], in_=wu_v[:, :, f0:f0 + pn * 128])

            P.dma("pool", f_wg, f"wg{slot}", writes=[("wg", slot)])
            P.dma("pool", f_wu, f"wu{slot}", writes=[("wu", slot)])
            for ci in range(pn):
                ca = pc + ci
                for j in range(4):
                    ts = tsl(j)
                    bg = C.bank()
                    bu = C.bank()

                    def f_mm(eng, slot=slot, ci=ci, ts=ts, bg=bg, bu=bu):
                        r = None
                        for k in range(8):
                            r = eng.matmul(C.ps[:, bg, :], C.wgu[slot][0][:, k, ci * 128:(ci + 1) * 128],
                                           C.hT[:, k, ts], start=(k == 0), stop=(k == 7))
                        for k in range(8):
                            r = eng.matmul(C.ps[:, bu, :], C.wgu[slot][1][:, k, ci * 128:(ci + 1) * 128],
                                           C.hT[:, k, ts], start=(k == 0), stop=(k == 7))
                        return r

                    P.op("pe", f_mm, reads=[("wg", slot), ("wu", slot)] + [("hT", k, j) for k in range(8)],
                         writes=[("ps", bg), ("ps", bu)])
                    ss = C.sgslot
                    C.sgslot = (C.sgslot + 1) % 2

                    def f_silu(eng, ss=ss, bg=bg):
                        return eng.activation(C.sg[ss][:, :], C.ps[:, bg, :], AF.Silu)

                    P.op("act", f_silu, reads=[("ps", bg)], writes=[("sg", ss)])

                    def f_mul(eng, ss=ss, bu=bu, ca=ca, ts=ts):
                        return eng.tensor_tensor(C.aT[:, ca, ts], C.sg[ss][:, :], C.ps[:, bu, :], ALU.mult)

                    P.op("dve", f_mul, reads=[("sg", ss), ("ps", bu)], writes=[("aT", ca, j)])
        for d in range(8):
            for j in range(4):
                ts = tsl(j)
                by = C.bank()

                def f_mm(eng, d=d, ts=ts, by=by, ng=ng):
                    r = None
                    for ca in range(ng):
                        r = eng.matmul(C.ps[:, by, :], C.wd_sb[:, ca, d * 128:(d + 1) * 128], C.aT[:, ca, ts],
                                       start=(ca == 0), stop=(ca == ng - 1))
                    return r

                P.op("pe", f_mm, reads=[("wd",)] + [("aT", ca, j) for ca in range(ng)], writes=[("ps", by)])

                def f_res(eng, d=d, ts=ts, by=by):
                    return eng.scalar_tensor_tensor(C.xT[:, d, ts], C.ps[:, by, :], 0.5, C.xT[:, d, ts],
                                                    ALU.mult, ALU.add)

                P.op("dve", f_res, reads=[("ps", by), ("xT", d, j)], writes=[("xT", d, j)])


def norm_heads64(P, C, bank, gcol_name, out_ap, sq, rstd):
    gc = C.ccols[gcol_name][0]

    def f_sq(eng):
        return eng.activation(sq, C.ps[:, bank, :], AF.Square)

    P.op("act", f_sq, reads=[("ps", bank)], writes=[("nsq",)])
    b2 = C.bank()

    def f_mm(eng):
        return eng.matmul(C.ps[:, b2, :], C.bd_bf[:, :], sq, start=True, stop=True)

    P.op("pe", f_mm, reads=[("nsq",), ("bd",)], writes=[("ps", b2)])

    def f_r1(eng):
        return eng.activation(rstd, C.ps[:, b2, :], AF.Sqrt, bias=C.consts[:, C.eps_col:C.eps_col + 1], scale=1.0 / 64)

    P.op("act", f_r1, reads=[("ps", b2)], writes=[("nrstd",)])

    def f_r2(eng):
        return eng.reciprocal(rstd, rstd)

    P.op("dve", f_r2, reads=[("nrstd",)], writes=[("nrstd",)])

    def f_o(eng):
        return eng.scalar_tensor_tensor(out_ap, C.ps[:, bank, :], C.consts[:, gc:gc + 1], rstd, ALU.mult, ALU.mult)

    return f_o


def swa_stage(P, C, s):
    dr = C.dr
    w_in = C.need("o_w_in", [D, 2320], lambda inp: inp["o_w_in"][0])
    w_out = C.need("o_w_out", [D, D], lambda inp: inp["o_w_out"][0])
    A = Arena(C)
    wqkv = A.bf(8, 768)
    wk2 = A.bf(8, 2, 128)
    woS = A.bf(8, D)
    qT = A.bf(4, 512)
    kT2 = A.bf(2, T)
    Vaug = A.bf(16, 2, 128)
    oT = A.bf(8, 512)
    posB = A.f32(T)
    posBi = bass.AP(C.ar_i[:, 0:1].tensor, posB.offset, [list(posB.ap[0]), [1, T]])
    pk = A.f32(16)
    pki = A.i32(16)
    dist = A.f32(128)
    D8 = A.f32(8, 128)
    tmp = A.f32(512)
    Pe = A.bf(512)
    Pm = [A.bf(512) for _ in range(4)]
    sq = A.bf(512)
    rstd = A.f32(512)
    dn = A.f32(512)
    dn0 = A.f32(512)
    es = A.f32(8)
    cc = C.ccols
    w_in_v = w_in.rearrange("(k p) f -> p k f", p=128)

    P.dma("pool", lambda e: e.dma_start(out=wqkv, in_=w_in_v[:, :, 0:768]), "wA", writes=[("wqkv",)])

    def f_wk(e):
        r = []
        for g in range(2):
            for hh in range(2):
                r.append(e.dma_start(out=wk2[:, :, g, hh * 64:(hh + 1) * 64], in_=w_in_v[:, :, 512 + g * 64:512 + (g + 1) * 64]))
        return r

    P.dma("pool", f_wk, "wB", writes=[("wk2",)], n=4)
    P.dma("pool", lambda e: e.dma_start(out=woS[0:64, :, :], in_=w_out[0:512, :].rearrange("(h p) d -> p h d", p=64)),
          "wC", writes=[("woS",)])
    P.dma("sp", lambda e: e.dma_start(out=posBi, in_=bass.AP(C.pos.tensor, C.pos[s:s + 1, :].offset, [[0, 128], [1, T]])),
          "posB", writes=[("posB",)])
    P.dma("sp", lambda e: e.dma_start(out=pki, in_=C.posT[s]), "pk", writes=[("pki",)])
    P.op("dve", lambda e: e.tensor_copy(posB, posBi), reads=[("posB",)], writes=[("posB",)])
    P.op("dve", lambda e: e.tensor_copy(pk, pki), reads=[("pki",)], writes=[("pk",)])
    P.op("act", lambda e: e.activation(es, C.consts[:, cc["sinks"][0]:cc["sinks"][0] + 8], AF.Exp), reads=[("consts",)], writes=[("es",)])
    P.op("pool", lambda e: e.memset(Vaug[:, :, :, 64:128], 1.0), writes=[("Vaug", n) for n in range(16)])
    SLc = cc["SL"][0]

    for j in range(4):
        ts = tsl(j)
        for c in range(4):
            b = C.bank()

            def f_mm(eng, c=c, b=b, ts=ts):
                r = None
                for k in range(8):
                    r = eng.matmul(C.ps[:, b, :], wqkv[:, k, c * 128:(c + 1) * 128], C.hT[:, k, ts], start=(k == 0), stop=(k == 7))
                return r

            P.op("pe", f_mm, reads=[("wqkv",)] + [("hT", k, j) for k in range(8)], writes=[("ps", b)])
            f_o = norm_heads64(P, C, b, "o_qg2", qT[:, c, :], sq, rstd)
            P.op("dve", f_o, reads=[("ps", b), ("nrstd",)], writes=[("qT", c)])
        for g in range(2):
            b = C.bank()

            def f_mm(eng, g=g, b=b, ts=ts):
                r = None
                for k in range(8):
                    r = eng.matmul(C.ps[:, b, :], wk2[:, k, g, :], C.hT[:, k, ts], start=(k == 0), stop=(k == 7))
                return r

            P.op("pe", f_mm, reads=[("wk2",)] + [("hT", k, j) for k in range(8)], writes=[("ps", b)])
            f_o = norm_heads64(P, C, b, "o_kg2", kT2[:, g, ts], sq, rstd)
            P.op("dve", f_o, reads=[("ps", b), ("nrstd",)], writes=[("kT2", g, j)])
        for nl in range(4):
            n = 4 * j + nl
            b = C.bank()

            def f_mm(eng, n=n, b=b):
                r = None
                for k in range(8):
                    r = eng.matmul(C.ps[:, b, 0:128], C.hT[:, k, n * 128:(n + 1) * 128], wqkv[:, k, 640:768], start=(k == 0), stop=(k == 7))
                return r

            P.op("pe", f_mm, reads=[("wqkv",)] + [("hT", k, j) for k in range(8)], writes=[("ps", b)])

            def f_v(eng, n=n, b=b):
                return eng.tensor_copy(Vaug[:, n, :, 0:64], C.ps[:, b, 0:128].rearrange("p (g d) -> p g d", g=2))

            P.op("dve", f_v, reads=[("ps", b)], writes=[("Vaug", n)])
        for nl in range((4 if DBG_NB <= 0 else (DBG_NB if j == 0 else 0)) if DBG_PHASE >= 2 else 0):
            n = 4 * j + nl
            qs = slice(nl * 128, (nl + 1) * 128)
            kbs = [n - 1, n] if n > 0 else [n]
            for kb in kbs:
                def f_dist(eng, n=n, kb=kb):
                    return eng.tensor_scalar(dist, posB[:, n * 128:(n + 1) * 128], pk[:, kb:kb + 1], None, ALU.subtract)

                P.op("dve", f_dist, reads=[("posB",), ("pk",)], writes=[("dist",)])
                P.op("dve", lambda e: e.scalar_tensor_tensor(dist, dist, -1.0, dist, ALU.mult, ALU.max), reads=[("dist",)], writes=[("dist",)])

                def f_d8(eng):
                    return eng.tensor_tensor(D8, bc(dist, [[0, 8], [1, 128]]),
                                             bc(C.consts[:, SLc:SLc + 8], [[1, 8], [0, 128]]), ALU.mult)

                P.op("pool", f_d8, reads=[("dist",), ("consts",)], writes=[("D8",)])
                for g in range(2 if DBG_STEP >= 2 else 0):
                    bA = C.bank()
                    bB = C.bank()

                    def f_sc(eng, g=g, bA=bA, bB=bB, kb=kb, qs=qs):
                        r = None
                        for hl in range(4):
                            c = 2 * g + hl // 2
                            hp = slice((hl % 2) * 64, (hl % 2) * 64 + 64)
                            bb = bA if hl % 2 == 0 else bB
                            r = eng.matmul(C.ps[:, bb, (hl // 2) * 128:(hl // 2 + 1) * 128], kT2[hp, g, kb * 128:(kb + 1) * 128],
                                           qT[hp, c, qs], start=True, stop=True)
                        return r

                    P.op("pe", f_sc, reads=[("kT2", g, kb // 4), ("qT", 2 * g), ("qT", 2 * g + 1)], writes=[("ps", bA), ("ps", bB)])
                    if DBG_STEP < 3:
                        continue
                    for par, bb in ((0, bA), (1, bB)):
                        def f_t(eng, g=g, bb=bb, par=par):
                            return eng.tensor_tensor(tmp.rearrange("p (h f) -> p h f", h=4)[:, par:4:2, :],
                                                     D8[:, 4 * g + par:4 * g + 4:2, :],
                                                     C.ps[:, bb, 0:256].rearrange("p (h f) -> p h f", h=2), ALU.add)

                        P.op("dve", f_t, reads=[("D8",), ("ps", bb)], writes=[("tmp",)])
                    P.op("act", lambda e: e.activation(Pe, tmp, AF.Exp, scale=0.125), reads=[("tmp",)], writes=[("Pe",)])
                    pi = C.pmi
                    C.pmi = (C.pmi + 1) % 4
                    mcol = 0 if kb == n else 128

                    def f_m(eng, pi=pi, mcol=mcol):
                        return eng.tensor_tensor(Pm[pi].rearrange("p (h f) -> p h f", h=4), Pe.rearrange("p (h f) -> p h f", h=4),
                                                 bc(C.mask[:, mcol:mcol + 128], [[0, 4], [1, 128]]), ALU.mult)

                    P.op("pool", f_m, reads=[("Pe",), ("mask",)], writes=[("Pm", pi)])
                    if DBG_STEP < 4:
                        continue

                    def f_pv(eng, g=g, kb=kb, pi=pi, first=(kb == kbs[0]), last=(kb == n)):
                        return eng.matmul(C.ps[:, g, :], Vaug[:, kb, g, :], Pm[pi], start=first, stop=last)

                    P.op("pe", f_pv, reads=[("Vaug", kb), ("Pm", pi)], writes=[("ps", g)])
            for g in range(2 if DBG_STEP >= 5 else 0):
                def f_dn(eng, g=g):
                    return eng.tensor_tensor(dn[64:128, :].rearrange("p (h f) -> p h f", h=4),
                                             C.ps[64:128, g, :].rearrange("p (h f) -> p h f", h=4),
                                             bc(es[64:128, 4 * g:4 * g + 4], [[1, 4], [0, 128]]), ALU.add)

                P.op("dve", f_dn, reads=[("ps", g), ("es",)], writes=[("dn",)])
                P.op("dve", lambda e: e.reciprocal(dn[64:128, :], dn[64:128, :]), reads=[("dn",)], writes=[("dn",)])
                P.op("dve", lambda e: e.tensor_copy(dn0[0:64, :], dn[64:128, :]), reads=[("dn",)], writes=[("dn0",)])

                def f_o(eng, g=g, qs=qs):
                    return eng.tensor_tensor(oT[0:64, 4 * g:4 * g + 4, qs], C.ps[0:64, g, :].rearrange("p (h f) -> p h f", h=4),
                                             dn0[0:64, :].rearrange("p (h f) -> p h f", h=4), ALU.mult)

                P.op("dve", f_o, reads=[("ps", g), ("dn0",)], writes=[("oT",)])
        for d in range(8 if DBG_PHASE >= 3 else 0):
            b = C.bank()

            def f_mm(eng, d=d, b=b):
                r = None
                for h in range(8):
                    r = eng.matmul(C.ps[:, b, :], woS[0:64, h, d * 128:(d + 1) * 128], oT[0:64, h, :], start=(h == 0), stop=(h == 7))
                return r

            P.op("pe", f_mm, reads=[("woS",), ("oT",)], writes=[("ps", b)])

            def f_res(eng, d=d, b=b, ts=ts):
                return eng.tensor_tensor(C.xT[:, d, ts], C.ps[:, b, :], C.xT[:, d, ts], ALU.add)

            P.op("dve", f_res, reads=[("ps", b), ("xT", d, j)], writes=[("xT", d, j)])


def gla_stage(P, C, s):
    w_in = C.need("o_w_in", [D, 2320], lambda inp: inp["o_w_in"][0])
    w_out = C.need("o_w_out", [D, D], lambda inp: inp["o_w_out"][0])
    w_gb = C.need("o_w_gate_b", [16, 256], lambda inp: inp["o_w_gate_b"][0])
    cc = C.ccols
    A = Arena(C)
    wG = A.bf(8, 1552)
    wgb = A.bf(256)
    woG = A.bf(4, D)
    gaT = A.bf(512)
    ebuf = A.f32(512)
    lbuf = A.f32(512)
    cl = A.f32(2, 512)
    eb = A.f32(512)
    einv = A.f32(512)
    dend = A.f32(512)
    decs = A.f32(2, 8)
    nb = A.f32(2)
    q_dec = A.bf(2, 512)
    k_inv = A.bf(2, 512)
    k_end = A.bf(2, 512)
    grs = A.bf(4, 512)
    gv_tok = A.bf(4, 512)
    ket = A.bf(4, 2, 128)
    attm = A.bf(4, 128)
    S = [A.f32(128) for _ in range(2)]
    S_bf = [A.bf(128) for _ in range(2)]
    sq = A.bf(256)
    rstd = A.f32(256)
    ytmp = A.f32(256)
    oG = A.bf(4, 512)
    w_in_v = w_in.rearrange("(k p) f -> p k f", p=128)
    onec = C.consts[:, cc["one"][0]:cc["one"][0] + 1]
    glan = C.consts[:, cc["o_glan"][0]:cc["o_glan"][0] + 1]
    gbc = cc["o_gbias"][0]

    P.dma("pool", lambda e: e.dma_start(out=wG, in_=w_in_v[:, :, 768:2320]), "wA", writes=[("wG",)])
    P.dma("pool", lambda e: e.dma_start(out=wgb[0:16, :], in_=w_gb), "wB", writes=[("wgb",)])
    P.dma("pool", lambda e: e.dma_start(out=woG, in_=w_out[512:1024, :].rearrange("(c p) d -> p c d", p=128)), "wC", writes=[("woG",)])
    P.op("dve", lambda e: e.tensor_scalar(nb, C.consts[:, gbc:gbc + 2], -1.0, None, ALU.mult), reads=[("consts",)], writes=[("nb",)])
    for cp in range(2):
        P.op("dve", lambda e, cp=cp: e.memset(S[cp], 0.0), writes=[("S", cp)])
        P.op("dve", lambda e, cp=cp: e.memset(S_bf[cp], 0.0), writes=[("Sbf", cp)])

    def proj(cols, j, M=128):
        b = C.bank()
        ts = tsl(j)

        def f(eng):
            r = None
            for k in range(8):
                r = eng.matmul(C.ps[0:M, b, :], wG[:, k, cols], C.hT[:, k, ts], start=(k == 0), stop=(k == 7))
            return r

        P.op("pe", f, reads=[("wG",)] + [("hT", k, j) for k in range(8)], writes=[("ps", b)])
        return b

    for j in range(4):
        ts = tsl(j)
        b = proj(slice(1024, 1040), j, M=16)
        P.op("act", lambda e, b=b: e.copy(gaT[0:16, :], C.ps[0:16, b, :]), reads=[("ps", b)], writes=[("gaT",)])
        for cp in range(2):
            b = C.bank()
            P.op("pe", lambda e, b=b, cp=cp: e.matmul(C.ps[:, b, :], wgb[0:16, cp * 128:(cp + 1) * 128], gaT[0:16, :], start=True, stop=True),
                 reads=[("wgb",), ("gaT",)], writes=[("ps", b)])
            P.op("act", lambda e, b=b, cp=cp: e.activation(ebuf, C.ps[:, b, :], AF.Exp, bias=nb[:, cp:cp + 1], scale=-1.0),
                 reads=[("ps", b), ("nb",)], writes=[("ebuf",)])
            P.op("act", lambda e: e.activation(lbuf, ebuf, AF.Ln, bias=onec, scale=1.0), reads=[("ebuf",)], writes=[("lbuf",)])
            P.op("dve", lambda e, cp=cp: e.tensor_tensor_scan(cl[:, cp, :], C.maskb[:, 0:512], lbuf, 0.0, ALU.mult, ALU.add),
                 reads=[("lbuf",), ("mask",)], writes=[("cl", cp)])
            P.op("act", lambda e, cp=cp: e.activation(eb, cl[:, cp, :], AF.Exp, scale=-1.0 / 16), reads=[("cl", cp)], writes=[("eb",)])
            P.op("act", lambda e, cp=cp: e.activation(einv, cl[:, cp, :], AF.Exp, scale=1.0 / 16), reads=[("cl", cp)], writes=[("einv",)])
            clv = cl[:, cp, :]
            clast_b = bc(clv[:, 63:64], [[64, 8], [0, 64]])
            clast = bc(clv[:, 63:64], [[64, 8]])
            P.op("dve", lambda e, cp=cp, clast_b=clast_b: e.tensor_tensor(dend.rearrange("p (c l) -> p c l", c=8),
                                                                       cl[:, cp, :].rearrange("p (c l) -> p c l", c=8), clast_b, ALU.subtract),
                 reads=[("cl", cp)], writes=[("dend",)])
            P.op("act", lambda e: e.activation(dend, dend, AF.Exp, scale=1.0 / 16), reads=[("dend",)], writes=[("dend",)])
            P.op("act", lambda e, cp=cp, clast=clast: e.activation(decs[:, cp, :], clast, AF.Exp, scale=-1.0 / 16),
                 reads=[("cl", cp)], writes=[("decs", cp)])
            b = proj(slice(cp * 128, (cp + 1) * 128), j)
            P.op("dve", lambda e, b=b, cp=cp: e.scalar_tensor_tensor(q_dec[:, cp, :], C.ps[:, b, :], 0.125, eb, ALU.mult, ALU.mult),
                 reads=[("ps", b), ("eb",)], writes=[("q_dec", cp)])
            b = proj(slice(256 + cp * 128, 256 + (cp + 1) * 128), j)
            P.op("dve", lambda e, b=b, cp=cp: e.tensor_tensor(k_inv[:, cp, :], C.ps[:, b, :], einv, ALU.mult),
                 reads=[("ps", b), ("einv",)], writes=[("k_inv", cp)])
            P.op("dve", lambda e, b=b, cp=cp: e.tensor_tensor(k_end[:, cp, :], C.ps[:, b, :], dend, ALU.mult),
                 reads=[("ps", b), ("dend",)], writes=[("k_end", cp)])
        for hh in range(4):
            b = proj(slice(1040 + hh * 128, 1040 + (hh + 1) * 128), j)
            P.op("act", lambda e, b=b, hh=hh: e.activation(grs[:, hh, :], C.ps[:, b, :], AF.Silu), reads=[("ps", b)], writes=[("grs", hh)])
        for nl in range(4):
            n = 4 * j + nl
            b = C.bank()

            def f_gv(eng, b=b, n=n):
                r = None
                for k in range(8):
                    r = eng.matmul(C.ps[:, b, :], C.hT[:, k, n * 128:(n + 1) * 128], wG[:, k, 512:1024], start=(k == 0), stop=(k == 7))
                return r

            P.op("pe", f_gv, reads=[("wG",)] + [("hT", k, j) for k in range(8)], writes=[("ps", b)])
            P.op("act", lambda e, b=b, nl=nl: e.copy(gv_tok[:, nl, :], C.ps[:, b, :]), reads=[("ps", b)], writes=[("gv", nl)])
            for cp in range(2):
                b = C.bank()
                P.op("pe", lambda e, b=b, cp=cp, nl=nl: e.matmul(C.ps[:, b, 0:128], k_end[:, cp, nl * 128:(nl + 1) * 128], C.ident_bf[:, :], start=True, stop=True),
                     reads=[("k_end", cp), ("ident",)], writes=[("ps", b)])
                P.op("dve", lambda e, b=b, cp=cp, nl=nl: e.tensor_copy(ket[:, nl, cp, :], C.ps[:, b, 0:128]), reads=[("ps", b)], writes=[("ket", nl, cp)])
        for nl in range(4):
            bs = slice(nl * 128, (nl + 1) * 128)
            bX = C.bank()
            bY = C.bank()

            def f_att(eng, bX=bX, bY=bY, bs=bs):
                r = None
                for h in range(4):
                    cp, half = h // 2, h % 2
                    hp = slice(half * 64, half * 64 + 64)
                    bb = bX if half == 0 else bY
                    r = eng.matmul(C.ps[:, bb, cp * 128:(cp + 1) * 128], k_inv[hp, cp, bs], q_dec[hp, cp, bs], start=True, stop=True)
                return r

            P.op("pe", f_att, reads=[("k_inv", 0), ("k_inv", 1), ("q_dec", 0), ("q_dec", 1)], writes=[("ps", bX), ("ps", bY)])
            for half, bb in ((0, bX), (1, bY)):
                P.op("dve", lambda e, half=half, bb=bb: e.tensor_tensor(attm[:, half:4:2, :], C.ps[:, bb, 0:256].rearrange("p (h f) -> p h f", h=2),
                                                                   bc(C.mask[:, 384:512], [[0, 2], [1, 128]]), ALU.mult),
                     reads=[("ps", bb), ("mask",)], writes=[("attm", half)])
            for x in range(2):
                xs = slice(nl * 128 + x * 64, nl * 128 + x * 64 + 64)
                rows = slice(x * 64, x * 64 + 64)
                ci = nl * 2 + x

                def f_inter(eng, xs=xs, x=x):
                    r = None
                    for h in range(4):
                        cp, half = h // 2, h % 2
                        hp = slice(half * 64, half * 64 + 64)
                        r = eng.matmul(C.ps[:, half, cp * 128 + x * 64:cp * 128 + x * 64 + 64], S_bf[cp][hp, :], q_dec[hp, cp, xs],
                                       start=(x == 0 and cp == 0), stop=False, skip_group_check=True)
                    return r

                P.op("pe", f_inter, reads=[("Sbf", 0), ("Sbf", 1), ("q_dec", 0), ("q_dec", 1)], writes=[("ps", 0), ("ps", 1)])
                for cp in range(2):
                    bk = C.bank()
                    P.op("pe", lambda e, bk=bk, cp=cp, rows=rows, nl=nl: e.matmul(C.ps[:, bk, 0:256], ket[rows, nl, cp, :],
                                                                            gv_tok[rows, nl, cp * 256:(cp + 1) * 256], start=True, stop=True),
                         reads=[("ket", nl, cp), ("gv", nl)], writes=[("ps", bk)])
                    for half in range(2):
                        hp = slice(half * 64, half * 64 + 64)
                        P.op("dve", lambda e, bk=bk, cp=cp, hp=hp, half=half, ci=ci: e.scalar_tensor_tensor(
                            S[cp][hp, :], S[cp][hp, :], decs[hp, cp, ci:ci + 1], C.ps[hp, bk, half * 128:(half + 1) * 128], ALU.mult, ALU.add),
                             reads=[("ps", bk), ("decs", cp), ("S", cp)], writes=[("S", cp)])
                    P.op("act", lambda e, cp=cp: e.copy(S_bf[cp], S[cp]), reads=[("S", cp)], writes=[("Sbf", cp)])

            def f_intra(eng, nl=nl):
                r = None
                for h in range(4):
                    cp, half = h // 2, h % 2
                    r = eng.matmul(C.ps[:, half, cp * 128:(cp + 1) * 128], gv_tok[:, nl, h * 128:(h + 1) * 128], attm[:, h, :], start=False, stop=True,
                                   skip_group_check=True)
                return r

            P.op("pe", f_intra, reads=[("gv", nl), ("attm", 0), ("attm", 1)], writes=[("ps", 0), ("ps", 1)])
            for half in range(2):
                P.op("act", lambda e, half=half: e.activation(sq, C.ps[:, half, 0:256], AF.Square), reads=[("ps", half)], writes=[("gsq",)])
                b2 = C.bank()
                P.op("pe", lambda e, b2=b2: e.matmul(C.ps[:, b2, 0:256], C.ones_bf[:, :], sq, start=True, stop=True), reads=[("gsq",), ("ones",)], writes=[("ps", b2)])
                P.op("act", lambda e, b2=b2: e.activation(rstd, C.ps[:, b2, 0:256], AF.Sqrt, bias=C.consts[:, C.eps_col:C.eps_col + 1], scale=1.0 / 128),
                     reads=[("ps", b2)], writes=[("grstd",)])
                P.op("dve", lambda e: e.reciprocal(rstd, rstd), reads=[("grstd",)], writes=[("grstd",)])
                P.op("dve", lambda e, half=half: e.scalar_tensor_tensor(ytmp, C.ps[:, half, 0:256], glan, rstd, ALU.mult, ALU.mult),
                     reads=[("ps", half), ("grstd",)], writes=[("ytmp",)])
                P.op("dve", lambda e, half=half, bs=bs: e.tensor_tensor(oG[:, half:4:2, bs], ytmp.rearrange("p (h f) -> p h f", h=2),
                                                                   grs[:, half:4:2, bs], ALU.mult),
                     reads=[("ytmp",), ("grs", half), ("grs", half + 2)], writes=[("oG",)])
        for d in range(8):
            b = C.bank()

            def f_mm(eng, d=d, b=b):
                r = None
                for c in range(4):
                    r = eng.matmul(C.ps[:, b, :], woG[:, c, d * 128:(d + 1) * 128], oG[:, c, :], start=(c == 0), stop=(c == 3))
                return r

            P.op("pe", f_mm, reads=[("woG",), ("oG",)], writes=[("ps", b)])
            P.op("dve", lambda e, d=d, b=b, ts=ts: e.tensor_tensor(C.xT[:, d, ts], C.ps[:, b, :], C.xT[:, d, ts], ALU.add),
                 reads=[("ps", b), ("xT", d, j)], writes=[("xT", d, j)])


def ssd_stage(P, C, s):
    w_in = C.need("e_w_in", [D, 3760], lambda inp: inp["e_w_in"][0])
    w_out = C.need("e_w_out", [1536, D], lambda inp: inp["e_w_out"][0])
    cc = C.ccols
    A = Arena(C)
    wp = [A.bf(8, 512) for _ in range(2)]
    wdt = A.bf(8, 16)
    woS = A.bf(8, D)
    zs = A.bf(8, 512)
    xsT = A.bf(8, 512)
    BT = A.bf(4, 512)
    CT = A.bf(4, 512)
    rb = [A.f32(515) for _ in range(2)]
    acc = [A.f32(512) for _ in range(2)]
    halo = A.f32(16, 3)
    x1 = A.f32(16)
    dt = A.f32(16)
    a_ = A.f32(16)
    aneg = A.f32(16)
    cst = A.f32(16)
    ncs = A.f32(16)
    ds = A.f32(16)
    cdec = A.f32(16)
    dtds = A.f32(16)
    xd = A.bf(1024)
    xdw = A.bf(1024)
    B_tok = A.bf(4, 128)
    CBs = A.f32(4, 128)
    Ecs = [A.f32(128) for _ in range(3)]
    E = [A.f32(128) for _ in range(3)]
    Coff = [A.bf(128) for _ in range(3)]
    Mh = [A.bf(128) for _ in range(3)]
    st = A.f32(1024)
    st_bf = A.bf(1024)
    yg = A.f32(8, 128)
    sq = A.bf(8, 128)
    rstd = A.f32(512)
    oS = A.bf(8, 512)
    w_in_v = w_in.rearrange("(k p) f -> p k f", p=128)
    onec = C.consts[:, cc["one"][0]:cc["one"][0] + 1]
    epsc = C.consts[:, C.eps_col:C.eps_col + 1]
    cwc, cbc, dsk, ssn = cc["e_convw"][0], cc["e_convb"][0], cc["e_dskip"][0], cc["e_ssmn"][0]
    MC = C.mask[:, 0:128]
    NEG = C.mask[:, 640:768]
    IDf = C.mask[:, 512:640]

    P.dma("pool", lambda e: e.dma_start(out=wdt, in_=w_in_v[:, :, 3072:3088]), "wB", writes=[("wdt",)])
    P.dma("pool", lambda e: e.dma_start(out=woS, in_=w_out[0:1024, :].rearrange("(c p) d -> p c d", p=128)), "wC", writes=[("woS",)])
    P.op("act", lambda e: e.activation(aneg, C.consts[:, cc["e_alog"][0]:cc["e_alog"][0] + 16], AF.Exp), reads=[("consts",)], writes=[("aneg",)])
    P.op("dve", lambda e: e.tensor_scalar(aneg, aneg, -1.0, None, ALU.mult), reads=[("aneg",)], writes=[("aneg",)])
    P.op("dve", lambda e: e.memset(st, 0.0), writes=[("st",)])
    P.op("dve", lambda e: e.memset(st_bf, 0.0), writes=[("st_bf",)])
    P.op("dve", lambda e: e.memset(halo, 0.0), writes=[("halo",)])
    ws = [0]
    rbi = [0]

    for j in range(4):
        ts = tsl(j)
        for pi in range(6):
            slot = ws[0]
            ws[0] = (ws[0] + 1) % 2
            P.dma("pool", lambda e, slot=slot, pi=pi: e.dma_start(out=wp[slot], in_=w_in_v[:, :, pi * 512:(pi + 1) * 512]),
                  f"wp{slot}", writes=[("wp", slot)])
            for ci in range(4):
                c = pi * 4 + ci
                b = C.bank()

                def f_mm(eng, slot=slot, ci=ci, b=b, ts=ts):
                    r = None
                    for k in range(8):
                        r = eng.matmul(C.ps[:, b, :], wp[slot][:, k, ci * 128:(ci + 1) * 128], C.hT[:, k, ts], start=(k == 0), stop=(k == 7))
                    return r

                P.op("pe", f_mm, reads=[("wp", slot)] + [("hT", k, j) for k in range(8)], writes=[("ps", b)])
                if c < 8:
                    P.op("act", lambda e, b=b, c=c: e.activation(zs[:, c, :], C.ps[:, b, :], AF.Silu), reads=[("ps", b)], writes=[("zs", c)])
                    continue
                xc = c - 8
                ri = rbi[0]
                rbi[0] = (rbi[0] + 1) % 2
                R, AC = rb[ri], acc[ri]
                P.op("act", lambda e, b=b, R=R: e.copy(R[:, 3:515], C.ps[:, b, :]), reads=[("ps", b)], writes=[("rb", ri)])
                P.op("dve", lambda e, R=R, xc=xc: e.tensor_copy(R[:, 0:3], halo[:, xc, :]), reads=[("halo",), ("rb", ri)], writes=[("rb", ri)])

                def wcol(xc, t):
                    return C.consts[:, cwc + xc * 4 + t:cwc + xc * 4 + t + 1]

                P.op("dve", lambda e, R=R, AC=AC, xc=xc: e.tensor_scalar(AC, R[:, 3:515], wcol(xc, 3), None, ALU.mult),
                     reads=[("rb", ri)], writes=[("acc", ri)])
                for t in (2, 1, 0):
                    P.op("dve", lambda e, R=R, AC=AC, xc=xc, t=t: e.scalar_tensor_tensor(AC, R[:, t:t + 512], wcol(xc, t), AC, ALU.mult, ALU.add),
                         reads=[("rb", ri), ("acc", ri)], writes=[("acc", ri)])
                P.op("dve", lambda e, R=R, xc=xc: e.tensor_copy(halo[:, xc, :], R[:, 512:515]), reads=[("rb", ri), ("halo",)], writes=[("halo",)])
                if xc < 8:
                    dest, key = xsT[:, xc, :], ("xsT", xc)
                elif xc < 12:
                    dest, key = BT[:, xc - 8, :], ("BT", xc - 8)
                else:
                    dest, key = CT[:, xc - 12, :], ("CT", xc - 12)
                P.op("act", lambda e, AC=AC, dest=dest, xc=xc: e.activation(dest, AC, AF.Silu, bias=C.consts[:, cbc + xc:cbc + xc + 1], scale=1.0),
                     reads=[("acc", ri)], writes=[key])
        for nl in range(4):
            n = 4 * j + nl
            bs = slice(nl * 128, (nl + 1) * 128)
            b = C.bank()

            def f_dt(eng, b=b, n=n):
                r = None
                for k in range(8):
                    r = eng.matmul(C.ps[:, b, 0:16], C.hT[:, k, n * 128:(n + 1) * 128], wdt[:, k, :], start=(k == 0), stop=(k == 7))
                return r

            P.op("pe", f_dt, reads=[("wdt",)] + [("hT", k, j) for k in range(8)], writes=[("ps", b)])
            P.op("dve", lambda e, b=b: e.tensor_tensor(x1, C.ps[:, b, 0:16], C.consts[:, cc["e_dtb"][0]:cc["e_dtb"][0] + 16], ALU.add),
                 reads=[("ps", b)], writes=[("x1",)])
            P.op("act", lambda e: e.activation(x1, x1, AF.Exp), reads=[("x1",)], writes=[("x1",)])
            P.op("act", lambda e: e.activation(dt, x1, AF.Ln, bias=onec, scale=1.0), reads=[("x1",)], writes=[("dt",)])
            P.op("dve", lambda e: e.tensor_tensor(a_, dt, aneg, ALU.mult), reads=[("dt",), ("aneg",)], writes=[("a",)])
            b = C.bank()

            def f_cs(eng, b=b):
                eng.matmul(C.ps[:, b, 0:16], MC, a_, start=True, stop=True)
                return eng.matmul(C.ps[:, b, 16:32], bc(onec, [[0, 128]]), a_, start=True, stop=True)

            P.op("pe", f_cs, reads=[("a",), ("mask",)], writes=[("ps", b)])
            P.op("dve", lambda e, b=b: e.tensor_copy(cst, C.ps[:, b, 0:16]), reads=[("ps", b)], writes=[("cst",)])
            P.op("dve", lambda e: e.tensor_scalar(ncs, cst, -1.0, None, ALU.mult), reads=[("cst",)], writes=[("ncs",)])
            P.op("dve", lambda e, b=b: e.tensor_tensor(ds, C.ps[:, b, 16:32], cst, ALU.subtract), reads=[("ps", b), ("cst",)], writes=[("ds",)])
            P.op("act", lambda e: e.activation(ds, ds, AF.Exp), reads=[("ds",)], writes=[("ds",)])
            P.op("act", lambda e, b=b: e.activation(cdec, C.ps[:, b, 16:32], AF.Exp), reads=[("ps", b)], writes=[("cdec",)])
            P.op("dve", lambda e: e.tensor_tensor(dtds, dt, ds, ALU.mult), reads=[("dt",), ("ds",)], writes=[("dtds",)])
            for hb in range(2):
                b = C.bank()

                def f_tr(eng, b=b, hb=hb, bs=bs):
                    r = None
                    for cq in range(4):
                        c = hb * 4 + cq
                        r = eng.matmul(C.ps[:, b, cq * 128:(cq + 1) * 128], xsT[:, c, bs], C.ident_bf[:, :], start=True, stop=True)
                    return r

                P.op("pe", f_tr, reads=[("xsT", hb * 4 + q) for q in range(4)] + [("ident",)], writes=[("ps", b)])
                P.op("dve", lambda e, b=b, hb=hb: e.tensor_tensor(xd[:, hb * 512:(hb + 1) * 512].rearrange("p (h q) -> p h q", h=8),
                                                             C.ps[:, b, :].rearrange("p (h q) -> p h q", h=8),
                                                             bc(dt[:, hb * 8:hb * 8 + 8], [[1, 8], [0, 64]]), ALU.mult),
                     reads=[("ps", b), ("dt",)], writes=[("xd", hb)])
                P.op("dve", lambda e, b=b, hb=hb: e.tensor_tensor(xdw[:, hb * 512:(hb + 1) * 512].rearrange("p (h q) -> p h q", h=8),
                                                             C.ps[:, b, :].rearrange("p (h q) -> p h q", h=8),
                                                             bc(dtds[:, hb * 8:hb * 8 + 8], [[1, 8], [0, 64]]), ALU.mult),
                     reads=[("ps", b), ("dtds",)], writes=[("xdw", hb)])
            b = C.bank()

            def f_trb(eng, b=b, bs=bs):
                r = None
                for g in range(4):
                    r = eng.matmul(C.ps[:, b, g * 128:(g + 1) * 128], BT[:, g, bs], C.ident_bf[:, :], start=True, stop=True)
                return r

            P.op("pe", f_trb, reads=[("BT", g) for g in range(4)] + [("ident",)], writes=[("ps", b)])
            P.op("act", lambda e, b=b: e.copy(B_tok.rearrange("p g n -> p (g n)"), C.ps[:, b, :]), reads=[("ps", b)], writes=[("B_tok",)])
            b = C.bank()

            def f_cb(eng, b=b, bs=bs):
                r = None
                for g in range(4):
                    r = eng.matmul(C.ps[:, b, g * 128:(g + 1) * 128], BT[:, g, bs], CT[:, g, bs], start=True, stop=True)
                return r

            P.op("pe", f_cb, reads=[("BT", g) for g in range(4)] + [("CT", g) for g in range(4)], writes=[("ps", b)])
            P.op("act", lambda e, b=b: e.copy(CBs.rearrange("p g n -> p (g n)"), C.ps[:, b, :]), reads=[("ps", b)], writes=[("CBs",)])
            def ssd_s1(h, bs=bs):
                g = h // 4
                hi = h % 3
                b = C.bank()

                def f_csb(eng, b=b, h=h):
                    al = bc(a_[:, h:h + 1], [[0, 128]])
                    eng.matmul(C.ps[:, b, 0:128], al, MC, start=True, stop=True)
                    eng.matmul(C.ps[:, b, 128:256], al, MC, start=False, stop=False, skip_group_check=True)
                    return eng.matmul(C.ps[:, b, 128:256], IDf, NEG, start=False, stop=True, skip_group_check=True)

                P.op("pe", f_csb, reads=[("a",), ("mask",)], writes=[("ps", b)])
                P.op("act", lambda e, b=b, hi=hi: e.activation(Ecs[hi], C.ps[:, b, 0:128], AF.Exp), reads=[("ps", b)], writes=[("Ecs", hi)])
                P.op("act", lambda e, b=b, hi=hi, h=h: e.activation(E[hi], C.ps[:, b, 128:256], AF.Exp, bias=ncs[:, h:h + 1], scale=1.0),
                     reads=[("ps", b), ("ncs",)], writes=[("E", hi)])
                P.op("pool", lambda e, hi=hi, g=g, bs=bs: e.tensor_tensor(Coff[hi], CT[:, g, bs], Ecs[hi], ALU.mult),
                     reads=[("Ecs", hi), ("CT", g)], writes=[("Coff", hi)])
                P.op("dve", lambda e, hi=hi, g=g: e.tensor_tensor(Mh[hi], E[hi], CBs[:, g, :], ALU.mult),
                     reads=[("E", hi), ("CBs",)], writes=[("Mh", hi)])

            def ssd_s2(h):
                hi = h % 3

                def f_y(eng, h=h, hi=hi):
                    yb = h // 8
                    col = ((h % 8) // 2) * 128
                    first = (h % 8) < 2
                    if h % 2 == 0:
                        out = C.ps[0:64, yb, col:col + 128]
                        kw = {}
                    else:
                        out = C.ps[64:128, yb, col:col + 128]
                        kw = {"tile_position": (0, 64)}
                    eng.matmul(out, xd[:, h * 64:(h + 1) * 64], Mh[hi], start=first, stop=False, skip_group_check=True, **kw)
                    return eng.matmul(out, st_bf[:, h * 64:(h + 1) * 64], Coff[hi], start=False, stop=True, skip_group_check=True, **kw)

                P.op("pe", f_y, reads=[("xd", h // 8), ("Mh", hi), ("st_bf",), ("Coff", hi)], writes=[("ps", h // 8)])

            for h in range(16 + 2):
                if h < 16:
                    ssd_s1(h)
                if h >= 2:
                    ssd_s2(h - 2)
            bst = [C.bank(), C.bank()]

            def f_st(eng, bst=bst):
                r = None
                for g in range(4):
                    r = eng.matmul(C.ps[:, bst[g // 2], (g % 2) * 256:(g % 2) * 256 + 256], B_tok[:, g, :], xdw[:, g * 256:(g + 1) * 256],
                                   start=True, stop=True)
                return r

            P.op("pe", f_st, reads=[("B_tok",), ("xdw", 0), ("xdw", 1)], writes=[("ps", bst[0]), ("ps", bst[1])])
            P.op("dve", lambda e: e.tensor_tensor(st.rearrange("p (h q) -> p h q", h=16), st.rearrange("p (h q) -> p h q", h=16),
                                             bc(cdec, [[1, 16], [0, 64]]), ALU.mult), reads=[("st",), ("cdec",)], writes=[("st",)])
            for hb in range(2):
                P.op("dve", lambda e, hb=hb, bst=bst: e.tensor_tensor(st[:, hb * 512:(hb + 1) * 512], st[:, hb * 512:(hb + 1) * 512], C.ps[:, bst[hb], :], ALU.add),
                     reads=[("st",), ("ps", bst[hb])], writes=[("st",)])
            P.op("act", lambda e: e.copy(st_bf, st), reads=[("st",)], writes=[("st_bf",)])
            for c in range(8):
                yb, col = c // 4, (c % 4) * 128
                P.op("dve", lambda e, c=c, yb=yb, col=col, bs=bs: e.scalar_tensor_tensor(yg[:, c, :], xsT[:, c, bs], C.consts[:, dsk + c:dsk + c + 1],
                                                                                C.ps[:, yb, col:col + 128], ALU.mult, ALU.add),
                     reads=[("xsT", c), ("ps", yb)], writes=[("yg", c)])
                P.op("pool", lambda e, c=c, bs=bs: e.tensor_tensor(yg[:, c, :], yg[:, c, :], zs[:, c, bs], ALU.mult),
                     reads=[("yg", c), ("zs", c)], writes=[("yg", c)])
            P.op("act", lambda e: e.activation(sq, yg, AF.Square), reads=[("yg", c) for c in range(8)], writes=[("ssq",)])
            b = C.bank()

            def f_ss(eng, b=b):
                r = None
                for gi in range(4):
                    eng.matmul(C.ps[:, b, gi * 128:(gi + 1) * 128], C.ones_bf[:, :], sq[:, 2 * gi, :], start=(gi == 0), stop=False, skip_group_check=True)
                    r = eng.matmul(C.ps[:, b, gi * 128:(gi + 1) * 128], C.ones_bf[:, :], sq[:, 2 * gi + 1, :], start=False, stop=True, skip_group_check=True)
                return r

            P.op("pe", f_ss, reads=[("ssq",), ("ones",)], writes=[("ps", b)])
            P.op("act", lambda e, b=b: e.activation(rstd, C.ps[:, b, :], AF.Sqrt, bias=epsc, scale=1.0 / 256), reads=[("ps", b)], writes=[("srstd",)])
            P.op("dve", lambda e: e.reciprocal(rstd, rstd), reads=[("srstd",)], writes=[("srstd",)])
            for c in range(8):
                P.op("dve", lambda e, c=c, bs=bs: e.scalar_tensor_tensor(oS[:, c, bs], yg[:, c, :], C.consts[:, ssn + c:ssn + c + 1],
                                                                    rstd[:, (c // 2) * 128:(c // 2 + 1) * 128], ALU.mult, ALU.mult),
                     reads=[("yg", c), ("srstd",)], writes=[("oS",)])
        for d in range(8):
            b = C.bank()

            def f_mm(eng, d=d, b=b):
                r = None
                for c in range(8):
                    r = eng.matmul(C.ps[:, b, :], woS[:, c, d * 128:(d + 1) * 128], oS[:, c, :], start=(c == 0), stop=(c == 7))
                return r

            P.op("pe", f_mm, reads=[("woS",), ("oS",)], writes=[("ps", b)])
            P.op("dve", lambda e, d=d, b=b, ts=ts: e.tensor_tensor(C.xT[:, d, ts], C.ps[:, b, :], C.xT[:, d, ts], ALU.add),
                 reads=[("ps", b), ("xT", d, j)], writes=[("xT", d, j)])


def rope_tables(P, C, s, j, posi, ang, tmpf, tmpi, C96, S96):
    cc = C.ccols
    invf = C.consts[:, cc["invf"][0]:cc["invf"][0] + 1]
    sgn = C.consts[:, cc["sgn"][0]:cc["sgn"][0] + 1]
    P.dma("sp", lambda e: e.dma_start(out=posi, in_=bass.AP(C.pos.tensor, C.pos[s:s + 1, j * 512:(j + 1) * 512].offset, [[0, 128], [1, 512]])),
          "posB", writes=[("posi",)])
    P.op("dve", lambda e: e.tensor_copy(ang, posi), reads=[("posi",)], writes=[("ang",)])
    P.op("dve", lambda e: e.tensor_scalar(ang, ang, invf, None, ALU.mult), reads=[("ang",)], writes=[("ang",)])

    def frac(buf):
        P.op("dve", lambda e: e.tensor_copy(tmpi, buf), reads=[("ang",)], writes=[("tmpi",)])
        P.op("dve", lambda e: e.tensor_copy(tmpf, tmpi), reads=[("tmpi",)], writes=[("tmpf",)])
        P.op("dve", lambda e: e.tensor_tensor(buf, buf, tmpf, ALU.subtract), reads=[("tmpf",), ("ang",)], writes=[("ang",)])
        P.op("dve", lambda e: e.tensor_single_scalar(tmpf, buf, 0.5, ALU.is_gt), reads=[("ang",)], writes=[("tmpf",)])
        P.op("dve", lambda e: e.tensor_tensor(buf, buf, tmpf, ALU.subtract), reads=[("tmpf",), ("ang",)], writes=[("ang",)])
        P.op("dve", lambda e: e.tensor_single_scalar(tmpf, buf, -0.5, ALU.is_lt), reads=[("ang",)], writes=[("tmpf",)])
        P.op("dve", lambda e: e.tensor_tensor(buf, buf, tmpf, ALU.add), reads=[("tmpf",), ("ang",)], writes=[("ang",)])

    frac(ang)
    P.op("act", lambda e: e.activation(S96, ang, AF.Sin, scale=float(2 * np.pi)), reads=[("ang",)], writes=[("S96",)])
    P.op("dve", lambda e: e.tensor_scalar(S96, S96, sgn, None, ALU.mult), reads=[("S96",)], writes=[("S96",)])
    P.op("dve", lambda e: e.tensor_scalar(ang, ang, 0.25, None, ALU.add), reads=[("ang",), ("S96",)], writes=[("ang",)])
    frac(ang)
    P.op("act", lambda e: e.activation(C96, ang, AF.Sin, scale=float(2 * np.pi)), reads=[("ang",)], writes=[("C96",)])


def mla_kv_stage(P, C, s):
    w_in = C.need("e_w_in", [D, 3760], lambda inp: inp["e_w_in"][0])
    w_kvb = C.need("e_w_kv_b", [256, 1024], lambda inp: inp["e_w_kv_b"][0])
    cc = C.ccols
    A = Arena(C)
    kT = A.bf(8, T)
    V_tok = A.bf(16, 512)
    wA = A.bf(8, 288)
    wpp = A.bf(8, 32)
    wkvb = A.bf(2, 1024)
    kvn = A.bf(2, 512)
    sq = A.bf(512)
    sqk = A.bf(512)
    rstd = A.f32(512)
    posi = A.i32(512)
    ang = A.f32(512)
    tmpf = A.f32(512)
    tmpi = A.i32(512)
    C96 = A.f32(512)
    S96 = A.f32(512)
    kr = A.f32(512)
    t1 = A.f32(512)
    w_in_v = w_in.rearrange("(k p) f -> p k f", p=128)
    epsc = C.consts[:, C.eps_col:C.eps_col + 1]
    kvan = cc["e_kvan"][0]
    gk = C.consts[:, cc["gk"][0]:cc["gk"][0] + 1]
    gkp = C.consts[:, cc["gkp"][0]:cc["gkp"][0] + 1]
    R = slice(64, 96)

    P.dma("pool", lambda e: e.dma_start(out=wA, in_=w_in_v[:, :, 3472:3760]), "wA", writes=[("wA",)])
    P.dma("pool", lambda e: [e.dma_start(out=wpp[:, :, 0:16], in_=w_in_v[:, :, 3744:3760]),
                             e.dma_start(out=wpp[:, :, 16:32], in_=w_in_v[:, :, 3728:3744])], "wB", writes=[("wpp",)], n=2)
    P.dma("pool", lambda e: e.dma_start(out=wkvb, in_=w_kvb.rearrange("(k p) f -> p k f", p=128)), "wC", writes=[("wkvb",)])

    for j in range(4):
        ts = tsl(j)
        rope_tables(P, C, s, j, posi, ang, tmpf, tmpi, C96, S96)
        bk = []
        for c in range(2):
            b = C.bank()
            bk.append(b)

            def f_mm(eng, c=c, b=b, ts=ts):
                r = None
                for k in range(8):
                    r = eng.matmul(C.ps[:, b, :], wA[:, k, c * 128:(c + 1) * 128], C.hT[:, k, ts], start=(k == 0), stop=(k == 7))
                return r

            P.op("pe", f_mm, reads=[("wA",)] + [("hT", k, j) for k in range(8)], writes=[("ps", b)])
        bss = C.bank()
        for c in range(2):
            P.op("act", lambda e, c=c, bk=bk: e.activation(sq, C.ps[:, bk[c], :], AF.Square), reads=[("ps", bk[c])], writes=[("msq",)])
            P.op("pe", lambda e, c=c, bss=bss: e.matmul(C.ps[:, bss, :], C.ones_bf[:, :], sq, start=(c == 0), stop=(c == 1)),
                 reads=[("msq",), ("ones",)], writes=[("ps", bss)])
        P.op("act", lambda e, bss=bss: e.activation(rstd, C.ps[:, bss, :], AF.Sqrt, bias=epsc, scale=1.0 / 256), reads=[("ps", bss)], writes=[("mrstd",)])
        P.op("dve", lambda e: e.reciprocal(rstd, rstd), reads=[("mrstd",)], writes=[("mrstd",)])
        for c in range(2):
            P.op("dve", lambda e, c=c, bk=bk: e.scalar_tensor_tensor(kvn[:, c, :], C.ps[:, bk[c], :], C.consts[:, kvan + c:kvan + c + 1], rstd, ALU.mult, ALU.mult),
                 reads=[("ps", bk[c]), ("mrstd",)], writes=[("kvn", c)])
        bx = C.bank()
        by = C.bank()

        def f_pe(eng, bx=bx, ts=ts):
            r = None
            for k in range(8):
                r = eng.matmul(C.ps[64:96, bx, :], wA[:, k, 256:288], C.hT[:, k, ts], start=(k == 0), stop=(k == 7), tile_position=(0, 64))
            return r

        def f_pp(eng, by=by, ts=ts):
            r = None
            for k in range(8):
                r = eng.matmul(C.ps[64:96, by, :], wpp[:, k, :], C.hT[:, k, ts], start=(k == 0), stop=(k == 7), tile_position=(0, 64))
            return r

        P.op("pe", f_pe, reads=[("wA",)] + [("hT", k, j) for k in range(8)], writes=[("ps", bx)])
        P.op("pe", f_pp, reads=[("wpp",)] + [("hT", k, j) for k in range(8)], writes=[("ps", by)])
        P.op("dve", lambda e, bx=bx: e.scalar_tensor_tensor(kr[R, :], C.ps[R, bx, :], gk[R, :], C96[R, :], ALU.mult, ALU.mult),
             reads=[("ps", bx), ("C96",)], writes=[("kr",)])
        P.op("dve", lambda e, by=by: e.scalar_tensor_tensor(t1[R, :], C.ps[R, by, :], gkp[R, :], S96[R, :], ALU.mult, ALU.mult),
             reads=[("ps", by), ("S96",)], writes=[("t1",)])
        P.op("dve", lambda e: e.tensor_tensor(kr[R, :], kr[R, :], t1[R, :], ALU.add), reads=[("kr",), ("t1",)], writes=[("kr",)])
        P.op("act", lambda e, bx=bx: e.activation(sqk[R, :], C.ps[R, bx, :], AF.Square), reads=[("ps", bx)], writes=[("sqk",)])
        for h in range(8):
            b = C.bank()

            def f_kn(eng, h=h, b=b):
                r = None
                for k in range(2):
                    r = eng.matmul(C.ps[0:64, b, :], wkvb[:, k, h * 128:h * 128 + 64], kvn[:, k, :], start=(k == 0), stop=(k == 1))
                return r

            P.op("pe", f_kn, reads=[("wkvb",), ("kvn", 0), ("kvn", 1)], writes=[("ps", b)])
            P.op("act", lambda e, b=b: e.activation(sqk[0:64, :], C.ps[0:64, b, :], AF.Square), reads=[("ps", b)], writes=[("sqk",)])
            b2 = C.bank()
            P.op("pe", lambda e, b2=b2: e.matmul(C.ps[0:96, b2, :], C.ones_bf[0:96, 0:96], sqk[0:96, :], start=True, stop=True),
                 reads=[("sqk",), ("ones",)], writes=[("ps", b2)])
            P.op("act", lambda e, b2=b2: e.activation(rstd[0:96, :], C.ps[0:96, b2, :], AF.Sqrt, bias=epsc[0:96, :], scale=1.0 / 96),
                 reads=[("ps", b2)], writes=[("mrstd",)])
            P.op("dve", lambda e: e.reciprocal(rstd[0:96, :], rstd[0:96, :]), reads=[("mrstd",)], writes=[("mrstd",)])
            P.op("dve", lambda e, h=h, b=b, ts=ts: e.scalar_tensor_tensor(kT[0:64, h, ts], C.ps[0:64, b, :], gk[0:64, :], rstd[0:64, :], ALU.mult, ALU.mult),
                 reads=[("ps", b), ("mrstd",)], writes=[("kT", h, j)])
            P.op("dve", lambda e, h=h, ts=ts: e.tensor_tensor(kT[R, h, ts], kr[R, :], rstd[R, :], ALU.mult),
                 reads=[("kr",), ("mrstd",)], writes=[("kT", h, j)])
        for nl in range(4):
            n = 4 * j + nl
            b = C.bank()

            def f_v(eng, b=b, nl=nl):
                r = None
                for k in range(2):
                    r = eng.matmul(C.ps[:, b, :], kvn[:, k, nl * 128:(nl + 1) * 128],
                                   wkvb[:, k, :].rearrange("p (h x) -> p h x", h=8)[:, :, 64:128], start=(k == 0), stop=(k == 1))
                return r

            P.op("pe", f_v, reads=[("wkvb",), ("kvn", 0), ("kvn", 1)], writes=[("ps", b)])
            P.op("act", lambda e, b=b, n=n: e.copy(V_tok[:, n, :], C.ps[:, b, :]), reads=[("ps", b)], writes=[("V_tok", n)])


def mla_attn_stage(P, C, s):
    w_in = C.need("e_w_in", [D, 3760], lambda inp: inp["e_w_in"][0])
    w_qb = C.need("e_w_q_b", [384, 768], lambda inp: inp["e_w_q_b"][0])
    w_out = C.need("e_w_out", [1536, D], lambda inp: inp["e_w_out"][0])
    cc = C.ccols
    A = Arena(C)
    kT = A.bf(8, T)
    V_tok = A.bf(16, 512)
    wA = A.bf(8, 384)
    wqb = A.bf(3, 768)
    wqbp = A.bf(3, 8, 32)
    woM = A.bf(4, D)
    qan = A.bf(3, 512)
    sq = A.bf(512)
    rstd = A.f32(512)
    posi = A.i32(512)
    ang = A.f32(512)
    tmpf = A.f32(512)
    tmpi = A.i32(512)
    C96 = A.f32(512)
    S96 = A.f32(512)
    t1 = A.f32(512)
    t2 = A.f32(512)
    qT = [A.bf(512) for _ in range(2)]
    Pb = [A.bf(512) for _ in range(4)]
    rden = A.f32(512)
    oT = A.bf(4, 512)
    w_in_v = w_in.rearrange("(k p) f -> p k f", p=128)
    epsc = C.consts[:, C.eps_col:C.eps_col + 1]
    qanc = cc["e_qan"][0]
    gq = C.consts[:, cc["gq"][0]:cc["gq"][0] + 1]
    gqp = C.consts[:, cc["gqp"][0]:cc["gqp"][0] + 1]
    R = slice(64, 96)
    SC = float(96 ** -0.5)
    w_qb_v = w_qb.rearrange("(k p) (h x) -> p k h x", p=128, h=8)

    P.dma("pool", lambda e: e.dma_start(out=wA, in_=w_in_v[:, :, 3088:3472]), "wA", writes=[("wA",)])
    P.dma("pool", lambda e: e.dma_start(out=wqb, in_=w_qb.rearrange("(k p) f -> p k f", p=128)), "wB", writes=[("wqb",)])
    P.dma("pool", lambda e: [e.dma_start(out=wqbp[:, k, :, 0:16], in_=w_qb_v[:, k, :, 80:96]) for k in range(3)]
          + [e.dma_start(out=wqbp[:, k, :, 16:32], in_=w_qb_v[:, k, :, 64:80]) for k in range(3)], "wC", writes=[("wqbp",)], n=6)
    P.dma("pool", lambda e: e.dma_start(out=woM, in_=w_out[1024:1536, :].rearrange("(c p) d -> p c d", p=128)), "wD", writes=[("woM",)])
    pbi = [0]
    qi = [0]

    for j in range(4):
        ts = tsl(j)
        rope_tables(P, C, s, j, posi, ang, tmpf, tmpi, C96, S96)
        bq = []
        for c in range(3):
            b = C.bank()
            bq.append(b)

            def f_mm(eng, c=c, b=b, ts=ts):
                r = None
                for k in range(8):
                    r = eng.matmul(C.ps[:, b, :], wA[:, k, c * 128:(c + 1) * 128], C.hT[:, k, ts], start=(k == 0), stop=(k == 7))
                return r

            P.op("pe", f_mm, reads=[("wA",)] + [("hT", k, j) for k in range(8)], writes=[("ps", b)])
        bss = C.bank()
        for c in range(3):
            P.op("act", lambda e, c=c, bq=bq: e.activation(sq, C.ps[:, bq[c], :], AF.Square), reads=[("ps", bq[c])], writes=[("msq",)])
            P.op("pe", lambda e, c=c, bss=bss: e.matmul(C.ps[:, bss, :], C.ones_bf[:, :], sq, start=(c == 0), stop=(c == 2)),
                 reads=[("msq",), ("ones",)], writes=[("ps", bss)])
        P.op("act", lambda e, bss=bss: e.activation(rstd, C.ps[:, bss, :], AF.Sqrt, bias=epsc, scale=1.0 / 384), reads=[("ps", bss)], writes=[("mrstd",)])
        P.op("dve", lambda e: e.reciprocal(rstd, rstd), reads=[("mrstd",)], writes=[("mrstd",)])
        for c in range(3):
            P.op("dve", lambda e, c=c, bq=bq: e.scalar_tensor_tensor(qan[:, c, :], C.ps[:, bq[c], :], C.consts[:, qanc + c:qanc + c + 1], rstd, ALU.mult, ALU.mult),
                 reads=[("ps", bq[c]), ("mrstd",)], writes=[("qan", c)])
        def qA(h):
            b = C.bank()
            bp = C.bank()

            def f_q(eng, h=h, b=b, bp=bp):
                r = None
                for k in range(3):
                    r = eng.matmul(C.ps[0:96, b, :], wqb[:, k, h * 96:(h + 1) * 96], qan[:, k, :], start=(k == 0), stop=(k == 2))
                for k in range(3):
                    r = eng.matmul(C.ps[64:96, bp, :], wqbp[:, k, h, :], qan[:, k, :], start=(k == 0), stop=(k == 2), tile_position=(0, 64))
                return r

            P.op("pe", f_q, reads=[("wqb",), ("wqbp",)] + [("qan", c) for c in range(3)], writes=[("ps", b), ("ps", bp)])
            P.op("act", lambda e, b=b: e.activation(sq[0:96, :], C.ps[0:96, b, :], AF.Square), reads=[("ps", b)], writes=[("msq",)])
            return b, bp

        def qB(h, b, bp):
            b2 = C.bank()
            P.op("pe", lambda e, b2=b2: e.matmul(C.ps[0:96, b2, :], C.ones_bf[0:96, 0:96], sq[0:96, :], start=True, stop=True),
                 reads=[("msq",), ("ones",)], writes=[("ps", b2)])
            P.op("act", lambda e, b2=b2: e.activation(rstd[0:96, :], C.ps[0:96, b2, :], AF.Sqrt, bias=epsc[0:96, :], scale=1.0 / 96),
                 reads=[("ps", b2)], writes=[("mrstd",)])
            P.op("dve", lambda e: e.reciprocal(rstd[0:96, :], rstd[0:96, :]), reads=[("mrstd",)], writes=[("mrstd",)])
            P.op("dve", lambda e, b=b: e.scalar_tensor_tensor(t1[0:96, :], C.ps[0:96, b, :], gq[0:96, :], C96[0:96, :], ALU.mult, ALU.mult),
                 reads=[("ps", b), ("C96",)], writes=[("t1",)])
            P.op("dve", lambda e, bp=bp: e.scalar_tensor_tensor(t2[R, :], C.ps[R, bp, :], gqp[R, :], S96[R, :], ALU.mult, ALU.mult),
                 reads=[("ps", bp), ("S96",)], writes=[("t2",)])
            P.op("dve", lambda e: e.tensor_tensor(t1[R, :], t1[R, :], t2[R, :], ALU.add), reads=[("t1",), ("t2",)], writes=[("t1",)])
            qk = qi[0]
            qi[0] = (qi[0] + 1) % 2
            Q = qT[qk]
            P.op("dve", lambda e, Q=Q: e.tensor_tensor(Q[0:96, :], t1[0:96, :], rstd[0:96, :], ALU.mult), reads=[("t1",), ("mrstd",)], writes=[("qT", qk)])
            return qk, Q

        LOOK = 2
        nxt = qB(0, *qA(0))
        for h in range(8):
            hi = h % 2
            qk, Q = nxt
            pend_q = qA(h + 1) if h + 1 < 8 else None
            nkb = 4 * j + 4
            po = slice(hi * 64, hi * 64 + 64)
            kw = {"tile_position": (0, 64)} if hi == 1 else {}

            def emit_st(kb, h=h, Q=Q, qk=qk):
                b = C.bank()
                P.op("pe", lambda e, b=b, h=h, kb=kb, Q=Q: e.matmul(C.ps[:, b, :], kT[0:96, h, kb * 128:(kb + 1) * 128], Q[0:96, :], start=True, stop=True),
                     reads=[("kT", h, kb // 4), ("qT", qk)], writes=[("ps", b)])
                pk = pbi[0]
                pbi[0] = (pbi[0] + 1) % 4
                PB = Pb[pk]
                P.op("act", lambda e, b=b, PB=PB: e.activation(PB, C.ps[:, b, :], AF.Exp, scale=SC), reads=[("ps", b)], writes=[("Pb", pk)])
                if kb >= 4 * j:
                    o = (kb - 4 * j) * 128
                    m0 = 512 + 384 - o
                    P.op("pool", lambda e, PB=PB, m0=m0: e.tensor_tensor(PB, PB, C.maskb[:, m0:m0 + 512], ALU.mult),
                         reads=[("Pb", pk), ("mask",)], writes=[("Pb", pk)])
                return pk, PB

            def emit_pv(kb, pk, PB, h=h, po=po, kw=kw, nkb=nkb):
                def f_pv(eng):
                    eng.matmul(C.ps[po, 0, :], V_tok[:, kb, h * 64:(h + 1) * 64], PB, start=(kb == 0), stop=(kb == nkb - 1), skip_group_check=True, **kw)
                    return eng.matmul(C.ps[po, 1, :], C.ones_bf[:, 0:64], PB, start=(kb == 0), stop=(kb == nkb - 1), skip_group_check=True, **kw)

                P.op("pe", f_pv, reads=[("V_tok", kb), ("Pb", pk), ("ones",)], writes=[("ps", 0), ("ps", 1)])

            pend = []
            for kb in range(nkb):
                pend.append((kb,) + emit_st(kb))
                if kb == 1 and pend_q is not None:
                    nxt = qB(h + 1, *pend_q)
                    pend_q = None
                if len(pend) > LOOK:
                    emit_pv(*pend.pop(0))
            while pend:
                emit_pv(*pend.pop(0))
            if hi == 1:
                P.op("dve", lambda e: e.reciprocal(rden, C.ps[:, 1, :]), reads=[("ps", 1)], writes=[("rden",)])
                P.op("dve", lambda e, h=h: e.tensor_tensor(oT[:, h // 2, :], C.ps[:, 0, :], rden, ALU.mult), reads=[("ps", 0), ("rden",)], writes=[("oT",)])
        for d in range(8):
            b = C.bank()

            def f_mm(eng, d=d, b=b):
                r = None
                for c in range(4):
                    r = eng.matmul(C.ps[:, b, :], woM[:, c, d * 128:(d + 1) * 128], oT[:, c, :], start=(c == 0), stop=(c == 3))
                return r

            P.op("pe", f_mm, reads=[("woM",), ("oT",)], writes=[("ps", b)])
            P.op("dve", lambda e, d=d, b=b, ts=ts: e.tensor_tensor(C.xT[:, d, ts], C.ps[:, b, :], C.xT[:, d, ts], ALU.add),
                 reads=[("ps", b), ("xT", d, j)], writes=[("xT", d, j)])


def load_x(P, C, xin):
    v = xin.rearrange("(k p) t -> p k t", p=128)
    for k in range(8):
        def f(eng, k=k):
            return eng.dma_start(out=C.xT[:, k, :], in_=v[:, k, :])

        P.dma("sp", f, f"xin{k}", writes=[("xT", k, j) for j in range(4)])


def store_x(P, C, yout):
    v = yout.rearrange("(k p) t -> p k t", p=128)
    for k in range(8):
        def f(eng, k=k):
            return eng.dma_start(out=v[:, k, :], in_=C.xT[:, k, :])

        P.dma("sp", f, f"xout{k}", reads=[("xT", k, j) for j in range(4)])


def build(nseq, stages, ccols, ncc):
    nc = bass.Bass("TRN2", target_bir_lowering=False)
    dr = {}
    hostprep = {}

    def din(name, shape, dt=F32):
        dr[name] = nc.dram_tensor(name, list(shape), dt, kind="ExternalInput").ap()
        return dr[name]

    def need(name, shape, fn, dt=F32):
        if name not in dr:
            din(name, shape, dt)
            hostprep[name] = fn
        return dr[name]

    xin = din("xT_in", [nseq * D, T])
    din("consts", [128, ncc])
    din("maskc", [128, NMASK])
    din("maskb", [128, NMASKB])
    yout = nc.dram_tensor("yT_out", [nseq * D, T], F32, kind="ExternalOutput").ap()

    P = Prog()
    C = Ctx()
    C.need = need
    C.ccols = ccols
    with ExitStack() as es:
        def sb(name, shape, dt):
            return es.enter_context(nc.sbuf_tensor(name, list(shape), dt))

        C.xT = sb("xT", [128, 8, T], F32)
        C.hT = sb("hT", [128, 8, T], BF16)
        C.consts = sb("consts_sb", [128, ncc], F32)
        C.ones_bf = sb("ones_bf", [128, 128], BF16)
        C.ar = sb("arena", [128, ARENA // 2], BF16)
        C.ar_f = C.ar.bitcast(F32)
        C.ar_i = C.ar.bitcast(I32)
        C.mask = sb("maskc_sb", [128, NMASK], F32)
        C.ident_bf = sb("ident_bf", [128, 128], BF16)
        C.maskb = sb("maskb_sb", [128, NMASKB], BF16)
        C.bd_bf = sb("bd_bf", [128, 128], BF16)
        C.pos = din("pos", [nseq, T], I32)
        C.posT = din("posT", [nseq, 128, 16], I32)
        C.dr = dr
        ffn_alloc(C)
        C.ps = es.enter_context(nc.psum_tensor("ps", [128, 8, 512], F32))
        C.wslot = 0
        C.eps_col = ccols['eps'][0]
        C.sgslot = 0
        C.nbank = 0
        C.pmi = 0

        def bank():
            b = 2 + C.nbank
            C.nbank = (C.nbank + 1) % 6
            return b

        C.bank = bank

        P.dma("sp", lambda eng: eng.dma_start(out=C.consts[:, :], in_=dr["consts"]), "consts", writes=[("consts",)])
        P.dma("sp", lambda eng: eng.dma_start(out=C.mask[:, :], in_=dr["maskc"]), "maskc", writes=[("mask",)])
        P.dma("pool", lambda eng: eng.dma_start(out=C.maskb[:, :], in_=dr["maskb"]), "maskb", writes=[("mask",)])
        P.op("dve", lambda eng: eng.memset(C.ones_bf[:, :], 1.0), writes=[("ones",)])
        P.op("dve", lambda eng: eng.tensor_copy(C.bd_bf[:, :], C.mask[:, 256:384]), reads=[("mask",)], writes=[("bd",)])
        P.op("dve", lambda eng: eng.tensor_copy(C.ident_bf[:, :], C.mask[:, 512:640]), reads=[("mask",)], writes=[("ident",)])
        P.barrier()

        for s in range(nseq):
            load_x(P, C, xin[s * D:(s + 1) * D, :])
            for st in stages:
                kind = st[0]
                if kind == "ffn":
                    _, nm, l = st
                    wg = need(f"{nm}_w_gate{l}", [D, DFF], lambda inp, nm=nm, l=l: inp[f"{nm}_w_gate"][l])
                    wu = need(f"{nm}_w_up{l}", [D, DFF], lambda inp, nm=nm, l=l: inp[f"{nm}_w_up"][l])
                    wd = need(f"{nm}_w_down{l}", [DFF, D], lambda inp, nm=nm, l=l: inp[f"{nm}_w_down"][l])
                    ffn_stage(P, C, ccols[f"{nm}_norm{l}"][0], wg, wu, wd)
                elif kind == "norm":
                    rmsnorm_stage(P, C, ccols[f"mix_norm{st[1]}"][0])
                elif kind == "swa":
                    swa_stage(P, C, s)
                elif kind == "gla":
                    gla_stage(P, C, s)
                elif kind == "ssd":
                    ssd_stage(P, C, s)
                elif kind == "mla":
                    mla_kv_stage(P, C, s)
                    P.barrier()
                    if os.environ.get("DBG_DUMP"):
                        dk = nc.dram_tensor("dbg_k", [128, 8 * T], BF16, kind="ExternalOutput").ap()
                        dv = nc.dram_tensor("dbg_v", [128, 16 * 512], BF16, kind="ExternalOutput").ap()
                        P.dma("sp", lambda e: [e.dma_start(out=dk[:, i * 1024:(i + 1) * 1024], in_=C.ar[:, i * 1024:(i + 1) * 1024]) for i in range(16)], "dbgk", n=16)
                        P.dma("sp", lambda e: [e.dma_start(out=dv[:, i * 1024:(i + 1) * 1024], in_=C.ar[:, 8 * T + i * 1024:8 * T + (i + 1) * 1024]) for i in range(8)], "dbgv", n=8)
                        P.barrier()
                    else:
                        mla_attn_stage(P, C, s)
                else:
                    raise ValueError(kind)
                P.barrier()
            store_x(P, C, yout[s * D:(s + 1) * D, :])
        P.emit(nc)
    return nc, hostprep


ALL_STAGES = [("ffn", "pre", 0), ("norm", 0), ("ssd", 0), ("mla", 0), ("ffn", "post", 0),
              ("ffn", "pre", 1), ("norm", 1), ("swa", 1), ("gla", 1), ("ffn", "post", 1)]


def run(inputs, stages=ALL_STAGES, ncores=8, nseq=4, trace=False):
    x = np.asarray(inputs["x"], np.float32)
    pos = np.asarray(inputs["positions"], np.int32)
    consts, ccols = pack_consts(inputs)
    nc, hostprep = build(nseq, stages, ccols, consts.shape[1])
    mk = make_masks()
    shared = {"consts": consts, "maskc": mk[0], "maskb": mk[1]}
    for name, fn in hostprep.items():
        shared[name] = np.ascontiguousarray(np.asarray(fn(inputs), np.float32))
    in_maps = []
    for c in range(ncores):
        xs = x[c * nseq:(c + 1) * nseq]
        xT = np.ascontiguousarray(xs.transpose(0, 2, 1)).reshape(nseq * D, T)
        ps = np.ascontiguousarray(pos[c * nseq:(c + 1) * nseq])
        pT = np.ascontiguousarray(ps.reshape(nseq, 16, 128).transpose(0, 2, 1))
        m = {"xT_in": xT, "pos": ps, "posT": pT}
        m.update(shared)
        in_maps.append(m)
    res = run_bass_kernel_spmd(nc, in_maps, core_ids=list(range(ncores)), trace=trace)
    outs = []
    for c in range(ncores):
        yT = np.asarray(res.results[c]["yT_out"]).reshape(nseq, D, T)
        outs.append(yT.transpose(0, 2, 1))
    out = np.ascontiguousarray(np.concatenate(outs, axis=0)).astype(np.float32)
    return out, res


def kernel(**inputs):
    out, _ = run(inputs)
    return out
```

```python
import numpy as np
from contextlib import ExitStack
import concourse.bass as bass
import concourse.mybir as mybir
from concourse.bass_utils import run_bass_kernel_spmd

F32 = mybir.dt.float32
BF16 = mybir.dt.bfloat16
I32 = mybir.dt.int32
AF = mybir.ActivationFunctionType
ALU = mybir.AluOpType

D = 1024
T = 2048
DFF = 2816
NCH = DFF // 128
EPS = 1e-6
ENGS = ("pe", "act", "dve", "pool", "sp")
import os
SERIAL = bool(os.environ.get('DBG_SERIAL'))


class Prog:
    def __init__(self):
        self.ops = {e: [] for e in ENGS}
        self.cnt = {e: 0 for e in ENGS}
        self.waited = {e: {} for e in ENGS}
        self.last_w = {}
        self.readers = {}
        self.dcnt = {}

    def _deps(self, eng, reads, writes):
        deps = {}

        def add(s, v):
            if deps.get(s, 0) < v:
                deps[s] = v

        for k in reads:
            if k in self.last_w:
                add(*self.last_w[k])
        for k in writes:
            if k in self.last_w:
                add(*self.last_w[k])
            for s, v in self.readers.get(k, {}).items():
                add(s, v)
        waits = []
        for s, v in deps.items():
            if s == "e:pe" and eng == "pe":
                continue
            if self.waited[eng].get(s, 0) < v:
                self.waited[eng][s] = v
                waits.append((s, v))
        return waits

    def _commit(self, tok, reads, writes):
        for k in writes:
            self.last_w[k] = tok
            self.readers[k] = {}
        for k in reads:
            r = self.readers.setdefault(k, {})
            if r.get(tok[0], 0) < tok[1]:
                r[tok[0]] = tok[1]

    def op(self, eng, fn, reads=(), writes=()):
        waits = self._deps(eng, reads, writes)
        self.cnt[eng] += 1
        tok = ("e:" + eng, self.cnt[eng])
        self.ops[eng].append((waits, fn, tok[0], 1))
        self._commit(tok, reads, writes)
        if SERIAL:
            self.barrier()

    def dma(self, eng, fn, key, reads=(), writes=(), n=1):
        waits = self._deps(eng, reads, writes)
        s = "d:" + key
        self.dcnt[s] = self.dcnt.get(s, 0) + 16 * n
        tok = (s, self.dcnt[s])
        self.ops[eng].append((waits, fn, s, 16))
        self._commit(tok, reads, writes)
        if SERIAL:
            self.barrier()

    def barrier(self):
        for e in ENGS:
            for e2 in ENGS:
                if e2 == "sp" or self.cnt[e2] == 0:
                    continue
                s = "e:" + e2
                v = self.cnt[e2]
                if self.waited[e].get(s, 0) < v:
                    self.waited[e][s] = v
                    self.ops[e].append(([(s, v)], None, None, 0))
            for s, v in self.dcnt.items():
                if self.waited[e].get(s, 0) < v:
                    self.waited[e][s] = v
                    self.ops[e].append(([(s, v)], None, None, 0))

    def emit(self, nc):
        with ExitStack() as es:
            sems = {}
            names = set()
            for e in ENGS:
                for waits, fn, s, inc in self.ops[e]:
                    if s is not None:
                        names.add(s)
                    for w in waits:
                        names.add(w[0])
            for s in sorted(names):
                sems[s] = es.enter_context(nc.semaphore(s.replace(":", "_")))
            block = es.enter_context(nc.Block())

            def run(e):
                def body(eng):
                    for waits, fn, s, inc in self.ops[e]:
                        for ws, wv in waits:
                            eng.wait_ge(sems[ws], wv)
                        if fn is None:
                            continue
                        r = fn(eng)
                        if isinstance(r, (list, tuple)):
                            for ins in r:
                                ins.then_inc(sems[s], inc)
                        else:
                            r.then_inc(sems[s], inc)
                    if e == "sp":
                        for s, v in self.dcnt.items():
                            eng.wait_ge(sems[s], v)

                return body

            block.tensor(run("pe"))
            block.scalar(run("act"))
            block.vector(run("dve"))
            block.gpsimd(run("pool"))
            block.sync(run("sp"))


class Ctx:
    pass


def tsl(j, n=512):
    return slice(j * n, (j + 1) * n)


def fm(v):
    v = np.asarray(v, np.float32)
    return np.ascontiguousarray(v.reshape(-1, 128).T)


def make_masks():
    p = np.arange(128)[:, None]
    f = np.arange(128)[None, :]
    mc = (f >= p)
    mp = (f < p)
    bd = (p // 64 == f // 64)
    gm = bd & (p <= f)
    m64 = np.broadcast_to((np.arange(512)[None, :] % 64 != 0), (128, 512))
    ident = (p == f)
    neg = -30000.0 * mp
    fw = np.arange(896)[None, :]
    mcw = (fw - 384 >= p)
    return (np.ascontiguousarray(np.concatenate([mc, mp, bd, gm, ident, neg], axis=1).astype(np.float32)),
            np.ascontiguousarray(np.concatenate([m64, mcw], axis=1).astype(np.float32)))


NMASK = 768
NMASKB = 512 + 896


def pack_consts(inp):
    cols = {}
    parts = []
    off = 0

    def put(name, arr):
        nonlocal off
        arr = np.asarray(arr, np.float32)
        assert arr.shape[0] == 128
        cols[name] = (off, arr.shape[1])
        parts.append(arr)
        off += arr.shape[1]

    for l in range(2):
        put(f"pre_norm{l}", fm(inp["pre_norm"][l]))
        put(f"mix_norm{l}", fm(inp["mix_norm"][l]))
        put(f"post_norm{l}", fm(inp["post_norm"][l]))
    put("eps", np.full((128, 1), EPS, np.float32))
    put("one", np.ones((128, 1), np.float32))
    put("o_qg2", np.tile(np.asarray(inp["o_q_norm"][0], np.float32), 2)[:, None])
    put("o_kg2", np.tile(np.asarray(inp["o_k_norm"][0], np.float32), 2)[:, None])
    cw = np.asarray(inp["e_conv_w"][0], np.float32)
    put("e_convw", np.ascontiguousarray(cw.T.reshape(16, 128, 4).transpose(1, 0, 2).reshape(128, 64)))
    put("e_convb", fm(inp["e_conv_b"][0]))
    put("e_dtb", np.broadcast_to(np.asarray(inp["e_dt_bias"][0], np.float32)[None, :], (128, 16)))
    put("e_alog", np.broadcast_to(np.asarray(inp["e_a_log"][0], np.float32)[None, :], (128, 16)))
    put("e_dskip", fm(np.repeat(np.asarray(inp["e_d_skip"][0], np.float32), 64)))
    put("e_ssmn", fm(inp["e_ssm_norm"][0]))
    put("e_qan", fm(inp["e_q_a_norm"][0]))
    put("e_kvan", fm(inp["e_kv_a_norm"][0]))
    for nm_, key_ in (("gq", "e_q_norm"), ("gk", "e_k_norm")):
        g96 = np.asarray(inp[key_][0], np.float32)
        col = np.zeros((128, 1), np.float32)
        col[0:96, 0] = g96
        put(nm_, col)
        colp = np.zeros((128, 1), np.float32)
        colp[64:80, 0] = g96[80:96]
        colp[80:96, 0] = g96[64:80]
        put(nm_ + "p", colp)
    invf = np.zeros((128, 1), np.float32)
    fr = (10000.0 ** (-np.arange(16, dtype=np.float64) / 16.0) / (2 * np.pi)).astype(np.float32)
    invf[64:80, 0] = fr
    invf[80:96, 0] = fr
    put("invf", invf)
    sgn = np.zeros((128, 1), np.float32)
    sgn[64:80, 0] = -1.0
    sgn[80:96, 0] = 1.0
    put("sgn", sgn)
    put("o_gbias", fm(inp["o_gate_bias"][0]))
    put("o_glan", np.asarray(inp["o_gla_norm"][0], np.float32)[:, None])
    put("sinks", np.broadcast_to(np.asarray(inp["o_sinks"][0], np.float32)[None, :], (128, 8)))
    put("SL", np.broadcast_to((-8.0 * 2.0 ** (-np.arange(1, 9, dtype=np.float64))).astype(np.float32)[None, :], (128, 8)))
    return np.ascontiguousarray(np.concatenate(parts, axis=1)), cols


def rmsnorm_stage(P, C, gcol):
    sqs = [C.aT[:, 2 * j:2 * j + 2, :].rearrange("p a (b c) -> p (a b) c", b=4) for j in range(4)]
    rstds = [C.sq[:, 2 * j:2 * j + 2, :].bitcast(F32).rearrange("p a c -> p (a c)") if False else None for j in range(4)]
    rstds = [C.rstd4[:, j, :] for j in range(4)]
    banks = {}

    def st_a(j):
        ts = tsl(j)

        def f_sq(e, j=j, ts=ts):
            r = None
            for k in range(8):
                r = e.activation(sqs[j][:, k, :], C.xT[:, k, ts], AF.Square)
            return r

        P.op("act", f_sq, reads=[("xT", d, j) for d in range(8)], writes=[("sq", j)])
        bank = C.bank()
        banks[j] = bank

        def f_mm(eng, bank=bank, j=j):
            r = None
            for k in range(8):
                r = eng.matmul(C.ps[:, bank, :], C.ones_bf[:, :], sqs[j][:, k, :], start=(k == 0), stop=(k == 7))
            return r

        P.op("pe", f_mm, reads=[("sq", j)], writes=[("ps", bank)])

    def st_b(j):
        ts = tsl(j)
        P.op("act", lambda e, j=j, bank=banks[j]: e.activation(rstds[j], C.ps[:, bank, :], AF.Sqrt, bias=C.consts[:, C.eps_col:C.eps_col + 1], scale=1.0 / D),
             reads=[("ps", banks[j])], writes=[("rstd", j)])
        P.op("dve", lambda e, j=j: e.reciprocal(rstds[j], rstds[j]), reads=[("rstd", j)], writes=[("rstd", j)])
        for k in range(8):
            P.op("dve", lambda e, k=k, ts=ts, j=j: e.scalar_tensor_tensor(C.hT[:, k, ts], C.xT[:, k, ts], C.consts[:, gcol + k:gcol + k + 1], rstds[j],
                                                                     ALU.mult, ALU.mult),
                 reads=[("xT", k, j), ("rstd", j)], writes=[("hT", k, j)])

    for j in range(5):
        if j < 4:
            st_a(j)
        if j >= 1:
            st_b(j - 1)


FFN_GROUPS = [(0, 8), (8, 7), (15, 7)]
ARENA = 102 * 1024
import os
DBG_PHASE = int(os.environ.get('DBG_PHASE', '3'))
DBG_NB = int(os.environ.get('DBG_NB', '0'))
DBG_STEP = int(os.environ.get('DBG_STEP', '9'))


class Arena:
    def __init__(self, C):
        self.C = C
        self.off = 0

    def _take(self, n, esz):
        self.off = (self.off + 3) // 4 * 4
        o = self.off
        self.off += n * esz
        assert self.off <= ARENA, self.off
        return o

    def bf(self, *free):
        n = int(np.prod(free))
        o = self._take(n, 2)
        return self._shape(self.C.ar[:, o // 2:o // 2 + n], free)

    def f32(self, *free):
        n = int(np.prod(free))
        o = self._take(n, 4)
        return self._shape(self.C.ar_f[:, o // 4:o // 4 + n], free)

    def i32(self, *free):
        n = int(np.prod(free))
        o = self._take(n, 4)
        return self._shape(self.C.ar_i[:, o // 4:o // 4 + n], free)

    @staticmethod
    def _shape(v, free):
        if len(free) == 1:
            return v
        if len(free) == 2:
            return v.rearrange("p (a b) -> p a b", a=free[0])
        if len(free) == 3:
            return v.rearrange("p (a b c) -> p a b c", a=free[0], b=free[1])
        raise ValueError


def bc(ap, dims):
    return bass.AP(ap.tensor, ap.offset, [list(ap.ap[0])] + [list(d) for d in dims])


def ffn_alloc(C):
    A = Arena(C)
    C.aT = A.bf(8, T)
    C.wgu = [[A.bf(8, 512) for i in range(2)] for s in range(2)]
    C.wd_sb = A.bf(8, D)
    C.sg = [A.f32(512) for s in range(2)]
    C.sq = A.bf(8, 512)
    C.rstd = A.f32(512)
    C.rstd4 = C.ar_f[:, C.sq.offset // 2:C.sq.offset // 2 + 2048].rearrange("p (a b) -> p a b", a=4)


def ffn_stage(P, C, gcol, wg, wu, wd):
    rmsnorm_stage(P, C, gcol)
    wg_v = wg.rearrange("(k p) f -> p k f", p=128)
    wu_v = wu.rearrange("(k p) f -> p k f", p=128)
    wd_v = wd.rearrange("(c p) d -> p c d", p=128)
    for (c0, ng) in FFN_GROUPS:
        def f_wd(eng, c0=c0, ng=ng):
            return eng.dma_start(out=C.wd_sb[:, 0:ng, :], in_=wd_v[:, c0:c0 + ng, :])

        P.dma("pool", f_wd, "wd", writes=[("wd",)])
        pieces = []
        cc = 0
        while cc < ng:
            pn = min(4, ng - cc)
            pieces.append((cc, pn))
            cc += pn
        for (pc, pn) in pieces:
            slot = C.wslot
            C.wslot = (C.wslot + 1) % 2
            f0 = (c0 + pc) * 128

            def f_wg(eng, slot=slot, f0=f0, pn=pn):
                return eng.dma_start(out=C.wgu[slot][0][:, :, 0:pn * 128], in_=wg_v[:, :, f0:f0 + pn * 128])

            def f_wu(eng, slot=slot, f0=f0, pn=pn):
                return eng.dma_start(out=C.wgu[slot][1][:, :, 0:pn * 128], in_=wu_v[:, :, f0:f0 + pn * 128])

            P.dma("pool", f_wg, f"wg{slot}", writes=[("wg", slot)])
            P.dma("pool", f_wu, f"wu{slot}", writes=[("wu", slot)])
            for ci in range(pn):
                ca = pc + ci
                for j in range(4):
                    ts = tsl(j)
                    bg = C.bank()
                    bu = C.bank()

                    def f_mm(eng, slot=slot, ci=ci, ts=ts, bg=bg, bu=bu):
                        r = None
                        for k in range(8):
                            r = eng.matmul(C.ps[:, bg, :], C.wgu[slot][0][:, k, ci * 128:(ci + 1) * 128],
                                           C.hT[:, k, ts], start=(k == 0), stop=(k == 7))
                        for k in range(8):
                            r = eng.matmul(C.ps[:, bu, :], C.wgu[slot][1][:, k, ci * 128:(ci + 1) * 128],
                                           C.hT[:, k, ts], start=(k == 0), stop=(k == 7))
                        return r

                    P.op("pe", f_mm, reads=[("wg", slot), ("wu", slot)] + [("hT", k, j) for k in range(8)],
                         writes=[("ps", bg), ("ps", bu)])
                    ss = C.sgslot
                    C.sgslot = (C.sgslot + 1) % 2

                    def f_silu(eng, ss=ss, bg=bg):
                        return eng.activation(C.sg[ss][:, :], C.ps[:, bg, :], AF.Silu)

                    P.op("act", f_silu, reads=[("ps", bg)], writes=[("sg", ss)])

                    def f_mul(eng, ss=ss, bu=bu, ca=ca, ts=ts):
                        return eng.tensor_tensor(C.aT[:, ca, ts], C.sg[ss][:, :], C.ps[:, bu, :], ALU.mult)

                    P.op("dve", f_mul, reads=[("sg", ss), ("ps", bu)], writes=[("aT", ca, j)])
        for d in range(8):
            for j in range(4):
                ts = tsl(j)
                by = C.bank()

                def f_mm(eng, d=d, ts=ts, by=by, ng=ng):
                    r = None
                    for ca in range(ng):
                        r = eng.matmul(C.ps[:, by, :], C.wd_sb[:, ca, d * 128:(d + 1) * 128], C.aT[:, ca, ts],
                                       start=(ca == 0), stop=(ca == ng - 1))
                    return r

                P.op("pe", f_mm, reads=[("wd",)] + [("aT", ca, j) for ca in range(ng)], writes=[("ps", by)])

                def f_res(eng, d=d, ts=ts, by=by):
                    return eng.scalar_tensor_tensor(C.xT[:, d, ts], C.ps[:, by, :], 0.5, C.xT[:, d, ts],
                                                    ALU.mult, ALU.add)

                P.op("dve", f_res, reads=[("ps", by), ("xT", d, j)], writes=[("xT", d, j)])


def norm_heads64(P, C, bank, gcol_name, out_ap, sq, rstd):
    gc = C.ccols[gcol_name][0]

    def f_sq(eng):
        return eng.activation(sq, C.ps[:, bank, :], AF.Square)

    P.op("act", f_sq, reads=[("ps", bank)], writes=[("nsq",)])
    b2 = C.bank()

    def f_mm(eng):
        return eng.matmul(C.ps[:, b2, :], C.bd_bf[:, :], sq, start=True, stop=True)

    P.op("pe", f_mm, reads=[("nsq",), ("bd",)], writes=[("ps", b2)])

    def f_r1(eng):
        return eng.activation(rstd, C.ps[:, b2, :], AF.Sqrt, bias=C.consts[:, C.eps_col:C.eps_col + 1], scale=1.0 / 64)

    P.op("act", f_r1, reads=[("ps", b2)], writes=[("nrstd",)])

    def f_r2(eng):
        return eng.reciprocal(rstd, rstd)

    P.op("dve", f_r2, reads=[("nrstd",)], writes=[("nrstd",)])

    def f_o(eng):
        return eng.scalar_tensor_tensor(out_ap, C.ps[:, bank, :], C.consts[:, gc:gc + 1], rstd, ALU.mult, ALU.mult)

    return f_o


def swa_stage(P, C, s):
    dr = C.dr
    w_in = C.need("o_w_in", [D, 2320], lambda inp: inp["o_w_in"][0])
    w_out = C.need("o_w_out", [D, D], lambda inp: inp["o_w_out"][0])
    A = Arena(C)
    wqkv = A.bf(8, 768)
    wk2 = A.bf(8, 2, 128)
    woS = A.bf(8, D)
    qT = A.bf(4, 512)
    kT2 = A.bf(2, T)
    Vaug = A.bf(16, 2, 128)
    oT = A.bf(8, 512)
    posB = A.f32(T)
    posBi = bass.AP(C.ar_i[:, 0:1].tensor, posB.offset, [list(posB.ap[0]), [1, T]])
    pk = A.f32(16)
    pki = A.i32(16)
    dist2 = [A.f32(128) for _ in range(2)]
    D82 = [A.f32(8, 128) for _ in range(2)]
    tmp2 = [A.f32(512) for _ in range(2)]
    Pe2 = [A.bf(512) for _ in range(2)]
    Pm = [A.bf(512) for _ in range(4)]
    sq = A.bf(512)
    rstd = A.f32(512)
    dn = A.f32(512)
    dn0 = A.f32(512)
    es = A.f32(8)
    cc = C.ccols
    w_in_v = w_in.rearrange("(k p) f -> p k f", p=128)

    P.dma("pool", lambda e: e.dma_start(out=wqkv, in_=w_in_v[:, :, 0:768]), "wA", writes=[("wqkv",)])

    def f_wk(e):
        r = []
        for g in range(2):
            for hh in range(2):
                r.append(e.dma_start(out=wk2[:, :, g, hh * 64:(hh + 1) * 64], in_=w_in_v[:, :, 512 + g * 64:512 + (g + 1) * 64]))
        return r

    P.dma("pool", f_wk, "wB", writes=[("wk2",)], n=4)
    P.dma("pool", lambda e: e.dma_start(out=woS[0:64, :, :], in_=w_out[0:512, :].rearrange("(h p) d -> p h d", p=64)),
          "wC", writes=[("woS",)])
    P.dma("sp", lambda e: e.dma_start(out=posBi, in_=bass.AP(C.pos.tensor, C.pos[s:s + 1, :].offset, [[0, 128], [1, T]])),
          "posB", writes=[("posB",)])
    P.dma("sp", lambda e: e.dma_start(out=pki, in_=C.posT[s]), "pk", writes=[("pki",)])
    P.op("dve", lambda e: e.tensor_copy(posB, posBi), reads=[("posB",)], writes=[("posB",)])
    P.op("dve", lambda e: e.tensor_copy(pk, pki), reads=[("pki",)], writes=[("pk",)])
    P.op("act", lambda e: e.activation(es, C.consts[:, cc["sinks"][0]:cc["sinks"][0] + 8], AF.Exp), reads=[("consts",)], writes=[("es",)])
    P.op("pool", lambda e: e.memset(Vaug[:, :, :, 64:128], 1.0), writes=[("Vaug", n) for n in range(16)])
    SLc = cc["SL"][0]

    for j in range(4):
        ts = tsl(j)
        for c in range(4):
            b = C.bank()

            def f_mm(eng, c=c, b=b, ts=ts):
                r = None
                for k in range(8):
                    r = eng.matmul(C.ps[:, b, :], wqkv[:, k, c * 128:(c + 1) * 128], C.hT[:, k, ts], start=(k == 0), stop=(k == 7))
                return r

            P.op("pe", f_mm, reads=[("wqkv",)] + [("hT", k, j) for k in range(8)], writes=[("ps", b)])
            f_o = norm_heads64(P, C, b, "o_qg2", qT[:, c, :], sq, rstd)
            P.op("dve", f_o, reads=[("ps", b), ("nrstd",)], writes=[("qT", c)])
        for g in range(2):
            b = C.bank()

            def f_mm(eng, g=g, b=b, ts=ts):
                r = None
                for k in range(8):
                    r = eng.matmul(C.ps[:, b, :], wk2[:, k, g, :], C.hT[:, k, ts], start=(k == 0), stop=(k == 7))
                return r

            P.op("pe", f_mm, reads=[("wk2",)] + [("hT", k, j) for k in range(8)], writes=[("ps", b)])
            f_o = norm_heads64(P, C, b, "o_kg2", kT2[:, g, ts], sq, rstd)
            P.op("dve", f_o, reads=[("ps", b), ("nrstd",)], writes=[("kT2", g, j)])
        for nl in range(4):
            n = 4 * j + nl
            b = C.bank()

            def f_mm(eng, n=n, b=b):
                r = None
                for k in range(8):
                    r = eng.matmul(C.ps[:, b, 0:128], C.hT[:, k, n * 128:(n + 1) * 128], wqkv[:, k, 640:768], start=(k == 0), stop=(k == 7))
                return r

            P.op("pe", f_mm, reads=[("wqkv",)] + [("hT", k, j) for k in range(8)], writes=[("ps", b)])

            def f_v(eng, n=n, b=b):
                return eng.tensor_copy(Vaug[:, n, :, 0:64], C.ps[:, b, 0:128].rearrange("p (g d) -> p g d", g=2))

            P.op("dve", f_v, reads=[("ps", b)], writes=[("Vaug", n)])
        items = []
        for nl in range(4):
            n = 4 * j + nl
            kbs = [n - 1, n] if n > 0 else [n]
            for kb in kbs:
                for g in range(2):
                    items.append((nl, n, kb, g, kb == kbs[0], kb == n))
        di = [0]
        cur = {}

        def s1(nl, n, kb, g, first, last):
            qs = slice(nl * 128, (nl + 1) * 128)
            if g == 0:
                k2 = di[0]
                di[0] = (di[0] + 1) % 2
                cur["k2"] = k2
                dd, d8 = dist2[k2], D82[k2]
                P.op("dve", lambda e, n=n, kb=kb, dd=dd: e.tensor_scalar(dd, posB[:, n * 128:(n + 1) * 128], pk[:, kb:kb + 1], None, ALU.subtract),
                     reads=[("posB",), ("pk",)], writes=[("dist", k2)])
                P.op("dve", lambda e, dd=dd: e.scalar_tensor_tensor(dd, dd, -1.0, dd, ALU.mult, ALU.max), reads=[("dist", k2)], writes=[("dist", k2)])
                P.op("pool", lambda e, dd=dd, d8=d8: e.tensor_tensor(d8, bc(dd, [[0, 8], [1, 128]]), bc(C.consts[:, SLc:SLc + 8], [[1, 8], [0, 128]]), ALU.mult),
                     reads=[("dist", k2), ("consts",)], writes=[("D8", k2)])
            k2 = cur["k2"]
            d8 = D82[k2]
            bA = C.bank()
            bB = C.bank()

            def f_sc(eng, g=g, bA=bA, bB=bB, kb=kb, qs=qs):
                r = None
                for hl in range(4):
                    c = 2 * g + hl // 2
                    hp = slice((hl % 2) * 64, (hl % 2) * 64 + 64)
                    bb = bA if hl % 2 == 0 else bB
                    r = eng.matmul(C.ps[:, bb, (hl // 2) * 128:(hl // 2 + 1) * 128], kT2[hp, g, kb * 128:(kb + 1) * 128],
                                   qT[hp, c, qs], start=True, stop=True)
                return r

            P.op("pe", f_sc, reads=[("kT2", g, kb // 4), ("qT", 2 * g), ("qT", 2 * g + 1)], writes=[("ps", bA), ("ps", bB)])
            ti = C.pmi % 2
            TM, PE_ = tmp2[ti], Pe2[ti]
            for par, bb in ((0, bA), (1, bB)):
                P.op("dve", lambda e, g=g, bb=bb, par=par, TM=TM, d8=d8: e.tensor_tensor(TM.rearrange("p (h f) -> p h f", h=4)[:, par:4:2, :],
                                                                                  d8[:, 4 * g + par:4 * g + 4:2, :],
                                                                                  C.ps[:, bb, 0:256].rearrange("p (h f) -> p h f", h=2), ALU.add),
                     reads=[("D8", k2), ("ps", bb)], writes=[("tmp", ti)])
            P.op("act", lambda e, TM=TM, PE_=PE_: e.activation(PE_, TM, AF.Exp, scale=0.125), reads=[("tmp", ti)], writes=[("Pe", ti)])
            pi = C.pmi
            C.pmi = (C.pmi + 1) % 4
            mcol = 0 if kb == n else 128
            P.op("pool", lambda e, pi=pi, mcol=mcol, PE_=PE_: e.tensor_tensor(Pm[pi].rearrange("p (h f) -> p h f", h=4), PE_.rearrange("p (h f) -> p h f", h=4),
                                                                        bc(C.mask[:, mcol:mcol + 128], [[0, 4], [1, 128]]), ALU.mult),
                 reads=[("Pe", ti), ("mask",)], writes=[("Pm", pi)])
            return pi

        def s2(item, pi):
            nl, n, kb, g, first, last = item
            qs = slice(nl * 128, (nl + 1) * 128)
            P.op("pe", lambda e, g=g, kb=kb, pi=pi, first=first, last=last: e.matmul(C.ps[:, g, :], Vaug[:, kb, g, :], Pm[pi], start=first, stop=last),
                 reads=[("Vaug", kb), ("Pm", pi)], writes=[("ps", g)])
            if last:
                def f_dn(eng, g=g):
                    return eng.tensor_tensor(dn[64:128, :].rearrange("p (h f) -> p h f", h=4),
                                             C.ps[64:128, g, :].rearrange("p (h f) -> p h f", h=4),
                                             bc(es[64:128, 4 * g:4 * g + 4], [[1, 4], [0, 128]]), ALU.add)

                P.op("dve", f_dn, reads=[("ps", g), ("es",)], writes=[("dn",)])
                P.op("dve", lambda e: e.reciprocal(dn[64:128, :], dn[64:128, :]), reads=[("dn",)], writes=[("dn",)])
                P.op("dve", lambda e: e.tensor_copy(dn0[0:64, :], dn[64:128, :]), reads=[("dn",)], writes=[("dn0",)])

                def f_o(eng, g=g, qs=qs):
                    return eng.tensor_tensor(oT[0:64, 4 * g:4 * g + 4, qs], C.ps[0:64, g, :].rearrange("p (h f) -> p h f", h=4),
                                             dn0[0:64, :].rearrange("p (h f) -> p h f", h=4), ALU.mult)

                P.op("dve", f_o, reads=[("ps", g), ("dn0",)], writes=[("oT",)])

        pend = []
        for it in items:
            pend.append((it, s1(*it)))
            if len(pend) > 2:
                s2(*pend.pop(0))
        while pend:
            s2(*pend.pop(0))
        for d in range(8 if DBG_PHASE >= 3 else 0):
            b = C.bank()

            def f_mm(eng, d=d, b=b):
                r = None
                for h in range(8):
                    r = eng.matmul(C.ps[:, b, :], woS[0:64, h, d * 128:(d + 1) * 128], oT[0:64, h, :], start=(h == 0), stop=(h == 7))
                return r

            P.op("pe", f_mm, reads=[("woS",), ("oT",)], writes=[("ps", b)])

            def f_res(eng, d=d, b=b, ts=ts):
                return eng.tensor_tensor(C.xT[:, d, ts], C.ps[:, b, :], C.xT[:, d, ts], ALU.add)

            P.op("dve", f_res, reads=[("ps", b), ("xT", d, j)], writes=[("xT", d, j)])


def gla_stage(P, C, s):
    w_in = C.need("o_w_in", [D, 2320], lambda inp: inp["o_w_in"][0])
    w_out = C.need("o_w_out", [D, D], lambda inp: inp["o_w_out"][0])
    w_gb = C.need("o_w_gate_b", [16, 256], lambda inp: inp["o_w_gate_b"][0])
    cc = C.ccols
    A = Arena(C)
    wG = A.bf(8, 1552)
    wgb = A.bf(256)
    woG = A.bf(4, D)
    gaT = A.bf(512)
    ebuf = A.f32(512)
    lbuf = A.f32(512)
    cl = A.f32(2, 512)
    eb = A.f32(512)
    einv = A.f32(512)
    dend = A.f32(512)
    decs = A.f32(2, 8)
    nb = A.f32(2)
    q_dec = A.bf(2, 512)
    k_inv = A.bf(2, 512)
    k_end = A.bf(2, 512)
    grs = A.bf(4, 512)
    gv_tok = A.bf(4, 512)
    ket = A.bf(4, 2, 128)
    attm = A.bf(4, 128)
    S = [A.f32(128) for _ in range(2)]
    S_bf = [A.bf(128) for _ in range(2)]
    sq = A.bf(256)
    rstd = A.f32(256)
    ytmp = A.f32(256)
    oG = A.bf(4, 512)
    w_in_v = w_in.rearrange("(k p) f -> p k f", p=128)
    onec = C.consts[:, cc["one"][0]:cc["one"][0] + 1]
    glan = C.consts[:, cc["o_glan"][0]:cc["o_glan"][0] + 1]
    gbc = cc["o_gbias"][0]

    P.dma("pool", lambda e: e.dma_start(out=wG, in_=w_in_v[:, :, 768:2320]), "wA", writes=[("wG",)])
    P.dma("pool", lambda e: e.dma_start(out=wgb[0:16, :], in_=w_gb), "wB", writes=[("wgb",)])
    P.dma("pool", lambda e: e.dma_start(out=woG, in_=w_out[512:1024, :].rearrange("(c p) d -> p c d", p=128)), "wC", writes=[("woG",)])
    P.op("dve", lambda e: e.tensor_scalar(nb, C.consts[:, gbc:gbc + 2], -1.0, None, ALU.mult), reads=[("consts",)], writes=[("nb",)])
    for cp in range(2):
        P.op("dve", lambda e, cp=cp: e.memset(S[cp], 0.0), writes=[("S", cp)])
        P.op("dve", lambda e, cp=cp: e.memset(S_bf[cp], 0.0), writes=[("Sbf", cp)])

    def proj(cols, j, M=128):
        b = C.bank()
        ts = tsl(j)

        def f(eng):
            r = None
            for k in range(8):
                r = eng.matmul(C.ps[0:M, b, :], wG[:, k, cols], C.hT[:, k, ts], start=(k == 0), stop=(k == 7))
            return r

        P.op("pe", f, reads=[("wG",)] + [("hT", k, j) for k in range(8)], writes=[("ps", b)])
        return b

    for j in range(4):
        ts = tsl(j)
        b = proj(slice(1024, 1040), j, M=16)
        P.op("act", lambda e, b=b: e.copy(gaT[0:16, :], C.ps[0:16, b, :]), reads=[("ps", b)], writes=[("gaT",)])
        for cp in range(2):
            b = C.bank()
            P.op("pe", lambda e, b=b, cp=cp: e.matmul(C.ps[:, b, :], wgb[0:16, cp * 128:(cp + 1) * 128], gaT[0:16, :], start=True, stop=True),
                 reads=[("wgb",), ("gaT",)], writes=[("ps", b)])
            P.op("act", lambda e, b=b, cp=cp: e.activation(ebuf, C.ps[:, b, :], AF.Exp, bias=nb[:, cp:cp + 1], scale=-1.0),
                 reads=[("ps", b), ("nb",)], writes=[("ebuf",)])
            P.op("act", lambda e: e.activation(lbuf, ebuf, AF.Ln, bias=onec, scale=1.0), reads=[("ebuf",)], writes=[("lbuf",)])
            P.op("dve", lambda e, cp=cp: e.tensor_tensor_scan(cl[:, cp, :], C.maskb[:, 0:512], lbuf, 0.0, ALU.mult, ALU.add),
                 reads=[("lbuf",), ("mask",)], writes=[("cl", cp)])
            P.op("act", lambda e, cp=cp: e.activation(eb, cl[:, cp, :], AF.Exp, scale=-1.0 / 16), reads=[("cl", cp)], writes=[("eb",)])
            P.op("act", lambda e, cp=cp: e.activation(einv, cl[:, cp, :], AF.Exp, scale=1.0 / 16), reads=[("cl", cp)], writes=[("einv",)])
            clv = cl[:, cp, :]
            clast_b = bc(clv[:, 63:64], [[64, 8], [0, 64]])
            clast = bc(clv[:, 63:64], [[64, 8]])
            P.op("dve", lambda e, cp=cp, clast_b=clast_b: e.tensor_tensor(dend.rearrange("p (c l) -> p c l", c=8),
                                                                       cl[:, cp, :].rearrange("p (c l) -> p c l", c=8), clast_b, ALU.subtract),
                 reads=[("cl", cp)], writes=[("dend",)])
            P.op("act", lambda e: e.activation(dend, dend, AF.Exp, scale=1.0 / 16), reads=[("dend",)], writes=[("dend",)])
            P.op("act", lambda e, cp=cp, clast=clast: e.activation(decs[:, cp, :], clast, AF.Exp, scale=-1.0 / 16),
                 reads=[("cl", cp)], writes=[("decs", cp)])
            b = proj(slice(cp * 128, (cp + 1) * 128), j)
            P.op("dve", lambda e, b=b, cp=cp: e.scalar_tensor_tensor(q_dec[:, cp, :], C.ps[:, b, :], 0.125, eb, ALU.mult, ALU.mult),
                 reads=[("ps", b), ("eb",)], writes=[("q_dec", cp)])
            b = proj(slice(256 + cp * 128, 256 + (cp + 1) * 128), j)
            P.op("dve", lambda e, b=b, cp=cp: e.tensor_tensor(k_inv[:, cp, :], C.ps[:, b, :], einv, ALU.mult),
                 reads=[("ps", b), ("einv",)], writes=[("k_inv", cp)])
            P.op("dve", lambda e, b=b, cp=cp: e.tensor_tensor(k_end[:, cp, :], C.ps[:, b, :], dend, ALU.mult),
                 reads=[("ps", b), ("dend",)], writes=[("k_end", cp)])
        for hh in range(4):
            b = proj(slice(1040 + hh * 128, 1040 + (hh + 1) * 128), j)
            P.op("act", lambda e, b=b, hh=hh: e.activation(grs[:, hh, :], C.ps[:, b, :], AF.Silu), reads=[("ps", b)], writes=[("grs", hh)])
        for nl in range(4):
            n = 4 * j + nl
            b = C.bank()

            def f_gv(eng, b=b, n=n):
                r = None
                for k in range(8):
                    r = eng.matmul(C.ps[:, b, :], C.hT[:, k, n * 128:(n + 1) * 128], wG[:, k, 512:1024], start=(k == 0), stop=(k == 7))
                return r

            P.op("pe", f_gv, reads=[("wG",)] + [("hT", k, j) for k in range(8)], writes=[("ps", b)])
            P.op("act", lambda e, b=b, nl=nl: e.copy(gv_tok[:, nl, :], C.ps[:, b, :]), reads=[("ps", b)], writes=[("gv", nl)])
            for cp in range(2):
                b = C.bank()
                P.op("pe", lambda e, b=b, cp=cp, nl=nl: e.matmul(C.ps[:, b, 0:128], k_end[:, cp, nl * 128:(nl + 1) * 128], C.ident_bf[:, :], start=True, stop=True),
                     reads=[("k_end", cp), ("ident",)], writes=[("ps", b)])
                P.op("dve", lambda e, b=b, cp=cp, nl=nl: e.tensor_copy(ket[:, nl, cp, :], C.ps[:, b, 0:128]), reads=[("ps", b)], writes=[("ket", nl, cp)])
        for nl in range(4):
            bs = slice(nl * 128, (nl + 1) * 128)
            bX = C.bank()
            bY = C.bank()

            def f_att(eng, bX=bX, bY=bY, bs=bs):
                r = None
                for h in range(4):
                    cp, half = h // 2, h % 2
                    hp = slice(half * 64, half * 64 + 64)
                    bb = bX if half == 0 else bY
                    r = eng.matmul(C.ps[:, bb, cp * 128:(cp + 1) * 128], k_inv[hp, cp, bs], q_dec[hp, cp, bs], start=True, stop=True)
                return r

            P.op("pe", f_att, reads=[("k_inv", 0), ("k_inv", 1), ("q_dec", 0), ("q_dec", 1)], writes=[("ps", bX), ("ps", bY)])
            for half, bb in ((0, bX), (1, bY)):
                P.op("dve", lambda e, half=half, bb=bb: e.tensor_tensor(attm[:, half:4:2, :], C.ps[:, bb, 0:256].rearrange("p (h f) -> p h f", h=2),
                                                                   bc(C.mask[:, 384:512], [[0, 2], [1, 128]]), ALU.mult),
                     reads=[("ps", bb), ("mask",)], writes=[("attm", half)])
            for x in range(2):
                xs = slice(nl * 128 + x * 64, nl * 128 + x * 64 + 64)
                rows = slice(x * 64, x * 64 + 64)
                ci = nl * 2 + x

                def f_inter(eng, xs=xs, x=x):
                    r = None
                    for h in range(4):
                        cp, half = h // 2, h % 2
                        hp = slice(half * 64, half * 64 + 64)
                        r = eng.matmul(C.ps[:, half, cp * 128 + x * 64:cp * 128 + x * 64 + 64], S_bf[cp][hp, :], q_dec[hp, cp, xs],
                                       start=(x == 0 and cp == 0), stop=False, skip_group_check=True)
                    return r

                P.op("pe", f_inter, reads=[("Sbf", 0), ("Sbf", 1), ("q_dec", 0), ("q_dec", 1)], writes=[("ps", 0), ("ps", 1)])
                for cp in range(2):
                    bk = C.bank()
                    P.op("pe", lambda e, bk=bk, cp=cp, rows=rows, nl=nl: e.matmul(C.ps[:, bk, 0:256], ket[rows, nl, cp, :],
                                                                            gv_tok[rows, nl, cp * 256:(cp + 1) * 256], start=True, stop=True),
                         reads=[("ket", nl, cp), ("gv", nl)], writes=[("ps", bk)])
                    for half in range(2):
                        hp = slice(half * 64, half * 64 + 64)
                        P.op("dve", lambda e, bk=bk, cp=cp, hp=hp, half=half, ci=ci: e.scalar_tensor_tensor(
                            S[cp][hp, :], S[cp][hp, :], decs[hp, cp, ci:ci + 1], C.ps[hp, bk, half * 128:(half + 1) * 128], ALU.mult, ALU.add),
                             reads=[("ps", bk), ("decs", cp), ("S", cp)], writes=[("S", cp)])
                    P.op("act", lambda e, cp=cp: e.copy(S_bf[cp], S[cp]), reads=[("S", cp)], writes=[("Sbf", cp)])

            def f_intra(eng, nl=nl):
                r = None
                for h in range(4):
                    cp, half = h // 2, h % 2
                    r = eng.matmul(C.ps[:, half, cp * 128:(cp + 1) * 128], gv_tok[:, nl, h * 128:(h + 1) * 128], attm[:, h, :], start=False, stop=True,
                                   skip_group_check=True)
                return r

            P.op("pe", f_intra, reads=[("gv", nl), ("attm", 0), ("attm", 1)], writes=[("ps", 0), ("ps", 1)])
            for half in range(2):
                P.op("act", lambda e, half=half: e.activation(sq, C.ps[:, half, 0:256], AF.Square), reads=[("ps", half)], writes=[("gsq",)])
                b2 = C.bank()
                P.op("pe", lambda e, b2=b2: e.matmul(C.ps[:, b2, 0:256], C.ones_bf[:, :], sq, start=True, stop=True), reads=[("gsq",), ("ones",)], writes=[("ps", b2)])
                P.op("act", lambda e, b2=b2: e.activation(rstd, C.ps[:, b2, 0:256], AF.Sqrt, bias=C.consts[:, C.eps_col:C.eps_col + 1], scale=1.0 / 128),
                     reads=[("ps", b2)], writes=[("grstd",)])
                P.op("dve", lambda e: e.reciprocal(rstd, rstd), reads=[("grstd",)], writes=[("grstd",)])
                P.op("dve", lambda e, half=half: e.scalar_tensor_tensor(ytmp, C.ps[:, half, 0:256], glan, rstd, ALU.mult, ALU.mult),
                     reads=[("ps", half), ("grstd",)], writes=[("ytmp",)])
                P.op("dve", lambda e, half=half, bs=bs: e.tensor_tensor(oG[:, half:4:2, bs], ytmp.rearrange("p (h f) -> p h f", h=2),
                                                                   grs[:, half:4:2, bs], ALU.mult),
                     reads=[("ytmp",), ("grs", half), ("grs", half + 2)], writes=[("oG",)])
        for d in range(8):
            b = C.bank()

            def f_mm(eng, d=d, b=b):
                r = None
                for c in range(4):
                    r = eng.matmul(C.ps[:, b, :], woG[:, c, d * 128:(d + 1) * 128], oG[:, c, :], start=(c == 0), stop=(c == 3))
                return r

            P.op("pe", f_mm, reads=[("woG",), ("oG",)], writes=[("ps", b)])
            P.op("dve", lambda e, d=d, b=b, ts=ts: e.tensor_tensor(C.xT[:, d, ts], C.ps[:, b, :], C.xT[:, d, ts], ALU.add),
                 reads=[("ps", b), ("xT", d, j)], writes=[("xT", d, j)])


def ssd_stage(P, C, s):
    w_in = C.need("e_w_in", [D, 3760], lambda inp: inp["e_w_in"][0])
    w_out = C.need("e_w_out", [1536, D], lambda inp: inp["e_w_out"][0])
    cc = C.ccols
    A = Arena(C)
    wp = [A.bf(8, 256) for _ in range(2)]
    wdt = A.bf(8, 16)
    woS = A.bf(8, D)
    zs = A.bf(8, 512)
    xsT = A.bf(8, 512)
    BT = A.bf(4, 512)
    CT = A.bf(4, 512)
    rb = [A.f32(515) for _ in range(2)]
    acc = [A.f32(512) for _ in range(2)]
    halo = A.f32(16, 3)
    aneg = A.f32(16)
    x1s = [A.f32(16) for _ in range(2)]
    dts = [A.f32(16) for _ in range(2)]
    a_s = [A.f32(16) for _ in range(2)]
    csts = [A.f32(16) for _ in range(2)]
    ncss = [A.f32(16) for _ in range(2)]
    dss = [A.f32(16) for _ in range(2)]
    cdecs = [A.f32(16) for _ in range(2)]
    dtdss = [A.f32(16) for _ in range(2)]
    xds = [A.bf(1024) for _ in range(2)]
    xdws = [A.bf(1024) for _ in range(2)]
    B_toks = [A.bf(4, 128) for _ in range(2)]
    CBss = [A.f32(4, 128) for _ in range(2)]
    Ecs = [A.f32(128) for _ in range(3)]
    E = [A.f32(128) for _ in range(3)]
    Coff = [A.bf(128) for _ in range(3)]
    Mh = [A.bf(128) for _ in range(3)]
    st = A.f32(1024)
    st_bf = A.bf(1024)
    yg = A.f32(8, 128)
    sq = A.bf(8, 128)
    rstd = A.f32(512)
    oS = A.bf(8, 512)
    w_in_v = w_in.rearrange("(k p) f -> p k f", p=128)
    onec = C.consts[:, cc["one"][0]:cc["one"][0] + 1]
    epsc = C.consts[:, C.eps_col:C.eps_col + 1]
    cwc, cbc, dsk, ssn = cc["e_convw"][0], cc["e_convb"][0], cc["e_dskip"][0], cc["e_ssmn"][0]
    MC = C.mask[:, 0:128]
    NEG = C.mask[:, 640:768]
    IDf = C.mask[:, 512:640]

    P.dma("pool", lambda e: e.dma_start(out=wdt, in_=w_in_v[:, :, 3072:3088]), "wB", writes=[("wdt",)])
    P.dma("pool", lambda e: e.dma_start(out=woS, in_=w_out[0:1024, :].rearrange("(c p) d -> p c d", p=128)), "wC", writes=[("woS",)])
    P.op("act", lambda e: e.activation(aneg, C.consts[:, cc["e_alog"][0]:cc["e_alog"][0] + 16], AF.Exp), reads=[("consts",)], writes=[("aneg",)])
    P.op("dve", lambda e: e.tensor_scalar(aneg, aneg, -1.0, None, ALU.mult), reads=[("aneg",)], writes=[("aneg",)])
    P.op("dve", lambda e: e.memset(st, 0.0), writes=[("st",)])
    P.op("dve", lambda e: e.memset(st_bf, 0.0), writes=[("st_bf",)])
    P.op("dve", lambda e: e.memset(halo, 0.0), writes=[("halo",)])
    ws = [0]
    rbi = [0]

    for j in range(4):
        ts = tsl(j)
        for pi in range(12):
            slot = ws[0]
            ws[0] = (ws[0] + 1) % 2
            P.dma("pool", lambda e, slot=slot, pi=pi: e.dma_start(out=wp[slot], in_=w_in_v[:, :, pi * 256:(pi + 1) * 256]),
                  f"wp{slot}", writes=[("wp", slot)])
            for ci in range(2):
                c = pi * 2 + ci
                b = C.bank()

                def f_mm(eng, slot=slot, ci=ci, b=b, ts=ts):
                    r = None
                    for k in range(8):
                        r = eng.matmul(C.ps[:, b, :], wp[slot][:, k, ci * 128:(ci + 1) * 128], C.hT[:, k, ts], start=(k == 0), stop=(k == 7))
                    return r

                P.op("pe", f_mm, reads=[("wp", slot)] + [("hT", k, j) for k in range(8)], writes=[("ps", b)])
                if c < 8:
                    P.op("act", lambda e, b=b, c=c: e.activation(zs[:, c, :], C.ps[:, b, :], AF.Silu), reads=[("ps", b)], writes=[("zs", c)])
                    continue
                xc = c - 8
                ri = rbi[0]
                rbi[0] = (rbi[0] + 1) % 2
                R, AC = rb[ri], acc[ri]
                P.op("act", lambda e, b=b, R=R: e.copy(R[:, 3:515], C.ps[:, b, :]), reads=[("ps", b)], writes=[("rb", ri)])
                P.op("dve", lambda e, R=R, xc=xc: e.tensor_copy(R[:, 0:3], halo[:, xc, :]), reads=[("halo",), ("rb", ri)], writes=[("rb", ri)])

                def wcol(xc, t):
                    return C.consts[:, cwc + xc * 4 + t:cwc + xc * 4 + t + 1]

                P.op("dve", lambda e, R=R, AC=AC, xc=xc: e.tensor_scalar(AC, R[:, 3:515], wcol(xc, 3), None, ALU.mult),
                     reads=[("rb", ri)], writes=[("acc", ri)])
                for t in (2, 1, 0):
                    P.op("dve", lambda e, R=R, AC=AC, xc=xc, t=t: e.scalar_tensor_tensor(AC, R[:, t:t + 512], wcol(xc, t), AC, ALU.mult, ALU.add),
                         reads=[("rb", ri), ("acc", ri)], writes=[("acc", ri)])
                P.op("dve", lambda e, R=R, xc=xc: e.tensor_copy(halo[:, xc, :], R[:, 512:515]), reads=[("rb", ri), ("halo",)], writes=[("halo",)])
                if xc < 8:
                    dest, key = xsT[:, xc, :], ("xsT", xc)
                elif xc < 12:
                    dest, key = BT[:, xc - 8, :], ("BT", xc - 8)
                else:
                    dest, key = CT[:, xc - 12, :], ("CT", xc - 12)
                P.op("act", lambda e, AC=AC, dest=dest, xc=xc: e.activation(dest, AC, AF.Silu, bias=C.consts[:, cbc + xc:cbc + xc + 1], scale=1.0),
                     reads=[("acc", ri)], writes=[key])
        def prologue(nl):
            pp = nl % 2
            n = 4 * j + nl
            bs = slice(nl * 128, (nl + 1) * 128)
            x1, dt, a_, cst, ncs, ds, cdec, dtds = (x1s[pp], dts[pp], a_s[pp], csts[pp], ncss[pp], dss[pp], cdecs[pp], dtdss[pp])
            xd, xdw, B_tok, CBs = xds[pp], xdws[pp], B_toks[pp], CBss[pp]
            b = C.bank()

            def f_dt(eng, b=b, n=n):
                r = None
                for k in range(8):
                    r = eng.matmul(C.ps[:, b, 0:16], C.hT[:, k, n * 128:(n + 1) * 128], wdt[:, k, :], start=(k == 0), stop=(k == 7))
                return r

            P.op("pe", f_dt, reads=[("wdt",)] + [("hT", k, j) for k in range(8)], writes=[("ps", b)])
            yield
            P.op("dve", lambda e, b=b: e.tensor_tensor(x1, C.ps[:, b, 0:16], C.consts[:, cc["e_dtb"][0]:cc["e_dtb"][0] + 16], ALU.add),
                 reads=[("ps", b)], writes=[("x1", pp)])
            yield
            P.op("act", lambda e: e.activation(x1, x1, AF.Exp), reads=[("x1", pp)], writes=[("x1", pp)])
            P.op("act", lambda e: e.activation(dt, x1, AF.Ln, bias=onec, scale=1.0), reads=[("x1", pp)], writes=[("dt", pp)])
            yield
            P.op("dve", lambda e: e.tensor_tensor(a_, dt, aneg, ALU.mult), reads=[("dt", pp), ("aneg",)], writes=[("a", pp)])
            yield
            b = C.bank()

            def f_cs(eng, b=b):
                eng.matmul(C.ps[:, b, 0:16], MC, a_, start=True, stop=True)
                return eng.matmul(C.ps[:, b, 16:32], bc(onec, [[0, 128]]), a_, start=True, stop=True)

            P.op("pe", f_cs, reads=[("a", pp), ("mask",)], writes=[("ps", b)])
            yield
            P.op("dve", lambda e, b=b: e.tensor_copy(cst, C.ps[:, b, 0:16]), reads=[("ps", b)], writes=[("cst", pp)])
            P.op("dve", lambda e: e.tensor_scalar(ncs, cst, -1.0, None, ALU.mult), reads=[("cst", pp)], writes=[("ncs", pp)])
            yield
            P.op("dve", lambda e, b=b: e.tensor_tensor(ds, C.ps[:, b, 16:32], cst, ALU.subtract), reads=[("ps", b), ("cst", pp)], writes=[("ds", pp)])
            yield
            P.op("act", lambda e: e.activation(ds, ds, AF.Exp), reads=[("ds", pp)], writes=[("ds", pp)])
            P.op("act", lambda e, b=b: e.activation(cdec, C.ps[:, b, 16:32], AF.Exp), reads=[("ps", b)], writes=[("cdec", pp)])
            yield
            P.op("dve", lambda e: e.tensor_tensor(dtds, dt, ds, ALU.mult), reads=[("dt", pp), ("ds", pp)], writes=[("dtds", pp)])
            yield
            for hb in range(2):
                b = C.bank()

                def f_tr(eng, b=b, hb=hb, bs=bs):
                    r = None
                    for cq in range(4):
                        c = hb * 4 + cq
                        r = eng.matmul(C.ps[:, b, cq * 128:(cq + 1) * 128], xsT[:, c, bs], C.ident_bf[:, :], start=True, stop=True)
                    return r

                P.op("pe", f_tr, reads=[("xsT", hb * 4 + q) for q in range(4)] + [("ident",)], writes=[("ps", b)])
                yield
                P.op("dve", lambda e, b=b, hb=hb: e.tensor_tensor(xd[:, hb * 512:(hb + 1) * 512].rearrange("p (h q) -> p h q", h=8),
                                                             C.ps[:, b, :].rearrange("p (h q) -> p h q", h=8),
                                                             bc(dt[:, hb * 8:hb * 8 + 8], [[1, 8], [0, 64]]), ALU.mult),
                     reads=[("ps", b), ("dt", pp)], writes=[("xd", pp, hb)])
                yield
                P.op("dve", lambda e, b=b, hb=hb: e.tensor_tensor(xdw[:, hb * 512:(hb + 1) * 512].rearrange("p (h q) -> p h q", h=8),
                                                             C.ps[:, b, :].rearrange("p (h q) -> p h q", h=8),
                                                             bc(dtds[:, hb * 8:hb * 8 + 8], [[1, 8], [0, 64]]), ALU.mult),
                     reads=[("ps", b), ("dtds", pp)], writes=[("xdw", pp, hb)])
                yield
            b = C.bank()

            def f_trb(eng, b=b, bs=bs):
                r = None
                for g in range(4):
                    r = eng.matmul(C.ps[:, b, g * 128:(g + 1) * 128], BT[:, g, bs], C.ident_bf[:, :], start=True, stop=True)
                return r

            P.op("pe", f_trb, reads=[("BT", g) for g in range(4)] + [("ident",)], writes=[("ps", b)])
            yield
            P.op("act", lambda e, b=b: e.copy(B_tok.rearrange("p g n -> p (g n)"), C.ps[:, b, :]), reads=[("ps", b)], writes=[("B_tok", pp)])
            yield
            b = C.bank()

            def f_cb(eng, b=b, bs=bs):
                r = None
                for g in range(4):
                    r = eng.matmul(C.ps[:, b, g * 128:(g + 1) * 128], BT[:, g, bs], CT[:, g, bs], start=True, stop=True)
                return r

            P.op("pe", f_cb, reads=[("BT", g) for g in range(4)] + [("CT", g) for g in range(4)], writes=[("ps", b)])
            yield
            P.op("act", lambda e, b=b: e.copy(CBs.rearrange("p g n -> p (g n)"), C.ps[:, b, :]), reads=[("ps", b)], writes=[("CBs", pp)])
            yield

        gen = prologue(0)
        for _ in gen:
            pass
        for nl in range(4):
            pp = nl % 2
            bs = slice(nl * 128, (nl + 1) * 128)
            a_, ncs, cdec = a_s[pp], ncss[pp], cdecs[pp]
            xd, xdw, B_tok, CBs = xds[pp], xdws[pp], B_toks[pp], CBss[pp]
            gen = prologue(nl + 1) if nl + 1 < 4 else iter(())

            def ssd_s1(h, bs=bs, pp=pp, a_=a_, ncs=ncs, CBs=CBs):
                g = h // 4
                hi = h % 3
                b = C.bank()

                def f_csb(eng, b=b, h=h):
                    al = bc(a_[:, h:h + 1], [[0, 128]])
                    eng.matmul(C.ps[:, b, 0:128], al, MC, start=True, stop=True)
                    eng.matmul(C.ps[:, b, 128:256], al, MC, start=False, stop=False, skip_group_check=True)
                    return eng.matmul(C.ps[:, b, 128:256], IDf, NEG, start=False, stop=True, skip_group_check=True)

                P.op("pe", f_csb, reads=[("a", pp), ("mask",)], writes=[("ps", b)])
                P.op("act", lambda e, b=b, hi=hi: e.activation(Ecs[hi], C.ps[:, b, 0:128], AF.Exp), reads=[("ps", b)], writes=[("Ecs", hi)])
                P.op("act", lambda e, b=b, hi=hi, h=h: e.activation(E[hi], C.ps[:, b, 128:256], AF.Exp, bias=ncs[:, h:h + 1], scale=1.0),
                     reads=[("ps", b), ("ncs", pp)], writes=[("E", hi)])
                P.op("pool", lambda e, hi=hi, g=g, bs=bs: e.tensor_tensor(Coff[hi], CT[:, g, bs], Ecs[hi], ALU.mult),
                     reads=[("Ecs", hi), ("CT", g)], writes=[("Coff", hi)])
                P.op("dve", lambda e, hi=hi, g=g: e.tensor_tensor(Mh[hi], E[hi], CBs[:, g, :], ALU.mult),
                     reads=[("E", hi), ("CBs", pp)], writes=[("Mh", hi)])

            def ssd_s2(h, pp=pp, xd=xd):
                hi = h % 3

                def f_y(eng, h=h, hi=hi):
                    yb = h // 8
                    col = ((h % 8) // 2) * 128
                    first = (h % 8) < 2
                    if h % 2 == 0:
                        out = C.ps[0:64, yb, col:col + 128]
                        kw = {}
                    else:
                        out = C.ps[64:128, yb, col:col + 128]
                        kw = {"tile_position": (0, 64)}
                    eng.matmul(out, xd[:, h * 64:(h + 1) * 64], Mh[hi], start=first, stop=False, skip_group_check=True, **kw)
                    return eng.matmul(out, st_bf[:, h * 64:(h + 1) * 64], Coff[hi], start=False, stop=True, skip_group_check=True, **kw)

                P.op("pe", f_y, reads=[("xd", pp, h // 8), ("Mh", hi), ("st_bf",), ("Coff", hi)], writes=[("ps", h // 8)])

            for h in range(16 + 2):
                if h < 16:
                    ssd_s1(h)
                if h >= 2:
                    ssd_s2(h - 2)
                if h >= 2:
                    next(gen, None)
                    next(gen, None)
            for _ in gen:
                pass
            bst = [C.bank(), C.bank()]

            def f_st(eng, bst=bst, B_tok=B_tok, xdw=xdw):
                r = None
                for g in range(4):
                    r = eng.matmul(C.ps[:, bst[g // 2], (g % 2) * 256:(g % 2) * 256 + 256], B_tok[:, g, :], xdw[:, g * 256:(g + 1) * 256],
                                   start=True, stop=True)
                return r

            P.op("pe", f_st, reads=[("B_tok", pp), ("xdw", pp, 0), ("xdw", pp, 1)], writes=[("ps", bst[0]), ("ps", bst[1])])
            P.op("dve", lambda e, cdec=cdec: e.tensor_tensor(st.rearrange("p (h q) -> p h q", h=16), st.rearrange("p (h q) -> p h q", h=16),
                                                        bc(cdec, [[1, 16], [0, 64]]), ALU.mult), reads=[("st",), ("cdec", pp)], writes=[("st",)])
            for hb in range(2):
                P.op("dve", lambda e, hb=hb, bst=bst: e.tensor_tensor(st[:, hb * 512:(hb + 1) * 512], st[:, hb * 512:(hb + 1) * 512], C.ps[:, bst[hb], :], ALU.add),
                     reads=[("st",), ("ps", bst[hb])], writes=[("st",)])
            P.op("act", lambda e: e.copy(st_bf, st), reads=[("st",)], writes=[("st_bf",)])
            for c in range(8):
                yb, col = c // 4, (c % 4) * 128
                P.op("dve", lambda e, c=c, yb=yb, col=col, bs=bs: e.scalar_tensor_tensor(yg[:, c, :], xsT[:, c, bs], C.consts[:, dsk + c:dsk + c + 1],
                                                                                C.ps[:, yb, col:col + 128], ALU.mult, ALU.add),
                     reads=[("xsT", c), ("ps", yb)], writes=[("yg", c)])
                P.op("pool", lambda e, c=c, bs=bs: e.tensor_tensor(yg[:, c, :], yg[:, c, :], zs[:, c, bs], ALU.mult),
                     reads=[("yg", c), ("zs", c)], writes=[("yg", c)])
            P.op("act", lambda e: e.activation(sq, yg, AF.Square), reads=[("yg", c) for c in range(8)], writes=[("ssq",)])
            b = C.bank()

            def f_ss(eng, b=b):
                r = None
                for gi in range(4):
                    eng.matmul(C.ps[:, b, gi * 128:(gi + 1) * 128], C.ones_bf[:, :], sq[:, 2 * gi, :], start=(gi == 0), stop=False, skip_group_check=True)
                    r = eng.matmul(C.ps[:, b, gi * 128:(gi + 1) * 128], C.ones_bf[:, :], sq[:, 2 * gi + 1, :], start=False, stop=True, skip_group_check=True)
                return r

            P.op("pe", f_ss, reads=[("ssq",), ("ones",)], writes=[("ps", b)])
            P.op("act", lambda e, b=b: e.activation(rstd, C.ps[:, b, :], AF.Sqrt, bias=epsc, scale=1.0 / 256), reads=[("ps", b)], writes=[("srstd",)])
            P.op("dve", lambda e: e.reciprocal(rstd, rstd), reads=[("srstd",)], writes=[("srstd",)])
            for c in range(8):
                P.op("dve", lambda e, c=c, bs=bs: e.scalar_tensor_tensor(oS[:, c, bs], yg[:, c, :], C.consts[:, ssn + c:ssn + c + 1],
                                                                    rstd[:, (c // 2) * 128:(c // 2 + 1) * 128], ALU.mult, ALU.mult),
                     reads=[("yg", c), ("srstd",)], writes=[("oS",)])
        for d in range(8):
            b = C.bank()

            def f_mm(eng, d=d, b=b):
                r = None
                for c in range(8):
                    r = eng.matmul(C.ps[:, b, :], woS[:, c, d * 128:(d + 1) * 128], oS[:, c, :], start=(c == 0), stop=(c == 7))
                return r

            P.op("pe", f_mm, reads=[("woS",), ("oS",)], writes=[("ps", b)])
            P.op("dve", lambda e, d=d, b=b, ts=ts: e.tensor_tensor(C.xT[:, d, ts], C.ps[:, b, :], C.xT[:, d, ts], ALU.add),
                 reads=[("ps", b), ("xT", d, j)], writes=[("xT", d, j)])


def rope_tables(P, C, s, j, posi, ang, tmpf, tmpi, C96, S96):
    cc = C.ccols
    invf = C.consts[:, cc["invf"][0]:cc["invf"][0] + 1]
    sgn = C.consts[:, cc["sgn"][0]:cc["sgn"][0] + 1]
    P.dma("sp", lambda e: e.dma_start(out=posi, in_=bass.AP(C.pos.tensor, C.pos[s:s + 1, j * 512:(j + 1) * 512].offset, [[0, 128], [1, 512]])),
          "posB", writes=[("posi",)])
    P.op("dve", lambda e: e.tensor_copy(ang, posi), reads=[("posi",)], writes=[("ang",)])
    P.op("dve", lambda e: e.tensor_scalar(ang, ang, invf, None, ALU.mult), reads=[("ang",)], writes=[("ang",)])

    def frac(buf):
        P.op("dve", lambda e: e.tensor_copy(tmpi, buf), reads=[("ang",)], writes=[("tmpi",)])
        P.op("dve", lambda e: e.tensor_copy(tmpf, tmpi), reads=[("tmpi",)], writes=[("tmpf",)])
        P.op("dve", lambda e: e.tensor_tensor(buf, buf, tmpf, ALU.subtract), reads=[("tmpf",), ("ang",)], writes=[("ang",)])
        P.op("dve", lambda e: e.tensor_single_scalar(tmpf, buf, 0.5, ALU.is_gt), reads=[("ang",)], writes=[("tmpf",)])
        P.op("dve", lambda e: e.tensor_tensor(buf, buf, tmpf, ALU.subtract), reads=[("tmpf",), ("ang",)], writes=[("ang",)])
        P.op("dve", lambda e: e.tensor_single_scalar(tmpf, buf, -0.5, ALU.is_lt), reads=[("ang",)], writes=[("tmpf",)])
        P.op("dve", lambda e: e.tensor_tensor(buf, buf, tmpf, ALU.add), reads=[("tmpf",), ("ang",)], writes=[("ang",)])

    frac(ang)
    P.op("act", lambda e: e.activation(S96, ang, AF.Sin, scale=float(2 * np.pi)), reads=[("ang",)], writes=[("S96",)])
    P.op("dve", lambda e: e.tensor_scalar(S96, S96, sgn, None, ALU.mult), reads=[("S96",)], writes=[("S96",)])
    P.op("dve", lambda e: e.tensor_scalar(ang, ang, 0.25, None, ALU.add), reads=[("ang",), ("S96",)], writes=[("ang",)])
    frac(ang)
    P.op("act", lambda e: e.activation(C96, ang, AF.Sin, scale=float(2 * np.pi)), reads=[("ang",)], writes=[("C96",)])


def mla_kv_stage(P, C, s):
    w_in = C.need("e_w_in", [D, 3760], lambda inp: inp["e_w_in"][0])
    w_kvb = C.need("e_w_kv_b", [256, 1024], lambda inp: inp["e_w_kv_b"][0])
    cc = C.ccols
    A = Arena(C)
    kT = A.bf(8, T)
    V_tok = A.bf(16, 512)
    wA = A.bf(8, 288)
    wpp = A.bf(8, 32)
    wkvb = A.bf(2, 1024)
    kvn = A.bf(2, 512)
    sq = A.bf(512)
    sqk = A.bf(512)
    rstd = A.f32(512)
    posi = A.i32(512)
    ang = A.f32(512)
    tmpf = A.f32(512)
    tmpi = A.i32(512)
    C96 = A.f32(512)
    S96 = A.f32(512)
    kr = A.f32(512)
    t1 = A.f32(512)
    w_in_v = w_in.rearrange("(k p) f -> p k f", p=128)
    epsc = C.consts[:, C.eps_col:C.eps_col + 1]
    kvan = cc["e_kvan"][0]
    gk = C.consts[:, cc["gk"][0]:cc["gk"][0] + 1]
    gkp = C.consts[:, cc["gkp"][0]:cc["gkp"][0] + 1]
    R = slice(64, 96)

    P.dma("pool", lambda e: e.dma_start(out=wA, in_=w_in_v[:, :, 3472:3760]), "wA", writes=[("wA",)])
    P.dma("pool", lambda e: [e.dma_start(out=wpp[:, :, 0:16], in_=w_in_v[:, :, 3744:3760]),
                             e.dma_start(out=wpp[:, :, 16:32], in_=w_in_v[:, :, 3728:3744])], "wB", writes=[("wpp",)], n=2)
    P.dma("pool", lambda e: e.dma_start(out=wkvb, in_=w_kvb.rearrange("(k p) f -> p k f", p=128)), "wC", writes=[("wkvb",)])

    for j in range(4):
        ts = tsl(j)
        rope_tables(P, C, s, j, posi, ang, tmpf, tmpi, C96, S96)
        bk = []
        for c in range(2):
            b = C.bank()
            bk.append(b)

            def f_mm(eng, c=c, b=b, ts=ts):
                r = None
                for k in range(8):
                    r = eng.matmul(C.ps[:, b, :], wA[:, k, c * 128:(c + 1) * 128], C.hT[:, k, ts], start=(k == 0), stop=(k == 7))
                return r

            P.op("pe", f_mm, reads=[("wA",)] + [("hT", k, j) for k in range(8)], writes=[("ps", b)])
        bss = C.bank()
        for c in range(2):
            P.op("act", lambda e, c=c, bk=bk: e.activation(sq, C.ps[:, bk[c], :], AF.Square), reads=[("ps", bk[c])], writes=[("msq",)])
            P.op("pe", lambda e, c=c, bss=bss: e.matmul(C.ps[:, bss, :], C.ones_bf[:, :], sq, start=(c == 0), stop=(c == 1)),
                 reads=[("msq",), ("ones",)], writes=[("ps", bss)])
        P.op("act", lambda e, bss=bss: e.activation(rstd, C.ps[:, bss, :], AF.Sqrt, bias=epsc, scale=1.0 / 256), reads=[("ps", bss)], writes=[("mrstd",)])
        P.op("dve", lambda e: e.reciprocal(rstd, rstd), reads=[("mrstd",)], writes=[("mrstd",)])
        for c in range(2):
            P.op("dve", lambda e, c=c, bk=bk: e.scalar_tensor_tensor(kvn[:, c, :], C.ps[:, bk[c], :], C.consts[:, kvan + c:kvan + c + 1], rstd, ALU.mult, ALU.mult),
                 reads=[("ps", bk[c]), ("mrstd",)], writes=[("kvn", c)])
        bx = C.bank()
        by = C.bank()

        def f_pe(eng, bx=bx, ts=ts):
            r = None
            for k in range(8):
                r = eng.matmul(C.ps[64:96, bx, :], wA[:, k, 256:288], C.hT[:, k, ts], start=(k == 0), stop=(k == 7), tile_position=(0, 64))
            return r

        def f_pp(eng, by=by, ts=ts):
            r = None
            for k in range(8):
                r = eng.matmul(C.ps[64:96, by, :], wpp[:, k, :], C.hT[:, k, ts], start=(k == 0), stop=(k == 7), tile_position=(0, 64))
            return r

        P.op("pe", f_pe, reads=[("wA",)] + [("hT", k, j) for k in range(8)], writes=[("ps", bx)])
        P.op("pe", f_pp, reads=[("wpp",)] + [("hT", k, j) for k in range(8)], writes=[("ps", by)])
        P.op("dve", lambda e, bx=bx: e.scalar_tensor_tensor(kr[R, :], C.ps[R, bx, :], gk[R, :], C96[R, :], ALU.mult, ALU.mult),
             reads=[("ps", bx), ("C96",)], writes=[("kr",)])
        P.op("dve", lambda e, by=by: e.scalar_tensor_tensor(t1[R, :], C.ps[R, by, :], gkp[R, :], S96[R, :], ALU.mult, ALU.mult),
             reads=[("ps", by), ("S96",)], writes=[("t1",)])
        P.op("dve", lambda e: e.tensor_tensor(kr[R, :], kr[R, :], t1[R, :], ALU.add), reads=[("kr",), ("t1",)], writes=[("kr",)])
        P.op("act", lambda e, bx=bx: e.activation(sqk[R, :], C.ps[R, bx, :], AF.Square), reads=[("ps", bx)], writes=[("sqk",)])
        for h in range(8):
            b = C.bank()

            def f_kn(eng, h=h, b=b):
                r = None
                for k in range(2):
                    r = eng.matmul(C.ps[0:64, b, :], wkvb[:, k, h * 128:h * 128 + 64], kvn[:, k, :], start=(k == 0), stop=(k == 1))
                return r

            P.op("pe", f_kn, reads=[("wkvb",), ("kvn", 0), ("kvn", 1)], writes=[("ps", b)])
            P.op("act", lambda e, b=b: e.activation(sqk[0:64, :], C.ps[0:64, b, :], AF.Square), reads=[("ps", b)], writes=[("sqk",)])
            b2 = C.bank()
            P.op("pe", lambda e, b2=b2: e.matmul(C.ps[0:96, b2, :], C.ones_bf[0:96, 0:96], sqk[0:96, :], start=True, stop=True),
                 reads=[("sqk",), ("ones",)], writes=[("ps", b2)])
            P.op("act", lambda e, b2=b2: e.activation(rstd[0:96, :], C.ps[0:96, b2, :], AF.Sqrt, bias=epsc[0:96, :], scale=1.0 / 96),
                 reads=[("ps", b2)], writes=[("mrstd",)])
            P.op("dve", lambda e: e.reciprocal(rstd[0:96, :], rstd[0:96, :]), reads=[("mrstd",)], writes=[("mrstd",)])
            P.op("dve", lambda e, h=h, b=b, ts=ts: e.scalar_tensor_tensor(kT[0:64, h, ts], C.ps[0:64, b, :], gk[0:64, :], rstd[0:64, :], ALU.mult, ALU.mult),
                 reads=[("ps", b), ("mrstd",)], writes=[("kT", h, j)])
            P.op("dve", lambda e, h=h, ts=ts: e.tensor_tensor(kT[R, h, ts], kr[R, :], rstd[R, :], ALU.mult),
                 reads=[("kr",), ("mrstd",)], writes=[("kT", h, j)])
        for nl in range(4):
            n = 4 * j + nl
            b = C.bank()

            def f_v(eng, b=b, nl=nl):
                r = None
                for k in range(2):
                    r = eng.matmul(C.ps[:, b, :], kvn[:, k, nl * 128:(nl + 1) * 128],
                                   wkvb[:, k, :].rearrange("p (h x) -> p h x", h=8)[:, :, 64:128], start=(k == 0), stop=(k == 1))
                return r

            P.op("pe", f_v, reads=[("wkvb",), ("kvn", 0), ("kvn", 1)], writes=[("ps", b)])
            P.op("act", lambda e, b=b, n=n: e.copy(V_tok[:, n, :], C.ps[:, b, :]), reads=[("ps", b)], writes=[("V_tok", n)])


def mla_attn_stage(P, C, s):
    w_in = C.need("e_w_in", [D, 3760], lambda inp: inp["e_w_in"][0])
    w_qb = C.need("e_w_q_b", [384, 768], lambda inp: inp["e_w_q_b"][0])
    w_out = C.need("e_w_out", [1536, D], lambda inp: inp["e_w_out"][0])
    cc = C.ccols
    A = Arena(C)
    kT = A.bf(8, T)
    V_tok = A.bf(16, 512)
    wA = A.bf(8, 384)
    wqb = A.bf(3, 768)
    wqbp = A.bf(3, 8, 32)
    woM = A.bf(4, D)
    qan = A.bf(3, 512)
    sq = A.bf(512)
    rstd = A.f32(512)
    posi = A.i32(512)
    ang = A.f32(512)
    tmpf = A.f32(512)
    tmpi = A.i32(512)
    C96 = A.f32(512)
    S96 = A.f32(512)
    t1 = A.f32(512)
    t2 = A.f32(512)
    qT = [A.bf(512) for _ in range(2)]
    Pb = [A.bf(512) for _ in range(4)]
    rden = A.f32(512)
    oT = A.bf(4, 512)
    w_in_v = w_in.rearrange("(k p) f -> p k f", p=128)
    epsc = C.consts[:, C.eps_col:C.eps_col + 1]
    qanc = cc["e_qan"][0]
    gq = C.consts[:, cc["gq"][0]:cc["gq"][0] + 1]
    gqp = C.consts[:, cc["gqp"][0]:cc["gqp"][0] + 1]
    R = slice(64, 96)
    SC = float(96 ** -0.5)
    w_qb_v = w_qb.rearrange("(k p) (h x) -> p k h x", p=128, h=8)

    P.dma("pool", lambda e: e.dma_start(out=wA, in_=w_in_v[:, :, 3088:3472]), "wA", writes=[("wA",)])
    P.dma("pool", lambda e: e.dma_start(out=wqb, in_=w_qb.rearrange("(k p) f -> p k f", p=128)), "wB", writes=[("wqb",)])
    P.dma("pool", lambda e: [e.dma_start(out=wqbp[:, k, :, 0:16], in_=w_qb_v[:, k, :, 80:96]) for k in range(3)]
          + [e.dma_start(out=wqbp[:, k, :, 16:32], in_=w_qb_v[:, k, :, 64:80]) for k in range(3)], "wC", writes=[("wqbp",)], n=6)
    P.dma("pool", lambda e: e.dma_start(out=woM, in_=w_out[1024:1536, :].rearrange("(c p) d -> p c d", p=128)), "wD", writes=[("woM",)])
    pbi = [0]
    qi = [0]

    for j in range(4):
        ts = tsl(j)
        rope_tables(P, C, s, j, posi, ang, tmpf, tmpi, C96, S96)
        bq = []
        for c in range(3):
            b = C.bank()
            bq.append(b)

            def f_mm(eng, c=c, b=b, ts=ts):
                r = None
                for k in range(8):
                    r = eng.matmul(C.ps[:, b, :], wA[:, k, c * 128:(c + 1) * 128], C.hT[:, k, ts], start=(k == 0), stop=(k == 7))
                return r

            P.op("pe", f_mm, reads=[("wA",)] + [("hT", k, j) for k in range(8)], writes=[("ps", b)])
        bss = C.bank()
        for c in range(3):
            P.op("act", lambda e, c=c, bq=bq: e.activation(sq, C.ps[:, bq[c], :], AF.Square), reads=[("ps", bq[c])], writes=[("msq",)])
            P.op("pe", lambda e, c=c, bss=bss: e.matmul(C.ps[:, bss, :], C.ones_bf[:, :], sq, start=(c == 0), stop=(c == 2)),
                 reads=[("msq",), ("ones",)], writes=[("ps", bss)])
        P.op("act", lambda e, bss=bss: e.activation(rstd, C.ps[:, bss, :], AF.Sqrt, bias=epsc, scale=1.0 / 384), reads=[("ps", bss)], writes=[("mrstd",)])
        P.op("dve", lambda e: e.reciprocal(rstd, rstd), reads=[("mrstd",)], writes=[("mrstd",)])
        for c in range(3):
            P.op("dve", lambda e, c=c, bq=bq: e.scalar_tensor_tensor(qan[:, c, :], C.ps[:, bq[c], :], C.consts[:, qanc + c:qanc + c + 1], rstd, ALU.mult, ALU.mult),
                 reads=[("ps", bq[c]), ("mrstd",)], writes=[("qan", c)])
        def qA(h):
            b = C.bank()
            bp = C.bank()

            def f_q(eng, h=h, b=b, bp=bp):
                r = None
                for k in range(3):
                    r = eng.matmul(C.ps[0:96, b, :], wqb[:, k, h * 96:(h + 1) * 96], qan[:, k, :], start=(k == 0), stop=(k == 2))
                for k in range(3):
                    r = eng.matmul(C.ps[64:96, bp, :], wqbp[:, k, h, :], qan[:, k, :], start=(k == 0), stop=(k == 2), tile_position=(0, 64))
                return r

            P.op("pe", f_q, reads=[("wqb",), ("wqbp",)] + [("qan", c) for c in range(3)], writes=[("ps", b), ("ps", bp)])
            P.op("act", lambda e, b=b: e.activation(sq[0:96, :], C.ps[0:96, b, :], AF.Square), reads=[("ps", b)], writes=[("msq",)])
            return b, bp

        def qB(h, b, bp):
            b2 = C.bank()
            P.op("pe", lambda e, b2=b2: e.matmul(C.ps[0:96, b2, :], C.ones_bf[0:96, 0:96], sq[0:96, :], start=True, stop=True),
                 reads=[("msq",), ("ones",)], writes=[("ps", b2)])
            P.op("act", lambda e, b2=b2: e.activation(rstd[0:96, :], C.ps[0:96, b2, :], AF.Sqrt, bias=epsc[0:96, :], scale=1.0 / 96),
                 reads=[("ps", b2)], writes=[("mrstd",)])
            P.op("dve", lambda e: e.reciprocal(rstd[0:96, :], rstd[0:96, :]), reads=[("mrstd",)], writes=[("mrstd",)])
            P.op("dve", lambda e, b=b: e.scalar_tensor_tensor(t1[0:96, :], C.ps[0:96, b, :], gq[0:96, :], C96[0:96, :], ALU.mult, ALU.mult),
                 reads=[("ps", b), ("C96",)], writes=[("t1",)])
            P.op("dve", lambda e, bp=bp: e.scalar_tensor_tensor(t2[R, :], C.ps[R, bp, :], gqp[R, :], S96[R, :], ALU.mult, ALU.mult),
                 reads=[("ps", bp), ("S96",)], writes=[("t2",)])
            P.op("dve", lambda e: e.tensor_tensor(t1[R, :], t1[R, :], t2[R, :], ALU.add), reads=[("t1",), ("t2",)], writes=[("t1",)])
            qk = qi[0]
            qi[0] = (qi[0] + 1) % 2
            Q = qT[qk]
            P.op("dve", lambda e, Q=Q: e.tensor_tensor(Q[0:96, :], t1[0:96, :], rstd[0:96, :], ALU.mult), reads=[("t1",), ("mrstd",)], writes=[("qT", qk)])
            return qk, Q

        LOOK = 2
        nxt = qB(0, *qA(0))
        for h in range(8):
            hi = h % 2
            qk, Q = nxt
            pend_q = qA(h + 1) if h + 1 < 8 else None
            nkb = 4 * j + 4
            po = slice(hi * 64, hi * 64 + 64)
            kw = {"tile_position": (0, 64)} if hi == 1 else {}

            def emit_st(kb, h=h, Q=Q, qk=qk):
                b = C.bank()
                P.op("pe", lambda e, b=b, h=h, kb=kb, Q=Q: e.matmul(C.ps[:, b, :], kT[0:96, h, kb * 128:(kb + 1) * 128], Q[0:96, :], start=True, stop=True),
                     reads=[("kT", h, kb // 4), ("qT", qk)], writes=[("ps", b)])
                pk = pbi[0]
                pbi[0] = (pbi[0] + 1) % 4
                PB = Pb[pk]
                P.op("act", lambda e, b=b, PB=PB: e.activation(PB, C.ps[:, b, :], AF.Exp, scale=SC), reads=[("ps", b)], writes=[("Pb", pk)])
                if kb >= 4 * j:
                    o = (kb - 4 * j) * 128
                    m0 = 512 + 384 - o
                    P.op("pool", lambda e, PB=PB, m0=m0: e.tensor_tensor(PB, PB, C.maskb[:, m0:m0 + 512], ALU.mult),
                         reads=[("Pb", pk), ("mask",)], writes=[("Pb", pk)])
                return pk, PB

            def emit_pv(kb, pk, PB, h=h, po=po, kw=kw, nkb=nkb):
                def f_pv(eng):
                    eng.matmul(C.ps[po, 0, :], V_tok[:, kb, h * 64:(h + 1) * 64], PB, start=(kb == 0), stop=(kb == nkb - 1), skip_group_check=True, **kw)
                    return eng.matmul(C.ps[po, 1, :], C.ones_bf[:, 0:64], PB, start=(kb == 0), stop=(kb == nkb - 1), skip_group_check=True, **kw)

                P.op("pe", f_pv, reads=[("V_tok", kb), ("Pb", pk), ("ones",)], writes=[("ps", 0), ("ps", 1)])

            pend = []
            for kb in range(nkb):
                pend.append((kb,) + emit_st(kb))
                if kb == 1 and pend_q is not None:
                    nxt = qB(h + 1, *pend_q)
                    pend_q = None
                if len(pend) > LOOK:
                    emit_pv(*pend.pop(0))
            while pend:
                emit_pv(*pend.pop(0))
            if hi == 1:
                P.op("dve", lambda e: e.reciprocal(rden, C.ps[:, 1, :]), reads=[("ps", 1)], writes=[("rden",)])
                P.op("dve", lambda e, h=h: e.tensor_tensor(oT[:, h // 2, :], C.ps[:, 0, :], rden, ALU.mult), reads=[("ps", 0), ("rden",)], writes=[("oT",)])
        for d in range(8):
            b = C.bank()

            def f_mm(eng, d=d, b=b):
                r = None
                for c in range(4):
                    r = eng.matmul(C.ps[:, b, :], woM[:, c, d * 128:(d + 1) * 128], oT[:, c, :], start=(c == 0), stop=(c == 3))
                return r

            P.op("pe", f_mm, reads=[("woM",), ("oT",)], writes=[("ps", b)])
            P.op("dve", lambda e, d=d, b=b, ts=ts: e.tensor_tensor(C.xT[:, d, ts], C.ps[:, b, :], C.xT[:, d, ts], ALU.add),
                 reads=[("ps", b), ("xT", d, j)], writes=[("xT", d, j)])


def load_x(P, C, xin):
    v = xin.rearrange("(k p) t -> p k t", p=128)
    for k in range(8):
        def f(eng, k=k):
            return eng.dma_start(out=C.xT[:, k, :], in_=v[:, k, :])

        P.dma("sp", f, f"xin{k}", writes=[("xT", k, j) for j in range(4)])


def store_x(P, C, yout):
    v = yout.rearrange("(k p) t -> p k t", p=128)
    for k in range(8):
        def f(eng, k=k):
            return eng.dma_start(out=v[:, k, :], in_=C.xT[:, k, :])

        P.dma("sp", f, f"xout{k}", reads=[("xT", k, j) for j in range(4)])


def build(nseq, stages, ccols, ncc):
    nc = bass.Bass("TRN2", target_bir_lowering=False)
    dr = {}
    hostprep = {}

    def din(name, shape, dt=F32):
        dr[name] = nc.dram_tensor(name, list(shape), dt, kind="ExternalInput").ap()
        return dr[name]

    def need(name, shape, fn, dt=F32):
        if name not in dr:
            din(name, shape, dt)
            hostprep[name] = fn
        return dr[name]

    xin = din("xT_in", [nseq * D, T])
    din("consts", [128, ncc])
    din("maskc", [128, NMASK])
    din("maskb", [128, NMASKB])
    yout = nc.dram_tensor("yT_out", [nseq * D, T], F32, kind="ExternalOutput").ap()

    P = Prog()
    C = Ctx()
    C.need = need
    C.ccols = ccols
    with ExitStack() as es:
        def sb(name, shape, dt):
            return es.enter_context(nc.sbuf_tensor(name, list(shape), dt))

        C.xT = sb("xT", [128, 8, T], F32)
        C.hT = sb("hT", [128, 8, T], BF16)
        C.consts = sb("consts_sb", [128, ncc], F32)
        C.ones_bf = sb("ones_bf", [128, 128], BF16)
        C.ar = sb("arena", [128, ARENA // 2], BF16)
        C.ar_f = C.ar.bitcast(F32)
        C.ar_i = C.ar.bitcast(I32)
        C.mask = sb("maskc_sb", [128, NMASK], F32)
        C.ident_bf = sb("ident_bf", [128, 128], BF16)
        C.maskb = sb("maskb_sb", [128, NMASKB], BF16)
        C.bd_bf = sb("bd_bf", [128, 128], BF16)
        C.pos = din("pos", [nseq, T], I32)
        C.posT = din("posT", [nseq, 128, 16], I32)
        C.dr = dr
        ffn_alloc(C)
        C.ps = es.enter_context(nc.psum_tensor("ps", [128, 8, 512], F32))
        C.wslot = 0
        C.eps_col = ccols['eps'][0]
        C.sgslot = 0
        C.nbank = 0
        C.pmi = 0

        def bank():
            b = 2 + C.nbank
            C.nbank = (C.nbank + 1) % 6
            return b

        C.bank = bank

        P.dma("sp", lambda eng: eng.dma_start(out=C.consts[:, :], in_=dr["consts"]), "consts", writes=[("consts",)])
        P.dma("sp", lambda eng: eng.dma_start(out=C.mask[:, :], in_=dr["maskc"]), "maskc", writes=[("mask",)])
        P.dma("pool", lambda eng: eng.dma_start(out=C.maskb[:, :], in_=dr["maskb"]), "maskb", writes=[("mask",)])
        P.op("dve", lambda eng: eng.memset(C.ones_bf[:, :], 1.0), writes=[("ones",)])
        P.op("dve", lambda eng: eng.tensor_copy(C.bd_bf[:, :], C.mask[:, 256:384]), reads=[("mask",)], writes=[("bd",)])
        P.op("dve", lambda eng: eng.tensor_copy(C.ident_bf[:, :], C.mask[:, 512:640]), reads=[("mask",)], writes=[("ident",)])
        P.barrier()

        for s in range(nseq):
            load_x(P, C, xin[s * D:(s + 1) * D, :])
            for st in stages:
                kind = st[0]
                if kind == "ffn":
                    _, nm, l = st
                    wg = need(f"{nm}_w_gate{l}", [D, DFF], lambda inp, nm=nm, l=l: inp[f"{nm}_w_gate"][l])
                    wu = need(f"{nm}_w_up{l}", [D, DFF], lambda inp, nm=nm, l=l: inp[f"{nm}_w_up"][l])
                    wd = need(f"{nm}_w_down{l}", [DFF, D], lambda inp, nm=nm, l=l: inp[f"{nm}_w_down"][l])
                    ffn_stage(P, C, ccols[f"{nm}_norm{l}"][0], wg, wu, wd)
                elif kind == "norm":
                    rmsnorm_stage(P, C, ccols[f"mix_norm{st[1]}"][0])
                elif kind == "swa":
                    swa_stage(P, C, s)
                elif kind == "gla":
                    gla_stage(P, C, s)
                elif kind == "ssd":
                    ssd_stage(P, C, s)
                elif kind == "mla":
                    mla_kv_stage(P, C, s)
                    P.barrier()
                    if os.environ.get("DBG_DUMP"):
                        dk = nc.dram_tensor("dbg_k", [128, 8 * T], BF16, kind="ExternalOutput").ap()
                        dv = nc.dram_tensor("dbg_v", [128, 16 * 512], BF16, kind="ExternalOutput").ap()
                        P.dma("sp", lambda e: [e.dma_start(out=dk[:, i * 1024:(i + 1) * 1024], in_=C.ar[:, i * 1024:(i + 1) * 1024]) for i in range(16)], "dbgk", n=16)
                        P.dma("sp", lambda e: [e.dma_start(out=dv[:, i * 1024:(i + 1) * 1024], in_=C.ar[:, 8 * T + i * 1024:8 * T + (i + 1) * 1024]) for i in range(8)], "dbgv", n=8)
                        P.barrier()
                    else:
                        mla_attn_stage(P, C, s)
                else:
                    raise ValueError(kind)
                P.barrier()
            store_x(P, C, yout[s * D:(s + 1) * D, :])
        P.emit(nc)
    return nc, hostprep


ALL_STAGES = [("ffn", "pre", 0), ("norm", 0), ("ssd", 0), ("mla", 0), ("ffn", "post", 0),
              ("ffn", "pre", 1), ("norm", 1), ("swa", 1), ("gla", 1), ("ffn", "post", 1)]


def run(inputs, stages=ALL_STAGES, ncores=8, nseq=4, trace=False):
    x = np.asarray(inputs["x"], np.float32)
    pos = np.asarray(inputs["positions"], np.int32)
    consts, ccols = pack_consts(inputs)
    nc, hostprep = build(nseq, stages, ccols, consts.shape[1])
    mk = make_masks()
    shared = {"consts": consts, "maskc": mk[0], "maskb": mk[1]}
    for name, fn in hostprep.items():
        shared[name] = np.ascontiguousarray(np.asarray(fn(inputs), np.float32))
    in_maps = []
    for c in range(ncores):
        xs = x[c * nseq:(c + 1) * nseq]
        xT = np.ascontiguousarray(xs.transpose(0, 2, 1)).reshape(nseq * D, T)
        ps = np.ascontiguousarray(pos[c * nseq:(c + 1) * nseq])
        pT = np.ascontiguousarray(ps.reshape(nseq, 16, 128).transpose(0, 2, 1))
        m = {"xT_in": xT, "pos": ps, "posT": pT}
        m.update(shared)
        in_maps.append(m)
    res = run_bass_kernel_spmd(nc, in_maps, core_ids=list(range(ncores)), trace=trace)
    outs = []
    for c in range(ncores):
        yT = np.asarray(res.results[c]["yT_out"]).reshape(nseq, D, T)
        outs.append(yT.transpose(0, 2, 1))
    out = np.ascontiguousarray(np.concatenate(outs, axis=0)).astype(np.float32)
    return out, res


def kernel(**inputs):
    out, _ = run(inputs)
    return out
```

```python
import numpy as np
from contextlib import ExitStack
import concourse.bass as bass
import concourse.mybir as mybir
from concourse.bass_utils import run_bass_kernel_spmd

F32 = mybir.dt.float32
BF16 = mybir.dt.bfloat16
I32 = mybir.dt.int32
AF = mybir.ActivationFunctionType
ALU = mybir.AluOpType

D = 1024
T = 2048
DFF = 2816
NCH = DFF // 128
EPS = 1e-6
ENGS = ("pe", "act", "dve", "pool", "sp")
import os
SERIAL = bool(os.environ.get('DBG_SERIAL'))


class Prog:
    def __init__(self):
        self.ops = {e: [] for e in ENGS}
        self.cnt = {e: 0 for e in ENGS}
        self.waited = {e: {} for e in ENGS}
        self.last_w = {}
        self.readers = {}
        self.dcnt = {}

    def _deps(self, eng, reads, writes):
        deps = {}

        def add(s, v):
            if deps.get(s, 0) < v:
                deps[s] = v

        for k in reads:
            if k in self.last_w:
                add(*self.last_w[k])
        for k in writes:
            if k in self.last_w:
                add(*self.last_w[k])
            for s, v in self.readers.get(k, {}).items():
                add(s, v)
        waits = []
        for s, v in deps.items():
            if s == "e:pe" and eng == "pe":
                continue
            if self.waited[eng].get(s, 0) < v:
                self.waited[eng][s] = v
                waits.append((s, v))
        return waits

    def _commit(self, tok, reads, writes):
        for k in writes:
            self.last_w[k] = tok
            self.readers[k] = {}
        for k in reads:
            r = self.readers.setdefault(k, {})
            if r.get(tok[0], 0) < tok[1]:
                r[tok[0]] = tok[1]

    def op(self, eng, fn, reads=(), writes=()):
        waits = self._deps(eng, reads, writes)
        self.cnt[eng] += 1
        tok = ("e:" + eng, self.cnt[eng])
        self.ops[eng].append((waits, fn, tok[0], 1))
        self._commit(tok, reads, writes)
        if SERIAL:
            self.barrier()

    def dma(self, eng, fn, key, reads=(), writes=(), n=1):
        waits = self._deps(eng, reads, writes)
        s = "d:" + key
        self.dcnt[s] = self.dcnt.get(s, 0) + 16 * n
        tok = (s, self.dcnt[s])
        self.ops[eng].append((waits, fn, s, 16))
        self._commit(tok, reads, writes)
        if SERIAL:
            self.barrier()

    def barrier(self):
        for e in ENGS:
            for e2 in ENGS:
                if e2 == "sp" or self.cnt[e2] == 0:
                    continue
                s = "e:" + e2
                v = self.cnt[e2]
                if self.waited[e].get(s, 0) < v:
                    self.waited[e][s] = v
                    self.ops[e].append(([(s, v)], None, None, 0))
            for s, v in self.dcnt.items():
                if self.waited[e].get(s, 0) < v:
                    self.waited[e][s] = v
                    self.ops[e].append(([(s, v)], None, None, 0))

    def emit(self, nc):
        with ExitStack() as es:
            sems = {}
            names = set()
            for e in ENGS:
                for waits, fn, s, inc in self.ops[e]:
                    if s is not None:
                        names.add(s)
                    for w in waits:
                        names.add(w[0])
            for s in sorted(names):
                sems[s] = es.enter_context(nc.semaphore(s.replace(":", "_")))
            block = es.enter_context(nc.Block())

            def run(e):
                def body(eng):
                    for waits, fn, s, inc in self.ops[e]:
                        for ws, wv in waits:
                            eng.wait_ge(sems[ws], wv)
                        if fn is None:
                            continue
                        r = fn(eng)
                        if isinstance(r, (list, tuple)):
                            for ins in r:
                                ins.then_inc(sems[s], inc)
                        else:
                            r.then_inc(sems[s], inc)
                    if e == "sp":
                        for s, v in self.dcnt.items():
                            eng.wait_ge(sems[s], v)

                return body

            block.tensor(run("pe"))
            block.scalar(run("act"))
            block.vector(run("dve"))
            block.gpsimd(run("pool"))
            block.sync(run("sp"))


class Ctx:
    pass


def tsl(j, n=512):
    return slice(j * n, (j + 1) * n)


def fm(v):
    v = np.asarray(v, np.float32)
    return np.ascontiguousarray(v.reshape(-1, 128).T)


def make_masks():
    p = np.arange(128)[:, None]
    f = np.arange(128)[None, :]
    mc = (f >= p)
    mp = (f < p)
    bd = (p // 64 == f // 64)
    gm = bd & (p <= f)
    m64 = np.broadcast_to((np.arange(512)[None, :] % 64 != 0), (128, 512))
    ident = (p == f)
    neg = -30000.0 * mp
    fw = np.arange(896)[None, :]
    mcw = (fw - 384 >= p)
    return (np.ascontiguousarray(np.concatenate([mc, mp, bd, gm, ident, neg], axis=1).astype(np.float32)),
            np.ascontiguousarray(np.concatenate([m64, mcw], axis=1).astype(np.float32)))


NMASK = 768
NMASKB = 512 + 896


def pack_consts(inp):
    cols = {}
    parts = []
    off = 0

    def put(name, arr):
        nonlocal off
        arr = np.asarray(arr, np.float32)
        assert arr.shape[0] == 128
        cols[name] = (off, arr.shape[1])
        parts.append(arr)
        off += arr.shape[1]

    for l in range(2):
        put(f"pre_norm{l}", fm(inp["pre_norm"][l]))
        put(f"mix_norm{l}", fm(inp["mix_norm"][l]))
        put(f"post_norm{l}", fm(inp["post_norm"][l]))
    put("eps", np.full((128, 1), EPS, np.float32))
    put("one", np.ones((128, 1), np.float32))
    put("o_qg2", np.tile(np.asarray(inp["o_q_norm"][0], np.float32), 2)[:, None])
    put("o_kg2", np.tile(np.asarray(inp["o_k_norm"][0], np.float32), 2)[:, None])
    cw = np.asarray(inp["e_conv_w"][0], np.float32)
    put("e_convw", np.ascontiguousarray(cw.T.reshape(16, 128, 4).transpose(1, 0, 2).reshape(128, 64)))
    put("e_convb", fm(inp["e_conv_b"][0]))
    put("e_dtb", np.broadcast_to(np.asarray(inp["e_dt_bias"][0], np.float32)[None, :], (128, 16)))
    put("e_alog", np.broadcast_to(np.asarray(inp["e_a_log"][0], np.float32)[None, :], (128, 16)))
    put("e_dskip", fm(np.repeat(np.asarray(inp["e_d_skip"][0], np.float32), 64)))
    put("e_ssmn", fm(inp["e_ssm_norm"][0]))
    put("e_qan", fm(inp["e_q_a_norm"][0]))
    put("e_kvan", fm(inp["e_kv_a_norm"][0]))
    for nm_, key_ in (("gq", "e_q_norm"), ("gk", "e_k_norm")):
        g96 = np.asarray(inp[key_][0], np.float32)
        col = np.zeros((128, 1), np.float32)
        col[0:96, 0] = g96
        put(nm_, col)
        colp = np.zeros((128, 1), np.float32)
        colp[64:80, 0] = g96[80:96]
        colp[80:96, 0] = g96[64:80]
        put(nm_ + "p", colp)
    invf = np.zeros((128, 1), np.float32)
    fr = (10000.0 ** (-np.arange(16, dtype=np.float64) / 16.0) / (2 * np.pi)).astype(np.float32)
    invf[64:80, 0] = fr
    invf[80:96, 0] = fr
    put("invf", invf)
    sgn = np.zeros((128, 1), np.float32)
    sgn[64:80, 0] = -1.0
    sgn[80:96, 0] = 1.0
    put("sgn", sgn)
    put("o_gbias", fm(inp["o_gate_bias"][0]))
    put("o_glan", np.asarray(inp["o_gla_norm"][0], np.float32)[:, None])
    put("sinks", np.broadcast_to(np.asarray(inp["o_sinks"][0], np.float32)[None, :], (128, 8)))
    put("SL", np.broadcast_to((-8.0 * 2.0 ** (-np.arange(1, 9, dtype=np.float64))).astype(np.float32)[None, :], (128, 8)))
    return np.ascontiguousarray(np.concatenate(parts, axis=1)), cols


def rmsnorm_stage(P, C, gcol):
    sqs = [C.aT[:, 2 * j:2 * j + 2, :].rearrange("p a (b c) -> p (a b) c", b=4) for j in range(4)]
    rstds = [C.sq[:, 2 * j:2 * j + 2, :].bitcast(F32).rearrange("p a c -> p (a c)") if False else None for j in range(4)]
    rstds = [C.rstd4[:, j, :] for j in range(4)]
    banks = {}

    def st_a(j):
        ts = tsl(j)

        def f_sq(e, j=j, ts=ts):
            r = None
            for k in range(8):
                r = e.activation(sqs[j][:, k, :], C.xT[:, k, ts], AF.Square)
            return r

        P.op("act", f_sq, reads=[("xT", d, j) for d in range(8)], writes=[("sq", j)])
        bank = C.bank()
        banks[j] = bank

        def f_mm(eng, bank=bank, j=j):
            r = None
            for k in range(8):
                r = eng.matmul(C.ps[:, bank, :], C.ones_bf[:, :], sqs[j][:, k, :], start=(k == 0), stop=(k == 7))
            return r

        P.op("pe", f_mm, reads=[("sq", j)], writes=[("ps", bank)])

    def st_b(j):
        ts = tsl(j)
        P.op("act", lambda e, j=j, bank=banks[j]: e.activation(rstds[j], C.ps[:, bank, :], AF.Ln, bias=C.consts[:, C.eps_col:C.eps_col + 1], scale=1.0 / D),
             reads=[("ps", banks[j])], writes=[("rstd", j)])
        P.op("act", lambda e, j=j: e.activation(rstds[j], rstds[j], AF.Exp, scale=-0.5), reads=[("rstd", j)], writes=[("rstd", j)])
        for k in range(8):
            P.op("dve", lambda e, k=k, ts=ts, j=j: e.scalar_tensor_tensor(C.hT[:, k, ts], C.xT[:, k, ts], C.consts[:, gcol + k:gcol + k + 1], rstds[j],
                                                                     ALU.mult, ALU.mult),
                 reads=[("xT", k, j), ("rstd", j)], writes=[("hT", k, j)])

    for j in range(5):
        if j < 4:
            st_a(j)
        if j >= 1:
            st_b(j - 1)


FFN_GROUPS = [(0, 8), (8, 7), (15, 7)]
ARENA = 102 * 1024
import os
DBG_PHASE = int(os.environ.get('DBG_PHASE', '3'))
DBG_NB = int(os.environ.get('DBG_NB', '0'))
DBG_STEP = int(os.environ.get('DBG_STEP', '9'))


class Arena:
    def __init__(self, C):
        self.C = C
        self.off = 0

    def _take(self, n, esz):
        self.off = (self.off + 3) // 4 * 4
        o = self.off
        self.off += n * esz
        assert self.off <= ARENA, self.off
        return o

    def bf(self, *free):
        n = int(np.prod(free))
        o = self._take(n, 2)
        return self._shape(self.C.ar[:, o // 2:o // 2 + n], free)

    def f32(self, *free):
        n = int(np.prod(free))
        o = self._take(n, 4)
        return self._shape(self.C.ar_f[:, o // 4:o // 4 + n], free)

    def i32(self, *free):
        n = int(np.prod(free))
        o = self._take(n, 4)
        return self._shape(self.C.ar_i[:, o // 4:o // 4 + n], free)

    @staticmethod
    def _shape(v, free):
        if len(free) == 1:
            return v
        if len(free) == 2:
            return v.rearrange("p (a b) -> p a b", a=free[0])
        if len(free) == 3:
            return v.rearrange("p (a b c) -> p a b c", a=free[0], b=free[1])
        raise ValueError


def bc(ap, dims):
    return bass.AP(ap.tensor, ap.offset, [list(ap.ap[0])] + [list(d) for d in dims])


def ffn_alloc(C):
    A = Arena(C)
    C.aT = A.bf(8, T)
    C.wgu = [[A.bf(8, 512) for i in range(2)] for s in range(2)]
    C.wd_sb = A.bf(8, D)
    C.sg = [A.f32(512) for s in range(2)]
    C.sq = A.bf(8, 512)
    C.rstd = A.f32(512)
    C.rstd4 = C.ar_f[:, C.sq.offset // 2:C.sq.offset // 2 + 2048].rearrange("p (a b) -> p a b", a=4)


def ffn_stage(P, C, gcol, wg, wu, wd):
    rmsnorm_stage(P, C, gcol)
    wg_v = wg.rearrange("(k p) f -> p k f", p=128)
    wu_v = wu.rearrange("(k p) f -> p k f", p=128)
    wd_v = wd.rearrange("(c p) d -> p c d", p=128)
    for (c0, ng) in FFN_GROUPS:
        def f_wd(eng, c0=c0, ng=ng):
            return eng.dma_start(out=C.wd_sb[:, 0:ng, :], in_=wd_v[:, c0:c0 + ng, :])

        P.dma("pool", f_wd, "wd", writes=[("wd",)])
        pieces = []
        cc = 0
        while cc < ng:
            pn = min(4, ng - cc)
            pieces.append((cc, pn))
            cc += pn
        for (pc, pn) in pieces:
            slot = C.wslot
            C.wslot = (C.wslot + 1) % 2
            f0 = (c0 + pc) * 128

            def f_wg(eng, slot=slot, f0=f0, pn=pn):
                return eng.dma_start(out=C.wgu[slot][0][:, :, 0:pn * 128], in_=wg_v[:, :, f0:f0 + pn * 128])

            def f_wu(eng, slot=slot, f0=f0, pn=pn):
                return eng.dma_start(out=C.wgu[slot][1][:, :, 0:pn * 128], in_=wu_v[:, :, f0:f0 + pn * 128])

            P.dma("pool", f_wg, f"wg{slot}", writes=[("wg", slot)])
            P.dma("pool", f_wu, f"wu{slot}", writes=[("wu", slot)])
            for ci in range(pn):
                ca = pc + ci
                for j in range(4):
                    ts = tsl(j)
                    bg = C.bank()
                    bu = C.bank()

                    def f_mm(eng, slot=slot, ci=ci, ts=ts, bg=bg, bu=bu):
                        r = None
                        for k in range(8):
                            r = eng.matmul(C.ps[:, bg, :], C.wgu[slot][0][:, k, ci * 128:(ci + 1) * 128],
                                           C.hT[:, k, ts], start=(k == 0), stop=(k == 7))
                        for k in range(8):
                            r = eng.matmul(C.ps[:, bu, :], C.wgu[slot][1][:, k, ci * 128:(ci + 1) * 128],
                                           C.hT[:, k, ts], start=(k == 0), stop=(k == 7))
                        return r

                    P.op("pe", f_mm, reads=[("wg", slot), ("wu", slot)] + [("hT", k, j) for k in range(8)],
                         writes=[("ps", bg), ("ps", bu)])
                    ss = C.sgslot
                    C.sgslot = (C.sgslot + 1) % 2

                    def f_silu(eng, ss=ss, bg=bg):
                        return eng.activation(C.sg[ss][:, :], C.ps[:, bg, :], AF.Silu)

                    P.op("act", f_silu, reads=[("ps", bg)], writes=[("sg", ss)])

                    def f_mul(eng, ss=ss, bu=bu, ca=ca, ts=ts):
                        return eng.tensor_tensor(C.aT[:, ca, ts], C.sg[ss][:, :], C.ps[:, bu, :], ALU.mult)

                    P.op("dve", f_mul, reads=[("sg", ss), ("ps", bu)], writes=[("aT", ca, j)])
        for d in range(8):
            for j in range(4):
                ts = tsl(j)
                by = C.bank()

                def f_mm(eng, d=d, ts=ts, by=by, ng=ng):
                    r = None
                    for ca in range(ng):
                        r = eng.matmul(C.ps[:, by, :], C.wd_sb[:, ca, d * 128:(d + 1) * 128], C.aT[:, ca, ts],
                                       start=(ca == 0), stop=(ca == ng - 1))
                    return r

                P.op("pe", f_mm, reads=[("wd",)] + [("aT", ca, j) for ca in range(ng)], writes=[("ps", by)])

                def f_res(eng, d=d, ts=ts, by=by):
                    return eng.scalar_tensor_tensor(C.xT[:, d, ts], C.ps[:, by, :], 0.5, C.xT[:, d, ts],
                                                    ALU.mult, ALU.add)

                P.op("dve", f_res, reads=[("ps", by), ("xT", d, j)], writes=[("xT", d, j)])


def norm_heads64(P, C, bank, gcol_name, out_ap, sq, rstd):
    gc = C.ccols[gcol_name][0]

    def f_sq(eng):
        return eng.activation(sq, C.ps[:, bank, :], AF.Square)

    P.op("act", f_sq, reads=[("ps", bank)], writes=[("nsq",)])
    b2 = C.bank()

    def f_mm(eng):
        return eng.matmul(C.ps[:, b2, :], C.bd_bf[:, :], sq, start=True, stop=True)

    P.op("pe", f_mm, reads=[("nsq",), ("bd",)], writes=[("ps", b2)])

    def f_r1(eng):
        return eng.activation(rstd, C.ps[:, b2, :], AF.Ln, bias=C.consts[:, C.eps_col:C.eps_col + 1], scale=1.0 / 64)

    P.op("act", f_r1, reads=[("ps", b2)], writes=[("nrstd",)])

    def f_r2(eng):
        return eng.activation(rstd, rstd, AF.Exp, scale=-0.5)

    P.op("act", f_r2, reads=[("nrstd",)], writes=[("nrstd",)])

    def f_o(eng):
        return eng.scalar_tensor_tensor(out_ap, C.ps[:, bank, :], C.consts[:, gc:gc + 1], rstd, ALU.mult, ALU.mult)

    return f_o


def swa_stage(P, C, s):
    dr = C.dr
    w_in = C.need("o_w_in", [D, 2320], lambda inp: inp["o_w_in"][0])
    w_out = C.need("o_w_out", [D, D], lambda inp: inp["o_w_out"][0])
    A = Arena(C)
    wqkv = A.bf(8, 768)
    wk2 = A.bf(8, 2, 128)
    woS = A.bf(8, D)
    qT = A.bf(4, 512)
    kT2 = A.bf(2, T)
    Vaug = A.bf(16, 2, 128)
    oT = A.bf(8, 512)
    posB = A.f32(T)
    posBi = bass.AP(C.ar_i[:, 0:1].tensor, posB.offset, [list(posB.ap[0]), [1, T]])
    pk = A.f32(16)
    pki = A.i32(16)
    dist2 = [A.f32(128) for _ in range(2)]
    D82 = [A.f32(8, 128) for _ in range(2)]
    tmp2 = [A.f32(512) for _ in range(2)]
    Pe2 = [A.bf(512) for _ in range(2)]
    Pm = [A.bf(512) for _ in range(4)]
    sq = A.bf(512)
    rstd = A.f32(512)
    dn = A.f32(512)
    dn0 = A.f32(512)
    es = A.f32(8)
    cc = C.ccols
    w_in_v = w_in.rearrange("(k p) f -> p k f", p=128)

    P.dma("pool", lambda e: e.dma_start(out=wqkv, in_=w_in_v[:, :, 0:768]), "wA", writes=[("wqkv",)])

    def f_wk(e):
        r = []
        for g in range(2):
            for hh in range(2):
                r.append(e.dma_start(out=wk2[:, :, g, hh * 64:(hh + 1) * 64], in_=w_in_v[:, :, 512 + g * 64:512 + (g + 1) * 64]))
        return r

    P.dma("pool", f_wk, "wB", writes=[("wk2",)], n=4)
    P.dma("pool", lambda e: e.dma_start(out=woS[0:64, :, :], in_=w_out[0:512, :].rearrange("(h p) d -> p h d", p=64)),
          "wC", writes=[("woS",)])
    P.dma("sp", lambda e: e.dma_start(out=posBi, in_=bass.AP(C.pos.tensor, C.pos[s:s + 1, :].offset, [[0, 128], [1, T]])),
          "posB", writes=[("posB",)])
    P.dma("sp", lambda e: e.dma_start(out=pki, in_=C.posT[s]), "pk", writes=[("pki",)])
    P.op("dve", lambda e: e.tensor_copy(posB, posBi), reads=[("posB",)], writes=[("posB",)])
    P.op("dve", lambda e: e.tensor_copy(pk, pki), reads=[("pki",)], writes=[("pk",)])
    P.op("act", lambda e: e.activation(es, C.consts[:, cc["sinks"][0]:cc["sinks"][0] + 8], AF.Exp), reads=[("consts",)], writes=[("es",)])
    P.op("pool", lambda e: e.memset(Vaug[:, :, :, 64:128], 1.0), writes=[("Vaug", n) for n in range(16)])
    SLc = cc["SL"][0]

    for j in range(4):
        ts = tsl(j)
        for c in range(4):
            b = C.bank()

            def f_mm(eng, c=c, b=b, ts=ts):
                r = None
                for k in range(8):
                    r = eng.matmul(C.ps[:, b, :], wqkv[:, k, c * 128:(c + 1) * 128], C.hT[:, k, ts], start=(k == 0), stop=(k == 7))
                return r

            P.op("pe", f_mm, reads=[("wqkv",)] + [("hT", k, j) for k in range(8)], writes=[("ps", b)])
            f_o = norm_heads64(P, C, b, "o_qg2", qT[:, c, :], sq, rstd)
            P.op("dve", f_o, reads=[("ps", b), ("nrstd",)], writes=[("qT", c)])
        for g in range(2):
            b = C.bank()

            def f_mm(eng, g=g, b=b, ts=ts):
                r = None
                for k in range(8):
                    r = eng.matmul(C.ps[:, b, :], wk2[:, k, g, :], C.hT[:, k, ts], start=(k == 0), stop=(k == 7))
                return r

            P.op("pe", f_mm, reads=[("wk2",)] + [("hT", k, j) for k in range(8)], writes=[("ps", b)])
            f_o = norm_heads64(P, C, b, "o_kg2", kT2[:, g, ts], sq, rstd)
            P.op("dve", f_o, reads=[("ps", b), ("nrstd",)], writes=[("kT2", g, j)])
        for nl in range(4):
            n = 4 * j + nl
            b = C.bank()

            def f_mm(eng, n=n, b=b):
                r = None
                for k in range(8):
                    r = eng.matmul(C.ps[:, b, 0:128], C.hT[:, k, n * 128:(n + 1) * 128], wqkv[:, k, 640:768], start=(k == 0), stop=(k == 7))
                return r

            P.op("pe", f_mm, reads=[("wqkv",)] + [("hT", k, j) for k in range(8)], writes=[("ps", b)])

            def f_v(eng, n=n, b=b):
                return eng.tensor_copy(Vaug[:, n, :, 0:64], C.ps[:, b, 0:128].rearrange("p (g d) -> p g d", g=2))

            P.op("dve", f_v, reads=[("ps", b)], writes=[("Vaug", n)])
        items = []
        for nl in range(4):
            n = 4 * j + nl
            kbs = [n - 1, n] if n > 0 else [n]
            for kb in kbs:
                for g in range(2):
                    items.append((nl, n, kb, g, kb == kbs[0], kb == n))
        di = [0]
        cur = {}

        def s1(nl, n, kb, g, first, last):
            qs = slice(nl * 128, (nl + 1) * 128)
            if g == 0:
                k2 = di[0]
                di[0] = (di[0] + 1) % 2
                cur["k2"] = k2
                dd, d8 = dist2[k2], D82[k2]
                P.op("dve", lambda e, n=n, kb=kb, dd=dd: e.tensor_scalar(dd, posB[:, n * 128:(n + 1) * 128], pk[:, kb:kb + 1], None, ALU.subtract),
                     reads=[("posB",), ("pk",)], writes=[("dist", k2)])
                P.op("dve", lambda e, dd=dd: e.scalar_tensor_tensor(dd, dd, -1.0, dd, ALU.mult, ALU.max), reads=[("dist", k2)], writes=[("dist", k2)])
                P.op("pool", lambda e, dd=dd, d8=d8: e.tensor_tensor(d8, bc(dd, [[0, 8], [1, 128]]), bc(C.consts[:, SLc:SLc + 8], [[1, 8], [0, 128]]), ALU.mult),
                     reads=[("dist", k2), ("consts",)], writes=[("D8", k2)])
            k2 = cur["k2"]
            d8 = D82[k2]
            bA = C.bank()
            bB = C.bank()

            def f_sc(eng, g=g, bA=bA, bB=bB, kb=kb, qs=qs):
                r = None
                for hl in range(4):
                    c = 2 * g + hl // 2
                    hp = slice((hl % 2) * 64, (hl % 2) * 64 + 64)
                    bb = bA if hl % 2 == 0 else bB
                    r = eng.matmul(C.ps[:, bb, (hl // 2) * 128:(hl // 2 + 1) * 128], kT2[hp, g, kb * 128:(kb + 1) * 128],
                                   qT[hp, c, qs], start=True, stop=True)
                return r

            P.op("pe", f_sc, reads=[("kT2", g, kb // 4), ("qT", 2 * g), ("qT", 2 * g + 1)], writes=[("ps", bA), ("ps", bB)])
            ti = C.pmi % 2
            TM, PE_ = tmp2[ti], Pe2[ti]
            for par, bb in ((0, bA), (1, bB)):
                P.op("dve", lambda e, g=g, bb=bb, par=par, TM=TM, d8=d8: e.tensor_tensor(TM.rearrange("p (h f) -> p h f", h=4)[:, par:4:2, :],
                                                                                  d8[:, 4 * g + par:4 * g + 4:2, :],
                                                                                  C.ps[:, bb, 0:256].rearrange("p (h f) -> p h f", h=2), ALU.add),
                     reads=[("D8", k2), ("ps", bb)], writes=[("tmp", ti)])
            P.op("act", lambda e, TM=TM, PE_=PE_: e.activation(PE_, TM, AF.Exp, scale=0.125), reads=[("tmp", ti)], writes=[("Pe", ti)])
            pi = C.pmi
            C.pmi = (C.pmi + 1) % 4
            mcol = 0 if kb == n else 128
            P.op("pool", lambda e, pi=pi, mcol=mcol, PE_=PE_: e.tensor_tensor(Pm[pi].rearrange("p (h f) -> p h f", h=4), PE_.rearrange("p (h f) -> p h f", h=4),
                                                                        bc(C.mask[:, mcol:mcol + 128], [[0, 4], [1, 128]]), ALU.mult),
                 reads=[("Pe", ti), ("mask",)], writes=[("Pm", pi)])
            return pi

        def s2(item, pi):
            nl, n, kb, g, first, last = item
            qs = slice(nl * 128, (nl + 1) * 128)
            P.op("pe", lambda e, g=g, kb=kb, pi=pi, first=first, last=last: e.matmul(C.ps[:, g, :], Vaug[:, kb, g, :], Pm[pi], start=first, stop=last),
                 reads=[("Vaug", kb), ("Pm", pi)], writes=[("ps", g)])
            if last:
                def f_dn(eng, g=g):
                    return eng.tensor_tensor(dn[64:128, :].rearrange("p (h f) -> p h f", h=4),
                                             C.ps[64:128, g, :].rearrange("p (h f) -> p h f", h=4),
                                             bc(es[64:128, 4 * g:4 * g + 4], [[1, 4], [0, 128]]), ALU.add)

                P.op("dve", f_dn, reads=[("ps", g), ("es",)], writes=[("dn",)])
                P.op("act", lambda e: e.activation(dn[64:128, :], dn[64:128, :], AF.Ln), reads=[("dn",)], writes=[("dn",)])
                P.op("act", lambda e: e.activation(dn[64:128, :], dn[64:128, :], AF.Exp, scale=-1.0), reads=[("dn",)], writes=[("dn",)])
                P.op("dve", lambda e: e.tensor_copy(dn0[0:64, :], dn[64:128, :]), reads=[("dn",)], writes=[("dn0",)])

                def f_o(eng, g=g, qs=qs):
                    return eng.tensor_tensor(oT[0:64, 4 * g:4 * g + 4, qs], C.ps[0:64, g, :].rearrange("p (h f) -> p h f", h=4),
                                             dn0[0:64, :].rearrange("p (h f) -> p h f", h=4), ALU.mult)

                P.op("dve", f_o, reads=[("ps", g), ("dn0",)], writes=[("oT",)])

        pend = []
        for it in items:
            pend.append((it, s1(*it)))
            if len(pend) > 2:
                s2(*pend.pop(0))
        while pend:
            s2(*pend.pop(0))
        for d in range(8 if DBG_PHASE >= 3 else 0):
            b = C.bank()

            def f_mm(eng, d=d, b=b):
                r = None
                for h in range(8):
                    r = eng.matmul(C.ps[:, b, :], woS[0:64, h, d * 128:(d + 1) * 128], oT[0:64, h, :], start=(h == 0), stop=(h == 7))
                return r

            P.op("pe", f_mm, reads=[("woS",), ("oT",)], writes=[("ps", b)])

            def f_res(eng, d=d, b=b, ts=ts):
                return eng.tensor_tensor(C.xT[:, d, ts], C.ps[:, b, :], C.xT[:, d, ts], ALU.add)

            P.op("dve", f_res, reads=[("ps", b), ("xT", d, j)], writes=[("xT", d, j)])


def gla_stage(P, C, s):
    w_in = C.need("o_w_in", [D, 2320], lambda inp: inp["o_w_in"][0])
    w_out = C.need("o_w_out", [D, D], lambda inp: inp["o_w_out"][0])
    w_gb = C.need("o_w_gate_b", [16, 256], lambda inp: inp["o_w_gate_b"][0])
    cc = C.ccols
    A = Arena(C)
    wG = A.bf(8, 1552)
    wgb = A.bf(256)
    woG = A.bf(4, D)
    gaT = A.bf(512)
    ebuf = A.f32(512)
    lbuf = A.f32(512)
    cl = A.f32(2, 512)
    eb = A.f32(512)
    einv = A.f32(512)
    dend = A.f32(512)
    decs = A.f32(2, 8)
    nb = A.f32(2)
    q_dec = A.bf(2, 512)
    k_inv = A.bf(2, 512)
    k_end = A.bf(2, 512)
    grs = A.bf(4, 512)
    gv_tok = A.bf(4, 512)
    ket = A.bf(4, 2, 128)
    attm = A.bf(4, 128)
    S = [A.f32(128) for _ in range(2)]
    S_bf = [A.bf(128) for _ in range(2)]
    sq = A.bf(256)
    rstd = A.f32(256)
    ytmp = A.f32(256)
    oG = A.bf(4, 512)
    w_in_v = w_in.rearrange("(k p) f -> p k f", p=128)
    onec = C.consts[:, cc["one"][0]:cc["one"][0] + 1]
    glan = C.consts[:, cc["o_glan"][0]:cc["o_glan"][0] + 1]
    gbc = cc["o_gbias"][0]

    P.dma("pool", lambda e: e.dma_start(out=wG, in_=w_in_v[:, :, 768:2320]), "wA", writes=[("wG",)])
    P.dma("pool", lambda e: e.dma_start(out=wgb[0:16, :], in_=w_gb), "wB", writes=[("wgb",)])
    P.dma("pool", lambda e: e.dma_start(out=woG, in_=w_out[512:1024, :].rearrange("(c p) d -> p c d", p=128)), "wC", writes=[("woG",)])
    P.op("dve", lambda e: e.tensor_scalar(nb, C.consts[:, gbc:gbc + 2], -1.0, None, ALU.mult), reads=[("consts",)], writes=[("nb",)])
    for cp in range(2):
        P.op("dve", lambda e, cp=cp: e.memset(S[cp], 0.0), writes=[("S", cp)])
        P.op("dve", lambda e, cp=cp: e.memset(S_bf[cp], 0.0), writes=[("Sbf", cp)])

    def proj(cols, j, M=128):
        b = C.bank()
        ts = tsl(j)

        def f(eng):
            r = None
            for k in range(8):
                r = eng.matmul(C.ps[0:M, b, :], wG[:, k, cols], C.hT[:, k, ts], start=(k == 0), stop=(k == 7))
            return r

        P.op("pe", f, reads=[("wG",)] + [("hT", k, j) for k in range(8)], writes=[("ps", b)])
        return b

    for j in range(4):
        ts = tsl(j)
        b = proj(slice(1024, 1040), j, M=16)
        P.op("act", lambda e, b=b: e.copy(gaT[0:16, :], C.ps[0:16, b, :]), reads=[("ps", b)], writes=[("gaT",)])
        for cp in range(2):
            b = C.bank()
            P.op("pe", lambda e, b=b, cp=cp: e.matmul(C.ps[:, b, :], wgb[0:16, cp * 128:(cp + 1) * 128], gaT[0:16, :], start=True, stop=True),
                 reads=[("wgb",), ("gaT",)], writes=[("ps", b)])
            P.op("act", lambda e, b=b, cp=cp: e.activation(ebuf, C.ps[:, b, :], AF.Exp, bias=nb[:, cp:cp + 1], scale=-1.0),
                 reads=[("ps", b), ("nb",)], writes=[("ebuf",)])
            P.op("act", lambda e: e.activation(lbuf, ebuf, AF.Ln, bias=onec, scale=1.0), reads=[("ebuf",)], writes=[("lbuf",)])
            P.op("dve", lambda e, cp=cp: e.tensor_tensor_scan(cl[:, cp, :], C.maskb[:, 0:512], lbuf, 0.0, ALU.mult, ALU.add),
                 reads=[("lbuf",), ("mask",)], writes=[("cl", cp)])
            P.op("act", lambda e, cp=cp: e.activation(eb, cl[:, cp, :], AF.Exp, scale=-1.0 / 16), reads=[("cl", cp)], writes=[("eb",)])
            P.op("act", lambda e, cp=cp: e.activation(einv, cl[:, cp, :], AF.Exp, scale=1.0 / 16), reads=[("cl", cp)], writes=[("einv",)])
            clv = cl[:, cp, :]
            clast_b = bc(clv[:, 63:64], [[64, 8], [0, 64]])
            clast = bc(clv[:, 63:64], [[64, 8]])
            P.op("dve", lambda e, cp=cp, clast_b=clast_b: e.tensor_tensor(dend.rearrange("p (c l) -> p c l", c=8),
                                                                       cl[:, cp, :].rearrange("p (c l) -> p c l", c=8), clast_b, ALU.subtract),
                 reads=[("cl", cp)], writes=[("dend",)])
            P.op("act", lambda e: e.activation(dend, dend, AF.Exp, scale=1.0 / 16), reads=[("dend",)], writes=[("dend",)])
            P.op("act", lambda e, cp=cp, clast=clast: e.activation(decs[:, cp, :], clast, AF.Exp, scale=-1.0 / 16),
                 reads=[("cl", cp)], writes=[("decs", cp)])
            b = proj(slice(cp * 128, (cp + 1) * 128), j)
            P.op("dve", lambda e, b=b, cp=cp: e.scalar_tensor_tensor(q_dec[:, cp, :], C.ps[:, b, :], 0.125, eb, ALU.mult, ALU.mult),
                 reads=[("ps", b), ("eb",)], writes=[("q_dec", cp)])
            b = proj(slice(256 + cp * 128, 256 + (cp + 1) * 128), j)
            P.op("dve", lambda e, b=b, cp=cp: e.tensor_tensor(k_inv[:, cp, :], C.ps[:, b, :], einv, ALU.mult),
                 reads=[("ps", b), ("einv",)], writes=[("k_inv", cp)])
            P.op("dve", lambda e, b=b, cp=cp: e.tensor_tensor(k_end[:, cp, :], C.ps[:, b, :], dend, ALU.mult),
                 reads=[("ps", b), ("dend",)], writes=[("k_end", cp)])
        for hh in range(4):
            b = proj(slice(1040 + hh * 128, 1040 + (hh + 1) * 128), j)
            P.op("act", lambda e, b=b, hh=hh: e.activation(grs[:, hh, :], C.ps[:, b, :], AF.Silu), reads=[("ps", b)], writes=[("grs", hh)])
        for nl in range(4):
            n = 4 * j + nl
            b = C.bank()

            def f_gv(eng, b=b, n=n):
                r = None
                for k in range(8):
                    r = eng.matmul(C.ps[:, b, :], C.hT[:, k, n * 128:(n + 1) * 128], wG[:, k, 512:1024], start=(k == 0), stop=(k == 7))
                return r

            P.op("pe", f_gv, reads=[("wG",)] + [("hT", k, j) for k in range(8)], writes=[("ps", b)])
            P.op("act", lambda e, b=b, nl=nl: e.copy(gv_tok[:, nl, :], C.ps[:, b, :]), reads=[("ps", b)], writes=[("gv", nl)])
            for cp in range(2):
                b = C.bank()
                P.op("pe", lambda e, b=b, cp=cp, nl=nl: e.matmul(C.ps[:, b, 0:128], k_end[:, cp, nl * 128:(nl + 1) * 128], C.ident_bf[:, :], start=True, stop=True),
                     reads=[("k_end", cp), ("ident",)], writes=[("ps", b)])
                P.op("dve", lambda e, b=b, cp=cp, nl=nl: e.tensor_copy(ket[:, nl, cp, :], C.ps[:, b, 0:128]), reads=[("ps", b)], writes=[("ket", nl, cp)])
        for nl in range(4):
            bs = slice(nl * 128, (nl + 1) * 128)
            bX = C.bank()
            bY = C.bank()

            def f_att(eng, bX=bX, bY=bY, bs=bs):
                r = None
                for h in range(4):
                    cp, half = h // 2, h % 2
                    hp = slice(half * 64, half * 64 + 64)
                    bb = bX if half == 0 else bY
                    r = eng.matmul(C.ps[:, bb, cp * 128:(cp + 1) * 128], k_inv[hp, cp, bs], q_dec[hp, cp, bs], start=True, stop=True)
                return r

            P.op("pe", f_att, reads=[("k_inv", 0), ("k_inv", 1), ("q_dec", 0), ("q_dec", 1)], writes=[("ps", bX), ("ps", bY)])
            for half, bb in ((0, bX), (1, bY)):
                P.op("dve", lambda e, half=half, bb=bb: e.tensor_tensor(attm[:, half:4:2, :], C.ps[:, bb, 0:256].rearrange("p (h f) -> p h f", h=2),
                                                                   bc(C.mask[:, 384:512], [[0, 2], [1, 128]]), ALU.mult),
                     reads=[("ps", bb), ("mask",)], writes=[("attm", half)])
            for x in range(2):
                xs = slice(nl * 128 + x * 64, nl * 128 + x * 64 + 64)
                rows = slice(x * 64, x * 64 + 64)
                ci = nl * 2 + x

                def f_inter(eng, xs=xs, x=x):
                    r = None
                    for h in range(4):
                        cp, half = h // 2, h % 2
                        hp = slice(half * 64, half * 64 + 64)
                        r = eng.matmul(C.ps[:, half, cp * 128 + x * 64:cp * 128 + x * 64 + 64], S_bf[cp][hp, :], q_dec[hp, cp, xs],
                                       start=(x == 0 and cp == 0), stop=False, skip_group_check=True)
                    return r

                P.op("pe", f_inter, reads=[("Sbf", 0), ("Sbf", 1), ("q_dec", 0), ("q_dec", 1)], writes=[("ps", 0), ("ps", 1)])
                for cp in range(2):
                    bk = C.bank()
                    P.op("pe", lambda e, bk=bk, cp=cp, rows=rows, nl=nl: e.matmul(C.ps[:, bk, 0:256], ket[rows, nl, cp, :],
                                                                            gv_tok[rows, nl, cp * 256:(cp + 1) * 256], start=True, stop=True),
                         reads=[("ket", nl, cp), ("gv", nl)], writes=[("ps", bk)])
                    for half in range(2):
                        hp = slice(half * 64, half * 64 + 64)
                        P.op("dve", lambda e, bk=bk, cp=cp, hp=hp, half=half, ci=ci: e.scalar_tensor_tensor(
                            S[cp][hp, :], S[cp][hp, :], decs[hp, cp, ci:ci + 1], C.ps[hp, bk, half * 128:(half + 1) * 128], ALU.mult, ALU.add),
                             reads=[("ps", bk), ("decs", cp), ("S", cp)], writes=[("S", cp)])
                    P.op("act", lambda e, cp=cp: e.copy(S_bf[cp], S[cp]), reads=[("S", cp)], writes=[("Sbf", cp)])

            def f_intra(eng, nl=nl):
                r = None
                for h in range(4):
                    cp, half = h // 2, h % 2
                    r = eng.matmul(C.ps[:, half, cp * 128:(cp + 1) * 128], gv_tok[:, nl, h * 128:(h + 1) * 128], attm[:, h, :], start=False, stop=True,
                                   skip_group_check=True)
                return r

            P.op("pe", f_intra, reads=[("gv", nl), ("attm", 0), ("attm", 1)], writes=[("ps", 0), ("ps", 1)])
            for half in range(2):
                P.op("act", lambda e, half=half: e.activation(sq, C.ps[:, half, 0:256], AF.Square), reads=[("ps", half)], writes=[("gsq",)])
                b2 = C.bank()
                P.op("pe", lambda e, b2=b2: e.matmul(C.ps[:, b2, 0:256], C.ones_bf[:, :], sq, start=True, stop=True), reads=[("gsq",), ("ones",)], writes=[("ps", b2)])
                P.op("act", lambda e, b2=b2: e.activation(rstd, C.ps[:, b2, 0:256], AF.Ln, bias=C.consts[:, C.eps_col:C.eps_col + 1], scale=1.0 / 128),
                     reads=[("ps", b2)], writes=[("grstd",)])
                P.op("act", lambda e: e.activation(rstd, rstd, AF.Exp, scale=-0.5), reads=[("grstd",)], writes=[("grstd",)])
                P.op("dve", lambda e, half=half: e.scalar_tensor_tensor(ytmp, C.ps[:, half, 0:256], glan, rstd, ALU.mult, ALU.mult),
                     reads=[("ps", half), ("grstd",)], writes=[("ytmp",)])
                P.op("dve", lambda e, half=half, bs=bs: e.tensor_tensor(oG[:, half:4:2, bs], ytmp.rearrange("p (h f) -> p h f", h=2),
                                                                   grs[:, half:4:2, bs], ALU.mult),
                     reads=[("ytmp",), ("grs", half), ("grs", half + 2)], writes=[("oG",)])
        for d in range(8):
            b = C.bank()

            def f_mm(eng, d=d, b=b):
                r = None
                for c in range(4):
                    r = eng.matmul(C.ps[:, b, :], woG[:, c, d * 128:(d + 1) * 128], oG[:, c, :], start=(c == 0), stop=(c == 3))
                return r

            P.op("pe", f_mm, reads=[("woG",), ("oG",)], writes=[("ps", b)])
            P.op("dve", lambda e, d=d, b=b, ts=ts: e.tensor_tensor(C.xT[:, d, ts], C.ps[:, b, :], C.xT[:, d, ts], ALU.add),
                 reads=[("ps", b), ("xT", d, j)], writes=[("xT", d, j)])


def ssd_stage(P, C, s):
    w_in = C.need("e_w_in", [D, 3760], lambda inp: inp["e_w_in"][0])
    w_out = C.need("e_w_out", [1536, D], lambda inp: inp["e_w_out"][0])
    cc = C.ccols
    A = Arena(C)
    wp = [A.bf(8, 256) for _ in range(2)]
    wdt = A.bf(8, 16)
    woS = A.bf(8, D)
    zs = A.bf(8, 512)
    xsT = A.bf(8, 512)
    BT = A.bf(4, 512)
    CT = A.bf(4, 512)
    rb = [A.f32(515) for _ in range(2)]
    acc = [A.f32(512) for _ in range(2)]
    halo = A.f32(16, 3)
    aneg = A.f32(16)
    x1s = [A.f32(16) for _ in range(2)]
    dts = [A.f32(16) for _ in range(2)]
    a_s = [A.f32(16) for _ in range(2)]
    csts = [A.f32(16) for _ in range(2)]
    ncss = [A.f32(16) for _ in range(2)]
    dss = [A.f32(16) for _ in range(2)]
    cdecs = [A.f32(16) for _ in range(2)]
    dtdss = [A.f32(16) for _ in range(2)]
    xds = [A.bf(1024) for _ in range(2)]
    xdws = [A.bf(1024) for _ in range(2)]
    B_toks = [A.bf(4, 128) for _ in range(2)]
    CBss = [A.f32(4, 128) for _ in range(2)]
    Ecs = [A.f32(128) for _ in range(3)]
    E = [A.f32(128) for _ in range(3)]
    Coff = [A.bf(128) for _ in range(3)]
    Mh = [A.bf(128) for _ in range(3)]
    st = A.f32(1024)
    st_bf = A.bf(1024)
    yg = A.f32(8, 128)
    sq = A.bf(8, 128)
    rstd = A.f32(512)
    oS = A.bf(8, 512)
    w_in_v = w_in.rearrange("(k p) f -> p k f", p=128)
    onec = C.consts[:, cc["one"][0]:cc["one"][0] + 1]
    epsc = C.consts[:, C.eps_col:C.eps_col + 1]
    cwc, cbc, dsk, ssn = cc["e_convw"][0], cc["e_convb"][0], cc["e_dskip"][0], cc["e_ssmn"][0]
    MC = C.mask[:, 0:128]
    NEG = C.mask[:, 640:768]
    IDf = C.mask[:, 512:640]

    P.dma("pool", lambda e: e.dma_start(out=wdt, in_=w_in_v[:, :, 3072:3088]), "wB", writes=[("wdt",)])
    P.dma("pool", lambda e: e.dma_start(out=woS, in_=w_out[0:1024, :].rearrange("(c p) d -> p c d", p=128)), "wC", writes=[("woS",)])
    P.op("act", lambda e: e.activation(aneg, C.consts[:, cc["e_alog"][0]:cc["e_alog"][0] + 16], AF.Exp), reads=[("consts",)], writes=[("aneg",)])
    P.op("dve", lambda e: e.tensor_scalar(aneg, aneg, -1.0, None, ALU.mult), reads=[("aneg",)], writes=[("aneg",)])
    P.op("dve", lambda e: e.memset(st, 0.0), writes=[("st",)])
    P.op("dve", lambda e: e.memset(st_bf, 0.0), writes=[("st_bf",)])
    P.op("dve", lambda e: e.memset(halo, 0.0), writes=[("halo",)])
    ws = [0]
    rbi = [0]

    for j in range(4):
        ts = tsl(j)
        for pi in range(12):
            slot = ws[0]
            ws[0] = (ws[0] + 1) % 2
            P.dma("pool", lambda e, slot=slot, pi=pi: e.dma_start(out=wp[slot], in_=w_in_v[:, :, pi * 256:(pi + 1) * 256]),
                  f"wp{slot}", writes=[("wp", slot)])
            for ci in range(2):
                c = pi * 2 + ci
                b = C.bank()

                def f_mm(eng, slot=slot, ci=ci, b=b, ts=ts):
                    r = None
                    for k in range(8):
                        r = eng.matmul(C.ps[:, b, :], wp[slot][:, k, ci * 128:(ci + 1) * 128], C.hT[:, k, ts], start=(k == 0), stop=(k == 7))
                    return r

                P.op("pe", f_mm, reads=[("wp", slot)] + [("hT", k, j) for k in range(8)], writes=[("ps", b)])
                if c < 8:
                    P.op("act", lambda e, b=b, c=c: e.activation(zs[:, c, :], C.ps[:, b, :], AF.Silu), reads=[("ps", b)], writes=[("zs", c)])
                    continue
                xc = c - 8
                ri = rbi[0]
                rbi[0] = (rbi[0] + 1) % 2
                R, AC = rb[ri], acc[ri]
                P.op("act", lambda e, b=b, R=R: e.copy(R[:, 3:515], C.ps[:, b, :]), reads=[("ps", b)], writes=[("rb", ri)])
                P.op("dve", lambda e, R=R, xc=xc: e.tensor_copy(R[:, 0:3], halo[:, xc, :]), reads=[("halo",), ("rb", ri)], writes=[("rb", ri)])

                def wcol(xc, t):
                    return C.consts[:, cwc + xc * 4 + t:cwc + xc * 4 + t + 1]

                P.op("dve", lambda e, R=R, AC=AC, xc=xc: e.tensor_scalar(AC, R[:, 3:515], wcol(xc, 3), None, ALU.mult),
                     reads=[("rb", ri)], writes=[("acc", ri)])
                for t in (2, 1, 0):
                    P.op("dve", lambda e, R=R, AC=AC, xc=xc, t=t: e.scalar_tensor_tensor(AC, R[:, t:t + 512], wcol(xc, t), AC, ALU.mult, ALU.add),
                         reads=[("rb", ri), ("acc", ri)], writes=[("acc", ri)])
                P.op("dve", lambda e, R=R, xc=xc: e.tensor_copy(halo[:, xc, :], R[:, 512:515]), reads=[("rb", ri), ("halo",)], writes=[("halo",)])
                if xc < 8:
                    dest, key = xsT[:, xc, :], ("xsT", xc)
                elif xc < 12:
                    dest, key = BT[:, xc - 8, :], ("BT", xc - 8)
                else:
                    dest, key = CT[:, xc - 12, :], ("CT", xc - 12)
                P.op("act", lambda e, AC=AC, dest=dest, xc=xc: e.activation(dest, AC, AF.Silu, bias=C.consts[:, cbc + xc:cbc + xc + 1], scale=1.0),
                     reads=[("acc", ri)], writes=[key])
        def prologue(nl):
            pp = nl % 2
            n = 4 * j + nl
            bs = slice(nl * 128, (nl + 1) * 128)
            x1, dt, a_, cst, ncs, ds, cdec, dtds = (x1s[pp], dts[pp], a_s[pp], csts[pp], ncss[pp], dss[pp], cdecs[pp], dtdss[pp])
            xd, xdw, B_tok, CBs = xds[pp], xdws[pp], B_toks[pp], CBss[pp]
            b = C.bank()

            def f_dt(eng, b=b, n=n):
                r = None
                for k in range(8):
                    r = eng.matmul(C.ps[:, b, 0:16], C.hT[:, k, n * 128:(n + 1) * 128], wdt[:, k, :], start=(k == 0), stop=(k == 7))
                return r

            P.op("pe", f_dt, reads=[("wdt",)] + [("hT", k, j) for k in range(8)], writes=[("ps", b)])
            yield
            P.op("dve", lambda e, b=b: e.tensor_tensor(x1, C.ps[:, b, 0:16], C.consts[:, cc["e_dtb"][0]:cc["e_dtb"][0] + 16], ALU.add),
                 reads=[("ps", b)], writes=[("x1", pp)])
            yield
            P.op("act", lambda e: e.activation(x1, x1, AF.Exp), reads=[("x1", pp)], writes=[("x1", pp)])
            P.op("act", lambda e: e.activation(dt, x1, AF.Ln, bias=onec, scale=1.0), reads=[("x1", pp)], writes=[("dt", pp)])
            yield
            P.op("dve", lambda e: e.tensor_tensor(a_, dt, aneg, ALU.mult), reads=[("dt", pp), ("aneg",)], writes=[("a", pp)])
            yield
            b = C.bank()

            def f_cs(eng, b=b):
                eng.matmul(C.ps[:, b, 0:16], MC, a_, start=True, stop=True)
                return eng.matmul(C.ps[:, b, 16:32], bc(onec, [[0, 128]]), a_, start=True, stop=True)

            P.op("pe", f_cs, reads=[("a", pp), ("mask",)], writes=[("ps", b)])
            yield
            P.op("dve", lambda e, b=b: e.tensor_copy(cst, C.ps[:, b, 0:16]), reads=[("ps", b)], writes=[("cst", pp)])
            P.op("dve", lambda e: e.tensor_scalar(ncs, cst, -1.0, None, ALU.mult), reads=[("cst", pp)], writes=[("ncs", pp)])
            yield
            P.op("dve", lambda e, b=b: e.tensor_tensor(ds, C.ps[:, b, 16:32], cst, ALU.subtract), reads=[("ps", b), ("cst", pp)], writes=[("ds", pp)])
            yield
            P.op("act", lambda e: e.activation(ds, ds, AF.Exp), reads=[("ds", pp)], writes=[("ds", pp)])
            P.op("act", lambda e, b=b: e.activation(cdec, C.ps[:, b, 16:32], AF.Exp), reads=[("ps", b)], writes=[("cdec", pp)])
            yield
            P.op("dve", lambda e: e.tensor_tensor(dtds, dt, ds, ALU.mult), reads=[("dt", pp), ("ds", pp)], writes=[("dtds", pp)])
            yield
            for hb in range(2):
                b = C.bank()

                def f_tr(eng, b=b, hb=hb, bs=bs):
                    r = None
                    for cq in range(4):
                        c = hb * 4 + cq
                        r = eng.matmul(C.ps[:, b, cq * 128:(cq + 1) * 128], xsT[:, c, bs], C.ident_bf[:, :], start=True, stop=True)
                    return r

                P.op("pe", f_tr, reads=[("xsT", hb * 4 + q) for q in range(4)] + [("ident",)], writes=[("ps", b)])
                yield
                P.op("dve", lambda e, b=b, hb=hb: e.tensor_tensor(xd[:, hb * 512:(hb + 1) * 512].rearrange("p (h q) -> p h q", h=8),
                                                             C.ps[:, b, :].rearrange("p (h q) -> p h q", h=8),
                                                             bc(dt[:, hb * 8:hb * 8 + 8], [[1, 8], [0, 64]]), ALU.mult),
                     reads=[("ps", b), ("dt", pp)], writes=[("xd", pp, hb)])
                yield
                P.op("dve", lambda e, b=b, hb=hb: e.tensor_tensor(xdw[:, hb * 512:(hb + 1) * 512].rearrange("p (h q) -> p h q", h=8),
                                                             C.ps[:, b, :].rearrange("p (h q) -> p h q", h=8),
                                                             bc(dtds[:, hb * 8:hb * 8 + 8], [[1, 8], [0, 64]]), ALU.mult),
                     reads=[("ps", b), ("dtds", pp)], writes=[("xdw", pp, hb)])
                yield
            b = C.bank()

            def f_trb(eng, b=b, bs=bs):
                r = None
                for g in range(4):
                    r = eng.matmul(C.ps[:, b, g * 128:(g + 1) * 128], BT[:, g, bs], C.ident_bf[:, :], start=True, stop=True)
                return r

            P.op("pe", f_trb, reads=[("BT", g) for g in range(4)] + [("ident",)], writes=[("ps", b)])
            yield
            P.op("act", lambda e, b=b: e.copy(B_tok.rearrange("p g n -> p (g n)"), C.ps[:, b, :]), reads=[("ps", b)], writes=[("B_tok", pp)])
            yield
            b = C.bank()

            def f_cb(eng, b=b, bs=bs):
                r = None
                for g in range(4):
                    r = eng.matmul(C.ps[:, b, g * 128:(g + 1) * 128], BT[:, g, bs], CT[:, g, bs], start=True, stop=True)
                return r

            P.op("pe", f_cb, reads=[("BT", g) for g in range(4)] + [("CT", g) for g in range(4)], writes=[("ps", b)])
            yield
            P.op("act", lambda e, b=b: e.copy(CBs.rearrange("p g n -> p (g n)"), C.ps[:, b, :]), reads=[("ps", b)], writes=[("CBs", pp)])
            yield

        gen = prologue(0)
        for _ in gen:
            pass
        for nl in range(4):
            pp = nl % 2
            bs = slice(nl * 128, (nl + 1) * 128)
            a_, ncs, cdec = a_s[pp], ncss[pp], cdecs[pp]
            xd, xdw, B_tok, CBs = xds[pp], xdws[pp], B_toks[pp], CBss[pp]
            gen = prologue(nl + 1) if nl + 1 < 4 else iter(())

            def ssd_s1(h, bs=bs, pp=pp, a_=a_, ncs=ncs, CBs=CBs):
                g = h // 4
                hi = h % 3
                b = C.bank()

                def f_csb(eng, b=b, h=h):
                    al = bc(a_[:, h:h + 1], [[0, 128]])
                    eng.matmul(C.ps[:, b, 0:128], al, MC, start=True, stop=True)
                    eng.matmul(C.ps[:, b, 128:256], al, MC, start=False, stop=False, skip_group_check=True)
                    return eng.matmul(C.ps[:, b, 128:256], IDf, NEG, start=False, stop=True, skip_group_check=True)

                P.op("pe", f_csb, reads=[("a", pp), ("mask",)], writes=[("ps", b)])
                P.op("act", lambda e, b=b, hi=hi: e.activation(Ecs[hi], C.ps[:, b, 0:128], AF.Exp), reads=[("ps", b)], writes=[("Ecs", hi)])
                P.op("act", lambda e, b=b, hi=hi, h=h: e.activation(E[hi], C.ps[:, b, 128:256], AF.Exp, bias=ncs[:, h:h + 1], scale=1.0),
                     reads=[("ps", b), ("ncs", pp)], writes=[("E", hi)])
                P.op("pool", lambda e, hi=hi, g=g, bs=bs: e.tensor_tensor(Coff[hi], CT[:, g, bs], Ecs[hi], ALU.mult),
                     reads=[("Ecs", hi), ("CT", g)], writes=[("Coff", hi)])
                P.op("dve", lambda e, hi=hi, g=g: e.tensor_tensor(Mh[hi], E[hi], CBs[:, g, :], ALU.mult),
                     reads=[("E", hi), ("CBs", pp)], writes=[("Mh", hi)])

            def ssd_s2(h, pp=pp, xd=xd):
                hi = h % 3

                def f_y(eng, h=h, hi=hi):
                    yb = h // 8
                    col = ((h % 8) // 2) * 128
                    first = (h % 8) < 2
                    if h % 2 == 0:
                        out = C.ps[0:64, yb, col:col + 128]
                        kw = {}
                    else:
                        out = C.ps[64:128, yb, col:col + 128]
                        kw = {"tile_position": (0, 64)}
                    eng.matmul(out, xd[:, h * 64:(h + 1) * 64], Mh[hi], start=first, stop=False, skip_group_check=True, **kw)
                    return eng.matmul(out, st_bf[:, h * 64:(h + 1) * 64], Coff[hi], start=False, stop=True, skip_group_check=True, **kw)

                P.op("pe", f_y, reads=[("xd", pp, h // 8), ("Mh", hi), ("st_bf",), ("Coff", hi)], writes=[("ps", h // 8)])

            for h in range(16 + 2):
                if h < 16:
                    ssd_s1(h)
                if h >= 2:
                    ssd_s2(h - 2)
                if h >= 2:
                    next(gen, None)
                    next(gen, None)
            for _ in gen:
                pass
            bst = [C.bank(), C.bank()]

            def f_st(eng, bst=bst, B_tok=B_tok, xdw=xdw):
                r = None
                for g in range(4):
                    r = eng.matmul(C.ps[:, bst[g // 2], (g % 2) * 256:(g % 2) * 256 + 256], B_tok[:, g, :], xdw[:, g * 256:(g + 1) * 256],
                                   start=True, stop=True)
                return r

            P.op("pe", f_st, reads=[("B_tok", pp), ("xdw", pp, 0), ("xdw", pp, 1)], writes=[("ps", bst[0]), ("ps", bst[1])])
            P.op("dve", lambda e, cdec=cdec: e.tensor_tensor(st.rearrange("p (h q) -> p h q", h=16), st.rearrange("p (h q) -> p h q", h=16),
                                                        bc(cdec, [[1, 16], [0, 64]]), ALU.mult), reads=[("st",), ("cdec", pp)], writes=[("st",)])
            for hb in range(2):
                P.op("dve", lambda e, hb=hb, bst=bst: e.tensor_tensor(st[:, hb * 512:(hb + 1) * 512], st[:, hb * 512:(hb + 1) * 512], C.ps[:, bst[hb], :], ALU.add),
                     reads=[("st",), ("ps", bst[hb])], writes=[("st",)])
            P.op("act", lambda e: e.copy(st_bf, st), reads=[("st",)], writes=[("st_bf",)])
            for c in range(8):
                yb, col = c // 4, (c % 4) * 128
                P.op("dve", lambda e, c=c, yb=yb, col=col, bs=bs: e.scalar_tensor_tensor(yg[:, c, :], xsT[:, c, bs], C.consts[:, dsk + c:dsk + c + 1],
                                                                                C.ps[:, yb, col:col + 128], ALU.mult, ALU.add),
                     reads=[("xsT", c), ("ps", yb)], writes=[("yg", c)])
                P.op("pool", lambda e, c=c, bs=bs: e.tensor_tensor(yg[:, c, :], yg[:, c, :], zs[:, c, bs], ALU.mult),
                     reads=[("yg", c), ("zs", c)], writes=[("yg", c)])
            P.op("act", lambda e: e.activation(sq, yg, AF.Square), reads=[("yg", c) for c in range(8)], writes=[("ssq",)])
            b = C.bank()

            def f_ss(eng, b=b):
                r = None
                for gi in range(4):
                    eng.matmul(C.ps[:, b, gi * 128:(gi + 1) * 128], C.ones_bf[:, :], sq[:, 2 * gi, :], start=(gi == 0), stop=False, skip_group_check=True)
                    r = eng.matmul(C.ps[:, b, gi * 128:(gi + 1) * 128], C.ones_bf[:, :], sq[:, 2 * gi + 1, :], start=False, stop=True, skip_group_check=True)
                return r

            P.op("pe", f_ss, reads=[("ssq",), ("ones",)], writes=[("ps", b)])
            P.op("act", lambda e, b=b: e.activation(rstd, C.ps[:, b, :], AF.Ln, bias=epsc, scale=1.0 / 256), reads=[("ps", b)], writes=[("srstd",)])
            P.op("act", lambda e: e.activation(rstd, rstd, AF.Exp, scale=-0.5), reads=[("srstd",)], writes=[("srstd",)])
            for c in range(8):
                P.op("dve", lambda e, c=c, bs=bs: e.scalar_tensor_tensor(oS[:, c, bs], yg[:, c, :], C.consts[:, ssn + c:ssn + c + 1],
                                                                    rstd[:, (c // 2) * 128:(c // 2 + 1) * 128], ALU.mult, ALU.mult),
                     reads=[("yg", c), ("srstd",)], writes=[("oS",)])
        for d in range(8):
            b = C.bank()

            def f_mm(eng, d=d, b=b):
                r = None
                for c in range(8):
                    r = eng.matmul(C.ps[:, b, :], woS[:, c, d * 128:(d + 1) * 128], oS[:, c, :], start=(c == 0), stop=(c == 7))
                return r

            P.op("pe", f_mm, reads=[("woS",), ("oS",)], writes=[("ps", b)])
            P.op("dve", lambda e, d=d, b=b, ts=ts: e.tensor_tensor(C.xT[:, d, ts], C.ps[:, b, :], C.xT[:, d, ts], ALU.add),
                 reads=[("ps", b), ("xT", d, j)], writes=[("xT", d, j)])


def rope_tables(P, C, s, j, posi, ang, tmpf, tmpi, C96, S96):
    cc = C.ccols
    invf = C.consts[:, cc["invf"][0]:cc["invf"][0] + 1]
    sgn = C.consts[:, cc["sgn"][0]:cc["sgn"][0] + 1]
    P.dma("sp", lambda e: e.dma_start(out=posi, in_=bass.AP(C.pos.tensor, C.pos[s:s + 1, j * 512:(j + 1) * 512].offset, [[0, 128], [1, 512]])),
          "posB", writes=[("posi",)])
    P.op("dve", lambda e: e.tensor_copy(ang, posi), reads=[("posi",)], writes=[("ang",)])
    P.op("dve", lambda e: e.tensor_scalar(ang, ang, invf, None, ALU.mult), reads=[("ang",)], writes=[("ang",)])

    def frac(buf):
        P.op("dve", lambda e: e.tensor_copy(tmpi, buf), reads=[("ang",)], writes=[("tmpi",)])
        P.op("dve", lambda e: e.tensor_copy(tmpf, tmpi), reads=[("tmpi",)], writes=[("tmpf",)])
        P.op("dve", lambda e: e.tensor_tensor(buf, buf, tmpf, ALU.subtract), reads=[("tmpf",), ("ang",)], writes=[("ang",)])
        P.op("dve", lambda e: e.tensor_single_scalar(tmpf, buf, 0.5, ALU.is_gt), reads=[("ang",)], writes=[("tmpf",)])
        P.op("dve", lambda e: e.tensor_tensor(buf, buf, tmpf, ALU.subtract), reads=[("tmpf",), ("ang",)], writes=[("ang",)])
        P.op("dve", lambda e: e.tensor_single_scalar(tmpf, buf, -0.5, ALU.is_lt), reads=[("ang",)], writes=[("tmpf",)])
        P.op("dve", lambda e: e.tensor_tensor(buf, buf, tmpf, ALU.add), reads=[("tmpf",), ("ang",)], writes=[("ang",)])

    frac(ang)
    P.op("act", lambda e: e.activation(S96, ang, AF.Sin, scale=float(2 * np.pi)), reads=[("ang",)], writes=[("S96",)])
    P.op("dve", lambda e: e.tensor_scalar(S96, S96, sgn, None, ALU.mult), reads=[("S96",)], writes=[("S96",)])
    P.op("dve", lambda e: e.tensor_scalar(ang, ang, 0.25, None, ALU.add), reads=[("ang",), ("S96",)], writes=[("ang",)])
    frac(ang)
    P.op("act", lambda e: e.activation(C96, ang, AF.Sin, scale=float(2 * np.pi)), reads=[("ang",)], writes=[("C96",)])


def mla_kv_stage(P, C, s):
    w_in = C.need("e_w_in", [D, 3760], lambda inp: inp["e_w_in"][0])
    w_kvb = C.need("e_w_kv_b", [256, 1024], lambda inp: inp["e_w_kv_b"][0])
    cc = C.ccols
    A = Arena(C)
    kT = A.bf(8, T)
    V_tok = A.bf(16, 512)
    wA = A.bf(8, 288)
    wpp = A.bf(8, 32)
    wkvb = A.bf(2, 1024)
    kvn = A.bf(2, 512)
    sq = A.bf(512)
    sqk = A.bf(512)
    rstd = A.f32(512)
    posi = A.i32(512)
    ang = A.f32(512)
    tmpf = A.f32(512)
    tmpi = A.i32(512)
    C96 = A.f32(512)
    S96 = A.f32(512)
    kr = A.f32(512)
    t1 = A.f32(512)
    w_in_v = w_in.rearrange("(k p) f -> p k f", p=128)
    epsc = C.consts[:, C.eps_col:C.eps_col + 1]
    kvan = cc["e_kvan"][0]
    gk = C.consts[:, cc["gk"][0]:cc["gk"][0] + 1]
    gkp = C.consts[:, cc["gkp"][0]:cc["gkp"][0] + 1]
    R = slice(64, 96)

    P.dma("pool", lambda e: e.dma_start(out=wA, in_=w_in_v[:, :, 3472:3760]), "wA", writes=[("wA",)])
    P.dma("pool", lambda e: [e.dma_start(out=wpp[:, :, 0:16], in_=w_in_v[:, :, 3744:3760]),
                             e.dma_start(out=wpp[:, :, 16:32], in_=w_in_v[:, :, 3728:3744])], "wB", writes=[("wpp",)], n=2)
    P.dma("pool", lambda e: e.dma_start(out=wkvb, in_=w_kvb.rearrange("(k p) f -> p k f", p=128)), "wC", writes=[("wkvb",)])

    for j in range(4):
        ts = tsl(j)
        rope_tables(P, C, s, j, posi, ang, tmpf, tmpi, C96, S96)
        bk = []
        for c in range(2):
            b = C.bank()
            bk.append(b)

            def f_mm(eng, c=c, b=b, ts=ts):
                r = None
                for k in range(8):
                    r = eng.matmul(C.ps[:, b, :], wA[:, k, c * 128:(c + 1) * 128], C.hT[:, k, ts], start=(k == 0), stop=(k == 7))
                return r

            P.op("pe", f_mm, reads=[("wA",)] + [("hT", k, j) for k in range(8)], writes=[("ps", b)])
        bss = C.bank()
        for c in range(2):
            P.op("act", lambda e, c=c, bk=bk: e.activation(sq, C.ps[:, bk[c], :], AF.Square), reads=[("ps", bk[c])], writes=[("msq",)])
            P.op("pe", lambda e, c=c, bss=bss: e.matmul(C.ps[:, bss, :], C.ones_bf[:, :], sq, start=(c == 0), stop=(c == 1)),
                 reads=[("msq",), ("ones",)], writes=[("ps", bss)])
        P.op("act", lambda e, bss=bss: e.activation(rstd, C.ps[:, bss, :], AF.Ln, bias=epsc, scale=1.0 / 256), reads=[("ps", bss)], writes=[("mrstd",)])
        P.op("act", lambda e: e.activation(rstd, rstd, AF.Exp, scale=-0.5), reads=[("mrstd",)], writes=[("mrstd",)])
        for c in range(2):
            P.op("dve", lambda e, c=c, bk=bk: e.scalar_tensor_tensor(kvn[:, c, :], C.ps[:, bk[c], :], C.consts[:, kvan + c:kvan + c + 1], rstd, ALU.mult, ALU.mult),
                 reads=[("ps", bk[c]), ("mrstd",)], writes=[("kvn", c)])
        bx = C.bank()
        by = C.bank()

        def f_pe(eng, bx=bx, ts=ts):
            r = None
            for k in range(8):
                r = eng.matmul(C.ps[64:96, bx, :], wA[:, k, 256:288], C.hT[:, k, ts], start=(k == 0), stop=(k == 7), tile_position=(0, 64))
            return r

        def f_pp(eng, by=by, ts=ts):
            r = None
            for k in range(8):
                r = eng.matmul(C.ps[64:96, by, :], wpp[:, k, :], C.hT[:, k, ts], start=(k == 0), stop=(k == 7), tile_position=(0, 64))
            return r

        P.op("pe", f_pe, reads=[("wA",)] + [("hT", k, j) for k in range(8)], writes=[("ps", bx)])
        P.op("pe", f_pp, reads=[("wpp",)] + [("hT", k, j) for k in range(8)], writes=[("ps", by)])
        P.op("dve", lambda e, bx=bx: e.scalar_tensor_tensor(kr[R, :], C.ps[R, bx, :], gk[R, :], C96[R, :], ALU.mult, ALU.mult),
             reads=[("ps", bx), ("C96",)], writes=[("kr",)])
        P.op("dve", lambda e, by=by: e.scalar_tensor_tensor(t1[R, :], C.ps[R, by, :], gkp[R, :], S96[R, :], ALU.mult, ALU.mult),
             reads=[("ps", by), ("S96",)], writes=[("t1",)])
        P.op("dve", lambda e: e.tensor_tensor(kr[R, :], kr[R, :], t1[R, :], ALU.add), reads=[("kr",), ("t1",)], writes=[("kr",)])
        P.op("act", lambda e, bx=bx: e.activation(sqk[R, :], C.ps[R, bx, :], AF.Square), reads=[("ps", bx)], writes=[("sqk",)])
        for h in range(8):
            b = C.bank()

            def f_kn(eng, h=h, b=b):
                r = None
                for k in range(2):
                    r = eng.matmul(C.ps[0:64, b, :], wkvb[:, k, h * 128:h * 128 + 64], kvn[:, k, :], start=(k == 0), stop=(k == 1))
                return r

            P.op("pe", f_kn, reads=[("wkvb",), ("kvn", 0), ("kvn", 1)], writes=[("ps", b)])
            P.op("act", lambda e, b=b: e.activation(sqk[0:64, :], C.ps[0:64, b, :], AF.Square), reads=[("ps", b)], writes=[("sqk",)])
            b2 = C.bank()
            P.op("pe", lambda e, b2=b2: e.matmul(C.ps[0:96, b2, :], C.ones_bf[0:96, 0:96], sqk[0:96, :], start=True, stop=True),
                 reads=[("sqk",), ("ones",)], writes=[("ps", b2)])
            P.op("act", lambda e, b2=b2: e.activation(rstd[0:96, :], C.ps[0:96, b2, :], AF.Ln, bias=epsc[0:96, :], scale=1.0 / 96),
                 reads=[("ps", b2)], writes=[("mrstd",)])
            P.op("act", lambda e: e.activation(rstd[0:96, :], rstd[0:96, :], AF.Exp, scale=-0.5), reads=[("mrstd",)], writes=[("mrstd",)])
            P.op("dve", lambda e, h=h, b=b, ts=ts: e.scalar_tensor_tensor(kT[0:64, h, ts], C.ps[0:64, b, :], gk[0:64, :], rstd[0:64, :], ALU.mult, ALU.mult),
                 reads=[("ps", b), ("mrstd",)], writes=[("kT", h, j)])
            P.op("dve", lambda e, h=h, ts=ts: e.tensor_tensor(kT[R, h, ts], kr[R, :], rstd[R, :], ALU.mult),
                 reads=[("kr",), ("mrstd",)], writes=[("kT", h, j)])
        for nl in range(4):
            n = 4 * j + nl
            b = C.bank()

            def f_v(eng, b=b, nl=nl):
                r = None
                for k in range(2):
                    r = eng.matmul(C.ps[:, b, :], kvn[:, k, nl * 128:(nl + 1) * 128],
                                   wkvb[:, k, :].rearrange("p (h x) -> p h x", h=8)[:, :, 64:128], start=(k == 0), stop=(k == 1))
                return r

            P.op("pe", f_v, reads=[("wkvb",), ("kvn", 0), ("kvn", 1)], writes=[("ps", b)])
            P.op("act", lambda e, b=b, n=n: e.copy(V_tok[:, n, :], C.ps[:, b, :]), reads=[("ps", b)], writes=[("V_tok", n)])


def mla_attn_stage(P, C, s):
    w_in = C.need("e_w_in", [D, 3760], lambda inp: inp["e_w_in"][0])
    w_qb = C.need("e_w_q_b", [384, 768], lambda inp: inp["e_w_q_b"][0])
    w_out = C.need("e_w_out", [1536, D], lambda inp: inp["e_w_out"][0])
    cc = C.ccols
    A = Arena(C)
    kT = A.bf(8, T)
    V_tok = A.bf(16, 512)
    wA = A.bf(8, 384)
    wqb = A.bf(3, 768)
    wqbp = A.bf(3, 8, 32)
    woM = A.bf(4, D)
    qan = A.bf(3, 512)
    sq = A.bf(512)
    rstd = A.f32(512)
    posi = A.i32(512)
    ang = A.f32(512)
    tmpf = A.f32(512)
    tmpi = A.i32(512)
    C96 = A.f32(512)
    S96 = A.f32(512)
    t1 = A.f32(512)
    t2 = A.f32(512)
    qT = [A.bf(512) for _ in range(2)]
    Pb = [A.bf(512) for _ in range(4)]
    rden = A.f32(512)
    oT = A.bf(4, 512)
    w_in_v = w_in.rearrange("(k p) f -> p k f", p=128)
    epsc = C.consts[:, C.eps_col:C.eps_col + 1]
    qanc = cc["e_qan"][0]
    gq = C.consts[:, cc["gq"][0]:cc["gq"][0] + 1]
    gqp = C.consts[:, cc["gqp"][0]:cc["gqp"][0] + 1]
    R = slice(64, 96)
    SC = float(96 ** -0.5)
    w_qb_v = w_qb.rearrange("(k p) (h x) -> p k h x", p=128, h=8)

    P.dma("pool", lambda e: e.dma_start(out=wA, in_=w_in_v[:, :, 3088:3472]), "wA", writes=[("wA",)])
    P.dma("pool", lambda e: e.dma_start(out=wqb, in_=w_qb.rearrange("(k p) f -> p k f", p=128)), "wB", writes=[("wqb",)])
    P.dma("pool", lambda e: [e.dma_start(out=wqbp[:, k, :, 0:16], in_=w_qb_v[:, k, :, 80:96]) for k in range(3)]
          + [e.dma_start(out=wqbp[:, k, :, 16:32], in_=w_qb_v[:, k, :, 64:80]) for k in range(3)], "wC", writes=[("wqbp",)], n=6)
    P.dma("pool", lambda e: e.dma_start(out=woM, in_=w_out[1024:1536, :].rearrange("(c p) d -> p c d", p=128)), "wD", writes=[("woM",)])
    pbi = [0]
    qi = [0]

    for j in range(4):
        ts = tsl(j)
        rope_tables(P, C, s, j, posi, ang, tmpf, tmpi, C96, S96)
        bq = []
        for c in range(3):
            b = C.bank()
            bq.append(b)

            def f_mm(eng, c=c, b=b, ts=ts):
                r = None
                for k in range(8):
                    r = eng.matmul(C.ps[:, b, :], wA[:, k, c * 128:(c + 1) * 128], C.hT[:, k, ts], start=(k == 0), stop=(k == 7))
                return r

            P.op("pe", f_mm, reads=[("wA",)] + [("hT", k, j) for k in range(8)], writes=[("ps", b)])
        bss = C.bank()
        for c in range(3):
            P.op("act", lambda e, c=c, bq=bq: e.activation(sq, C.ps[:, bq[c], :], AF.Square), reads=[("ps", bq[c])], writes=[("msq",)])
            P.op("pe", lambda e, c=c, bss=bss: e.matmul(C.ps[:, bss, :], C.ones_bf[:, :], sq, start=(c == 0), stop=(c == 2)),
                 reads=[("msq",), ("ones",)], writes=[("ps", bss)])
        P.op("act", lambda e, bss=bss: e.activation(rstd, C.ps[:, bss, :], AF.Sqrt, bias=epsc, scale=1.0 / 384), reads=[("ps", bss)], writes=[("mrstd",)])
        P.op("dve", lambda e: e.reciprocal(rstd, rstd), reads=[("mrstd",)], writes=[("mrstd",)])
        for c in range(3):
            P.op("dve", lambda e, c=c, bq=bq: e.scalar_tensor_tensor(qan[:, c, :], C.ps[:, bq[c], :], C.consts[:, qanc + c:qanc + c + 1], rstd, ALU.mult, ALU.mult),
                 reads=[("ps", bq[c]), ("mrstd",)], writes=[("qan", c)])
        def qA(h):
            b = C.bank()
            bp = C.bank()

            def f_q(eng, h=h, b=b, bp=bp):
                r = None
                for k in range(3):
                    r = eng.matmul(C.ps[0:96, b, :], wqb[:, k, h * 96:(h + 1) * 96], qan[:, k, :], start=(k == 0), stop=(k == 2))
                for k in range(3):
                    r = eng.matmul(C.ps[64:96, bp, :], wqbp[:, k, h, :], qan[:, k, :], start=(k == 0), stop=(k == 2), tile_position=(0, 64))
                return r

            P.op("pe", f_q, reads=[("wqb",), ("wqbp",)] + [("qan", c) for c in range(3)], writes=[("ps", b), ("ps", bp)])
            P.op("act", lambda e, b=b: e.activation(sq[0:96, :], C.ps[0:96, b, :], AF.Square), reads=[("ps", b)], writes=[("msq",)])
            return b, bp

        def qB(h, b, bp):
            b2 = C.bank()
            P.op("pe", lambda e, b2=b2: e.matmul(C.ps[0:96, b2, :], C.ones_bf[0:96, 0:96], sq[0:96, :], start=True, stop=True),
                 reads=[("msq",), ("ones",)], writes=[("ps", b2)])
            P.op("act", lambda e, b2=b2: e.activation(rstd[0:96, :], C.ps[0:96, b2, :], AF.Sqrt, bias=epsc[0:96, :], scale=1.0 / 96),
                 reads=[("ps", b2)], writes=[("mrstd",)])
            P.op("dve", lambda e: e.reciprocal(rstd[0:96, :], rstd[0:96, :]), reads=[("mrstd",)], writes=[("mrstd",)])
            P.op("dve", lambda e, b=b: e.scalar_tensor_tensor(t1[0:96, :], C.ps[0:96, b, :], gq[0:96, :], C96[0:96, :], ALU.mult, ALU.mult),
                 reads=[("ps", b), ("C96",)], writes=[("t1",)])
            P.op("dve", lambda e, bp=bp: e.scalar_tensor_tensor(t2[R, :], C.ps[R, bp, :], gqp[R, :], S96[R, :], ALU.mult, ALU.mult),
                 reads=[("ps", bp), ("S96",)], writes=[("t2",)])
            P.op("dve", lambda e: e.tensor_tensor(t1[R, :], t1[R, :], t2[R, :], ALU.add), reads=[("t1",), ("t2",)], writes=[("t1",)])
            qk = qi[0]
            qi[0] = (qi[0] + 1) % 2
            Q = qT[qk]
            P.op("dve", lambda e, Q=Q: e.tensor_tensor(Q[0:96, :], t1[0:96, :], rstd[0:96, :], ALU.mult), reads=[("t1",), ("mrstd",)], writes=[("qT", qk)])
            return qk, Q

        LOOK = 2
        nxt = qB(0, *qA(0))
        for h in range(8):
            hi = h % 2
            qk, Q = nxt
            pend_q = qA(h + 1) if h + 1 < 8 else None
            nkb = 4 * j + 4
            po = slice(hi * 64, hi * 64 + 64)
            kw = {"tile_position": (0, 64)} if hi == 1 else {}

            def emit_st(kb, h=h, Q=Q, qk=qk):
                b = C.bank()
                P.op("pe", lambda e, b=b, h=h, kb=kb, Q=Q: e.matmul(C.ps[:, b, :], kT[0:96, h, kb * 128:(kb + 1) * 128], Q[0:96, :], start=True, stop=True),
                     reads=[("kT", h, kb // 4), ("qT", qk)], writes=[("ps", b)])
                pk = pbi[0]
                pbi[0] = (pbi[0] + 1) % 4
                PB = Pb[pk]
                P.op("act", lambda e, b=b, PB=PB: e.activation(PB, C.ps[:, b, :], AF.Exp, scale=SC), reads=[("ps", b)], writes=[("Pb", pk)])
                if kb >= 4 * j:
                    o = (kb - 4 * j) * 128
                    m0 = 512 + 384 - o
                    P.op("pool", lambda e, PB=PB, m0=m0: e.tensor_tensor(PB, PB, C.maskb[:, m0:m0 + 512], ALU.mult),
                         reads=[("Pb", pk), ("mask",)], writes=[("Pb", pk)])
                return pk, PB

            def emit_pv(kb, pk, PB, h=h, po=po, kw=kw, nkb=nkb):
                def f_pv(eng):
                    eng.matmul(C.ps[po, 0, :], V_tok[:, kb, h * 64:(h + 1) * 64], PB, start=(kb == 0), stop=(kb == nkb - 1), skip_group_check=True, **kw)
                    return eng.matmul(C.ps[po, 1, :], C.ones_bf[:, 0:64], PB, start=(kb == 0), stop=(kb == nkb - 1), skip_group_check=True, **kw)

                P.op("pe", f_pv, reads=[("V_tok", kb), ("Pb", pk), ("ones",)], writes=[("ps", 0), ("ps", 1)])

            pend = []
            for kb in range(nkb):
                pend.append((kb,) + emit_st(kb))
                if kb == 1 and pend_q is not None:
                    nxt = qB(h + 1, *pend_q)
                    pend_q = None
                if len(pend) > LOOK:
                    emit_pv(*pend.pop(0))
            while pend:
                emit_pv(*pend.pop(0))
            if hi == 1:
                P.op("dve", lambda e: e.reciprocal(rden, C.ps[:, 1, :]), reads=[("ps", 1)], writes=[("rden",)])
                P.op("dve", lambda e, h=h: e.tensor_tensor(oT[:, h // 2, :], C.ps[:, 0, :], rden, ALU.mult), reads=[("ps", 0), ("rden",)], writes=[("oT",)])
        for d in range(8):
            b = C.bank()

            def f_mm(eng, d=d, b=b):
                r = None
                for c in range(4):
                    r = eng.matmul(C.ps[:, b, :], woM[:, c, d * 128:(d + 1) * 128], oT[:, c, :], start=(c == 0), stop=(c == 3))
                return r

            P.op("pe", f_mm, reads=[("woM",), ("oT",)], writes=[("ps", b)])
            P.op("dve", lambda e, d=d, b=b, ts=ts: e.tensor_tensor(C.xT[:, d, ts], C.ps[:, b, :], C.xT[:, d, ts], ALU.add),
                 reads=[("ps", b), ("xT", d, j)], writes=[("xT", d, j)])


def load_x(P, C, xin):
    v = xin.rearrange("(k p) t -> p k t", p=128)
    for k in range(8):
        def f(eng, k=k):
            return eng.dma_start(out=C.xT[:, k, :], in_=v[:, k, :])

        P.dma("sp", f, f"xin{k}", writes=[("xT", k, j) for j in range(4)])


def store_x(P, C, yout):
    v = yout.rearrange("(k p) t -> p k t", p=128)
    for k in range(8):
        def f(eng, k=k):
            return eng.dma_start(out=v[:, k, :], in_=C.xT[:, k, :])

        P.dma("sp", f, f"xout{k}", reads=[("xT", k, j) for j in range(4)])


def build(nseq, stages, ccols, ncc):
    nc = bass.Bass("TRN2", target_bir_lowering=False)
    dr = {}
    hostprep = {}

    def din(name, shape, dt=F32):
        dr[name] = nc.dram_tensor(name, list(shape), dt, kind="ExternalInput").ap()
        return dr[name]

    def need(name, shape, fn, dt=F32):
        if name not in dr:
            din(name, shape, dt)
            hostprep[name] = fn
        return dr[name]

    xin = din("xT_in", [nseq * D, T])
    din("consts", [128, ncc])
    din("maskc", [128, NMASK])
    din("maskb", [128, NMASKB])
    yout = nc.dram_tensor("yT_out", [nseq * D, T], F32, kind="ExternalOutput").ap()

    P = Prog()
    C = Ctx()
    C.need = need
    C.ccols = ccols
    with ExitStack() as es:
        def sb(name, shape, dt):
            return es.enter_context(nc.sbuf_tensor(name, list(shape), dt))

        C.xT = sb("xT", [128, 8, T], F32)
        C.hT = sb("hT", [128, 8, T], BF16)
        C.consts = sb("consts_sb", [128, ncc], F32)
        C.ones_bf = sb("ones_bf", [128, 128], BF16)
        C.ar = sb("arena", [128, ARENA // 2], BF16)
        C.ar_f = C.ar.bitcast(F32)
        C.ar_i = C.ar.bitcast(I32)
        C.mask = sb("maskc_sb", [128, NMASK], F32)
        C.ident_bf = sb("ident_bf", [128, 128], BF16)
        C.maskb = sb("maskb_sb", [128, NMASKB], BF16)
        C.bd_bf = sb("bd_bf", [128, 128], BF16)
        C.pos = din("pos", [nseq, T], I32)
        C.posT = din("posT", [nseq, 128, 16], I32)
        C.dr = dr
        ffn_alloc(C)
        C.ps = es.enter_context(nc.psum_tensor("ps", [128, 8, 512], F32))
        C.wslot = 0
        C.eps_col = ccols['eps'][0]
        C.sgslot = 0
        C.nbank = 0
        C.pmi = 0

        def bank():
            b = 2 + C.nbank
            C.nbank = (C.nbank + 1) % 6
            return b

        C.bank = bank

        P.dma("sp", lambda eng: eng.dma_start(out=C.consts[:, :], in_=dr["consts"]), "consts", writes=[("consts",)])
        P.dma("sp", lambda eng: eng.dma_start(out=C.mask[:, :], in_=dr["maskc"]), "maskc", writes=[("mask",)])
        P.dma("pool", lambda eng: eng.dma_start(out=C.maskb[:, :], in_=dr["maskb"]), "maskb", writes=[("mask",)])
        P.op("dve", lambda eng: eng.memset(C.ones_bf[:, :], 1.0), writes=[("ones",)])
        P.op("dve", lambda eng: eng.tensor_copy(C.bd_bf[:, :], C.mask[:, 256:384]), reads=[("mask",)], writes=[("bd",)])
        P.op("dve", lambda eng: eng.tensor_copy(C.ident_bf[:, :], C.mask[:, 512:640]), reads=[("mask",)], writes=[("ident",)])
        P.barrier()

        for s in range(nseq):
            load_x(P, C, xin[s * D:(s + 1) * D, :])
            for st in stages:
                kind = st[0]
                if kind == "ffn":
                    _, nm, l = st
                    wg = need(f"{nm}_w_gate{l}", [D, DFF], lambda inp, nm=nm, l=l: inp[f"{nm}_w_gate"][l])
                    wu = need(f"{nm}_w_up{l}", [D, DFF], lambda inp, nm=nm, l=l: inp[f"{nm}_w_up"][l])
                    wd = need(f"{nm}_w_down{l}", [DFF, D], lambda inp, nm=nm, l=l: inp[f"{nm}_w_down"][l])
                    ffn_stage(P, C, ccols[f"{nm}_norm{l}"][0], wg, wu, wd)
                elif kind == "norm":
                    rmsnorm_stage(P, C, ccols[f"mix_norm{st[1]}"][0])
                elif kind == "swa":
                    swa_stage(P, C, s)
                elif kind == "gla":
                    gla_stage(P, C, s)
                elif kind == "ssd":
                    ssd_stage(P, C, s)
                elif kind == "mla":
                    mla_kv_stage(P, C, s)
                    P.barrier()
                    if os.environ.get("DBG_DUMP"):
                        dk = nc.dram_tensor("dbg_k", [128, 8 * T], BF16, kind="ExternalOutput").ap()
                        dv = nc.dram_tensor("dbg_v", [128, 16 * 512], BF16, kind="ExternalOutput").ap()
                        P.dma("sp", lambda e: [e.dma_start(out=dk[:, i * 1024:(i + 1) * 1024], in_=C.ar[:, i * 1024:(i + 1) * 1024]) for i in range(16)], "dbgk", n=16)
                        P.dma("sp", lambda e: [e.dma_start(out=dv[:, i * 1024:(i + 1) * 1024], in_=C.ar[:, 8 * T + i * 1024:8 * T + (i + 1) * 1024]) for i in range(8)], "dbgv", n=8)
                        P.barrier()
                    else:
                        mla_attn_stage(P, C, s)
                else:
                    raise ValueError(kind)
                P.barrier()
            store_x(P, C, yout[s * D:(s + 1) * D, :])
        P.emit(nc)
    return nc, hostprep


ALL_STAGES = [("ffn", "pre", 0), ("norm", 0), ("ssd", 0), ("mla", 0), ("ffn", "post", 0),
              ("ffn", "pre", 1), ("norm", 1), ("swa", 1), ("gla", 1), ("ffn", "post", 1)]


def run(inputs, stages=ALL_STAGES, ncores=8, nseq=4, trace=False):
    x = np.asarray(inputs["x"], np.float32)
    pos = np.asarray(inputs["positions"], np.int32)
    consts, ccols = pack_consts(inputs)
    nc, hostprep = build(nseq, stages, ccols, consts.shape[1])
    mk = make_masks()
    shared = {"consts": consts, "maskc": mk[0], "maskb": mk[1]}
    for name, fn in hostprep.items():
        shared[name] = np.ascontiguousarray(np.asarray(fn(inputs), np.float32))
    in_maps = []
    for c in range(ncores):
        xs = x[c * nseq:(c + 1) * nseq]
        xT = np.ascontiguousarray(xs.transpose(0, 2, 1)).reshape(nseq * D, T)
        ps = np.ascontiguousarray(pos[c * nseq:(c + 1) * nseq])
        pT = np.ascontiguousarray(ps.reshape(nseq, 16, 128).transpose(0, 2, 1))
        m = {"xT_in": xT, "pos": ps, "posT": pT}
        m.update(shared)
        in_maps.append(m)
    res = run_bass_kernel_spmd(nc, in_maps, core_ids=list(range(ncores)), trace=trace)
    outs = []
    for c in range(ncores):
        yT = np.asarray(res.results[c]["yT_out"]).reshape(nseq, D, T)
        outs.append(yT.transpose(0, 2, 1))
    out = np.ascontiguousarray(np.concatenate(outs, axis=0)).astype(np.float32)
    return out, res


def kernel(**inputs):
    out, _ = run(inputs)
    return out
```
